# Optimizing a Trainium2 kernel written in Bass

```python
import math
import jax, jax.numpy as jnp
from jax import lax
import numpy as np

D_MODEL = 1024
BATCH = 2
SEQ = 16384
DEPTH = 1
DEC_BATCH = 32
DEC_SEQ = 32
PAST_LEN = 1024

CHUNK = 64
HEAD_DIM = 64
A_HEADS = D_MODEL // (2 * HEAD_DIM)
A_BAND = 8
A_REL_CLIP = 128
B_Q_HEADS = D_MODEL // (2 * HEAD_DIM)
B_KV_HEADS = 2
B_GROUP = B_Q_HEADS // B_KV_HEADS
B_WINDOW = 128
B_BAND = B_WINDOW // CHUNK
T5_BUCKETS = 32
T5_MAX_DIST = 128
PLE_DIM = 256
N_GROUPS = 4
EXPERTS_PER_GROUP = 8
N_EXPERTS = N_GROUPS * EXPERTS_PER_GROUP
TOP_K_IN_GROUP = 2
D_EXPERT = D_MODEL // 4
A_WIDTH = A_HEADS * HEAD_DIM
B_Q_WIDTH = B_Q_HEADS * HEAD_DIM
B_KV_WIDTH = B_KV_HEADS * HEAD_DIM
IN_SPLITS = [A_WIDTH, A_WIDTH, A_WIDTH, B_Q_WIDTH, B_KV_WIDTH, B_KV_WIDTH, D_MODEL, D_MODEL]
IN_WIDTH = sum(IN_SPLITS)
ALPHA = (2 * DEPTH) ** 0.25
BETA = (8 * DEPTH) ** -0.25
LN_EPS = 1e-5
NEG_INF = -1e30

kernel_name = "chunk_streaming_hybrid_encoder_step"


def layer_norm(x, g, b):
    xf = x.astype(jnp.float32)
    mu = jnp.mean(xf, axis=-1, keepdims=True)
    var = jnp.mean(jnp.square(xf - mu), axis=-1, keepdims=True)
    y = (xf - mu) * lax.rsqrt(var + LN_EPS) * g.astype(jnp.float32) + b.astype(jnp.float32)
    return y.astype(x.dtype)


def clipped_rel_bias(rel, table):
    idx = jnp.clip(rel, -A_REL_CLIP, A_REL_CLIP) + A_REL_CLIP
    return table[idx]


def t5_rel_bias(rel, table):
    mem_minus_q = -rel
    nb = T5_BUCKETS // 2
    max_exact = nb // 2
    base = jnp.where(mem_minus_q > 0, nb, 0)
    n = jnp.abs(mem_minus_q)
    nf = jnp.maximum(n, 1).astype(jnp.float32)
    large = max_exact + (jnp.log(nf / max_exact) / math.log(T5_MAX_DIST / max_exact)
                         * (nb - max_exact)).astype(jnp.int32)
    large = jnp.minimum(large, nb - 1)
    bucket = base + jnp.where(n < max_exact, n, large)
    return table[bucket]


def band_softmax_attention(q, k, v, key_valid, bias, sink):
    _, _, lq, hkv, g, d = q.shape
    lk = k.shape[2]
    logits = jnp.einsum('bnqhgd,bnkhd->bnhgqk', q, k,
                        preferred_element_type=jnp.float32) * (d ** -0.5)
    bias = jnp.transpose(bias.astype(jnp.float32), (2, 0, 1)).reshape(hkv, g, lq, lk)
    logits = logits + bias
    logits = jnp.where(key_valid[None, :, None, None, None, :], logits, NEG_INF)
    if sink is not None:
        s = jnp.broadcast_to(sink.astype(jnp.float32).reshape(1, 1, hkv, g, 1, 1),
                             logits.shape[:-1] + (1,))
        probs = jax.nn.softmax(jnp.concatenate([logits, s], axis=-1), axis=-1)[..., :-1]
    else:
        probs = jax.nn.softmax(logits, axis=-1)
    return jnp.einsum('bnhgqk,bnkhd->bnqhgd', probs.astype(v.dtype), v)


def chunk_band_prompt(q, k, v, band, bias_fn, table, sink):
    b, t, hkv, g, d = q.shape
    n_c = t // CHUNK
    lk = (band + 1) * CHUNK
    pad = jnp.zeros((b, band * CHUNK, hkv, d), k.dtype)
    kp = jnp.concatenate([pad, k], axis=1).reshape(b, n_c + band, CHUNK, hkv, d)
    vp = jnp.concatenate([pad.astype(v.dtype), v], axis=1).reshape(b, n_c + band, CHUNK, hkv, d)
    kb = jnp.concatenate([kp[:, j:j + n_c] for j in range(band + 1)], axis=2)
    vb = jnp.concatenate([vp[:, j:j + n_c] for j in range(band + 1)], axis=2)
    rel = jnp.arange(CHUNK)[:, None] + band * CHUNK - jnp.arange(lk)[None, :]
    k_pos = (jnp.arange(n_c)[:, None] - band) * CHUNK + jnp.arange(lk)[None, :]
    out = band_softmax_attention(q.reshape(b, n_c, CHUNK, hkv, g, d), kb, vb,
                                 k_pos >= 0, bias_fn(rel, table), sink)
    return out.reshape(b, t, hkv * g * d)


def chunk_band_sample(q, k, v, k_cache, v_cache, bias_fn, table, sink):
    b, s, hkv, g, d = q.shape
    keep = k_cache.shape[1]
    kf = jnp.concatenate([k_cache.astype(k.dtype), k], axis=1)
    vf = jnp.concatenate([v_cache.astype(v.dtype), v], axis=1)
    rel = jnp.arange(s)[:, None] + keep - jnp.arange(keep + s)[None, :]
    valid = jnp.ones((1, keep + s), dtype=bool)
    out = band_softmax_attention(q[:, None], kf[:, None], vf[:, None], valid,
                                 bias_fn(rel, table), sink)
    return out.reshape(b, s, hkv * g * d), kf[:, -keep:], vf[:, -keep:]


def hier_moe(x, w_rg, b_rg, w_re, b_re, w1, w3, w2):
    shp = x.shape
    xf = x.reshape(-1, D_MODEL)
    n = xf.shape[0]
    g_logits = (xf @ w_rg).astype(jnp.float32) + b_rg.astype(jnp.float32)
    g_prob = jax.nn.softmax(g_logits, axis=-1)
    grp = jnp.argmax(g_logits, axis=-1)
    g_w = jnp.take_along_axis(g_prob, grp[:, None], axis=-1)
    e_logits = ((xf @ w_re).astype(jnp.float32) + b_re.astype(jnp.float32)).reshape(
        n, N_GROUPS, EXPERTS_PER_GROUP)
    e_sel = jnp.take_along_axis(e_logits, grp[:, None, None], axis=1)[:, 0]
    top_v, top_i = lax.top_k(e_sel, TOP_K_IN_GROUP)
    w = jax.nn.softmax(top_v, axis=-1) * g_w
    eid = grp[:, None] * EXPERTS_PER_GROUP + top_i
    combine = jnp.sum(jax.nn.one_hot(eid, N_EXPERTS, dtype=jnp.float32) * w[..., None], axis=1)
    combine = combine.astype(x.dtype)
    y = jnp.zeros_like(xf)
    for e in range(N_EXPERTS):
        hdn = jax.nn.silu(xf @ w1[e]) * (xf @ w3[e])
        y = y + combine[:, e:e + 1] * (hdn @ w2[e])
    return y.reshape(shp)


def encoder_layer(x, p, w_in, a_rel_table, b_sinks, w_pa, w_pb, w_o, ln1_g, ln1_b,
                  w_rg, b_rg, w_re, b_re, w1, w3, w2, w_pe, w_pg, ln2_g, ln2_b,
                  t5_table, cache):
    b, t, _ = x.shape
    h = x @ w_in
    qa, ka, va, qb, kb, vb, ga, gb = jnp.split(h, np.cumsum(IN_SPLITS)[:-1].tolist(), axis=-1)
    qa = qa.reshape(b, t, A_HEADS, 1, HEAD_DIM)
    ka = ka.reshape(b, t, A_HEADS, HEAD_DIM)
    va = va.reshape(b, t, A_HEADS, HEAD_DIM)
    qb = qb.reshape(b, t, B_KV_HEADS, B_GROUP, HEAD_DIM)
    kb = kb.reshape(b, t, B_KV_HEADS, HEAD_DIM)
    vb = vb.reshape(b, t, B_KV_HEADS, HEAD_DIM)
    if cache is None:
        ya = chunk_band_prompt(qa, ka, va, A_BAND, clipped_rel_bias, a_rel_table, None)
        yb = chunk_band_prompt(qb, kb, vb, B_BAND, t5_rel_bias, t5_table, b_sinks)
        keep_a = min(A_BAND * CHUNK, t)
        keep_b = min(B_BAND * CHUNK, t)
        new_state = (ka[:, -keep_a:], va[:, -keep_a:], kb[:, -keep_b:], vb[:, -keep_b:])
    else:
        ck_a, cv_a, ck_b, cv_b = cache
        ya, nka, nva = chunk_band_sample(qa, ka, va, ck_a, cv_a, clipped_rel_bias, a_rel_table, None)
        yb, nkb, nvb = chunk_band_sample(qb, kb, vb, ck_b, cv_b, t5_rel_bias, t5_table, b_sinks)
        new_state = (nka, nva, nkb, nvb)
    merged = jax.nn.sigmoid(ga) * (ya @ w_pa) + jax.nn.sigmoid(gb) * (yb @ w_pb)
    x1 = layer_norm(ALPHA * x + merged @ w_o, ln1_g, ln1_b)
    ffn = hier_moe(x1, w_rg, b_rg, w_re, b_re, w1, w3, w2)
    ple = jax.nn.sigmoid(x1 @ w_pg) * (p @ w_pe)
    x2 = layer_norm(ALPHA * x1 + ffn + ple, ln2_g, ln2_b)
    return x2, new_state


def setup_inputs(seed: int = 0) -> dict:
    key = jax.random.key(seed)
    ks = jax.random.split(key, 32)

    def nrm(k, shape, scale=1.0):
        return jax.random.normal(k, shape, jnp.float32) * scale

    a_keep = min(A_BAND * CHUNK, PAST_LEN)
    b_keep = min(B_BAND * CHUNK, PAST_LEN)
    return {
        "x_prompt": nrm(ks[0], (BATCH, SEQ, D_MODEL)),
        "x_sample": nrm(ks[1], (DEC_BATCH, DEC_SEQ, D_MODEL)),
        "p_prompt": nrm(ks[2], (DEPTH, BATCH, SEQ, PLE_DIM)),
        "p_sample": nrm(ks[3], (DEPTH, DEC_BATCH, DEC_SEQ, PLE_DIM)),
        "cache_a_k": nrm(ks[4], (DEPTH, DEC_BATCH, a_keep, A_HEADS, HEAD_DIM)),
        "cache_a_v": nrm(ks[5], (DEPTH, DEC_BATCH, a_keep, A_HEADS, HEAD_DIM)),
        "cache_b_k": nrm(ks[6], (DEPTH, DEC_BATCH, b_keep, B_KV_HEADS, HEAD_DIM)),
        "cache_b_v": nrm(ks[7], (DEPTH, DEC_BATCH, b_keep, B_KV_HEADS, HEAD_DIM)),
        "w_in": nrm(ks[8], (DEPTH, D_MODEL, IN_WIDTH), D_MODEL ** -0.5),
        "a_rel_table": nrm(ks[9], (DEPTH, 2 * A_REL_CLIP + 1, A_HEADS), 0.1),
        "b_sinks": nrm(ks[10], (DEPTH, B_Q_HEADS), 0.5),
        "w_pa": nrm(ks[11], (DEPTH, A_WIDTH, D_MODEL), A_WIDTH ** -0.5),
        "w_pb": nrm(ks[12], (DEPTH, B_Q_WIDTH, D_MODEL), B_Q_WIDTH ** -0.5),
        "w_o": nrm(ks[13], (DEPTH, D_MODEL, D_MODEL), BETA * D_MODEL ** -0.5),
        "ln1_g": 1.0 + nrm(ks[14], (DEPTH, D_MODEL), 0.01),
        "ln1_b": nrm(ks[15], (DEPTH, D_MODEL), 0.01),
        "w_rg": nrm(ks[16], (DEPTH, D_MODEL, N_GROUPS), D_MODEL ** -0.5),
        "b_rg": nrm(ks[17], (DEPTH, N_GROUPS), 0.01),
        "w_re": nrm(ks[18], (DEPTH, D_MODEL, N_EXPERTS), D_MODEL ** -0.5),
        "b_re": nrm(ks[19], (DEPTH, N_EXPERTS), 0.01),
        "w1": nrm(ks[20], (DEPTH, N_EXPERTS, D_MODEL, D_EXPERT), D_MODEL ** -0.5),
        "w3": nrm(ks[21], (DEPTH, N_EXPERTS, D_MODEL, D_EXPERT), D_MODEL ** -0.5),
        "w2": nrm(ks[22], (DEPTH, N_EXPERTS, D_EXPERT, D_MODEL), BETA * D_EXPERT ** -0.5),
        "w_pe": nrm(ks[23], (DEPTH, PLE_DIM, D_MODEL), PLE_DIM ** -0.5),
        "w_pg": nrm(ks[24], (DEPTH, D_MODEL, D_MODEL), D_MODEL ** -0.5),
        "ln2_g": 1.0 + nrm(ks[25], (DEPTH, D_MODEL), 0.01),
        "ln2_b": nrm(ks[26], (DEPTH, D_MODEL), 0.01),
        "t5_table": nrm(ks[27], (T5_BUCKETS, B_Q_HEADS), 0.1),
    }


def reference(x_prompt, x_sample, p_prompt, p_sample, cache_a_k, cache_a_v, cache_b_k,
              cache_b_v, w_in, a_rel_table, b_sinks, w_pa, w_pb, w_o, ln1_g, ln1_b,
              w_rg, b_rg, w_re, b_re, w1, w3, w2, w_pe, w_pg, ln2_g, ln2_b, t5_table):
    yp, ys = x_prompt, x_sample
    pak, pav, pbk, pbv = [], [], [], []
    sak, sav, sbk, sbv = [], [], [], []
    for i in range(DEPTH):
        lw = (w_in[i], a_rel_table[i], b_sinks[i], w_pa[i], w_pb[i], w_o[i], ln1_g[i], ln1_b[i],
              w_rg[i], b_rg[i], w_re[i], b_re[i], w1[i], w3[i], w2[i], w_pe[i], w_pg[i],
              ln2_g[i], ln2_b[i], t5_table)
        yp, (ak, av, bk, bv) = encoder_layer(yp, p_prompt[i], *lw, None)
        ys, (sk_a, sv_a, sk_b, sv_b) = encoder_layer(
            ys, p_sample[i], *lw, (cache_a_k[i], cache_a_v[i], cache_b_k[i], cache_b_v[i]))
        pak.append(ak); pav.append(av); pbk.append(bk); pbv.append(bv)
        sak.append(sk_a); sav.append(sv_a); sbk.append(sk_b); sbv.append(sv_b)
    return (yp, ys, jnp.stack(pak), jnp.stack(pav), jnp.stack(pbk), jnp.stack(pbv),
            jnp.stack(sak), jnp.stack(sav), jnp.stack(sbk), jnp.stack(sbv))
```

```python
import math
from contextlib import ExitStack

import numpy as np

import concourse.bass as bass
import concourse.mybir as mybir
from concourse.bass_utils import run_bass_kernel_spmd

F32 = mybir.dt.float32
BF16 = mybir.dt.bfloat16
AF = mybir.ActivationFunctionType
ALU = mybir.AluOpType

NCORES = 8
D = 1024
SEG = 4096
HALO = 512
NS = 128
NBLK = 8
ALPHA = 2.0 ** 0.25
MASKV = -240000.0
NSLOT = 5
SLOT = 4096
SAME_ENGINE_SYNC = True

CH_ORDER = (["K1", "K2", "Q1", "Q2", "TVA", "TVB", "TKA", "TKB"] + ["D%d" % j for j in range(8)]
            + ["WO0", "WO1"])
for _e in range(32):
    CH_ORDER += ["EA%d" % _e, "EB%d" % _e]
CH_ORDER += ["PG0", "PG1", "PE"]
CH_SIZE = {"K1": 4096, "K2": 2048, "Q1": 4096, "Q2": 4096, "TVA": 4096, "TVB": 1024, "TKA": 4096,
           "TKB": 1024, "WO0": 4096, "WO1": 4096, "PG0": 4096, "PG1": 4096, "PE": 2048}
for _j in range(8):
    CH_SIZE["D%d" % _j] = 3072
for _e in range(32):
    CH_SIZE["EA%d" % _e] = 4096
    CH_SIZE["EB%d" % _e] = 2048
CH_OFF = {}
_o = 0
for _n in CH_ORDER:
    CH_OFF[_n] = _o
    _o += CH_SIZE[_n]
FTOT = _o
PIECES = ([("K1", "TKB"), ("D0", "D7"), ("WO0", "WO1")]
          + [("EA%d" % (2 * _i), "EB%d" % (2 * _i + 1)) for _i in range(16)] + [("PG0", "PE")])
CH_PIECE = {}
for _pi, (_a, _b) in enumerate(PIECES):
    for _n in CH_ORDER[CH_ORDER.index(_a):CH_ORDER.index(_b) + 1]:
        CH_PIECE[_n] = _pi


def _t5_bucket(rel):
    m = -rel
    base = np.where(m > 0, 16, 0)
    n = np.abs(m)
    nf = np.maximum(n, 1).astype(np.float32)
    v = np.log(nf / np.float32(8)) / np.float32(math.log(16.0)) * np.float32(8)
    large = 8 + v.astype(np.int32)
    large = np.minimum(large, 15)
    return base + np.where(n < 8, n, large)


def _pack_weights(w_in, w_pa, w_pb, w_o, w1, w3, w2, w_pe, w_pg):
    W = np.empty((128, FTOT), np.float32)

    def put(name, arr, off=0):
        a = arr.reshape(128, -1)
        W[:, CH_OFF[name] + off:CH_OFF[name] + off + a.shape[1]] = a

    def fm(cols):
        return w_in[:, cols].reshape(8, 128, len(cols)).transpose(1, 0, 2)

    def kmaj(w, nk):
        return w.reshape(nk, 128, w.shape[1]).transpose(1, 0, 2)

    ar = np.arange
    put("K1", np.stack([fm(512 + g * 128 + ar(128)) for g in range(4)], 1))
    put("K2", np.stack([fm(np.concatenate([2048 + kv * 64 + ar(64)] * 2)) for kv in range(2)], 1))
    put("Q1", np.stack([fm(g * 128 + ar(128)) for g in range(4)], 1))
    qb = []
    for g in range(4):
        kv, a = g // 2, g % 2
        qb.append(fm(np.concatenate([1536 + (4 * kv + a) * 64 + ar(64), 1536 + (4 * kv + a + 2) * 64 + ar(64)])))
    put("Q2", np.stack(qb, 1))
    put("TVA", fm(1024 + ar(512)))
    put("TVB", fm(2176 + ar(128)))
    put("TKA", fm(512 + ar(512)))
    put("TKB", fm(2048 + ar(128)))
    pa = kmaj(w_pa, 4)
    pb = kmaj(w_pb, 4)
    for j in range(8):
        n = "D%d" % j
        put(n, fm(2304 + j * 128 + ar(128)), 0)
        put(n, fm(3328 + j * 128 + ar(128)), 1024)
        put(n, pa[:, :, j * 128:(j + 1) * 128], 2048)
        put(n, pb[:, :, j * 128:(j + 1) * 128], 2560)
    wo = kmaj(w_o, 8)
    put("WO0", wo[:, 0:4])
    put("WO1", wo[:, 4:8])
    for e in range(32):
        put("EA%d" % e, kmaj(w1[e], 8), 0)
        put("EA%d" % e, kmaj(w3[e], 8), 2048)
        put("EB%d" % e, kmaj(w2[e], 2))
    pg = kmaj(w_pg, 8)
    put("PG0", pg[:, 0:4])
    put("PG1", pg[:, 4:8])
    put("PE", kmaj(w_pe, 2))
    return W


class Buf:
    __slots__ = ("name", "w", "r", "excl")

    def __init__(self, name="", excl=False):
        self.name = name
        self.w = None
        self.r = []
        self.excl = excl


class Sched:
    ENGS = ("pe", "act", "dve", "pool", "sp")

    def __init__(self, nc, ctx):
        self.nc = nc
        self.ctx = ctx
        self.ops = {e: [] for e in self.ENGS}
        self.sems = {}
        self.cnt = {}
        self.known = {e: {} for e in self.ENGS}
        for e in self.ENGS:
            self.newsem("E_" + e)

    def newsem(self, key):
        self.sems[key] = self.ctx.enter_context(self.nc.semaphore(key))
        self.cnt[key] = 0
        return key

    def _waits(self, eng, reads, writes):
        need = {}

        def add(tok, raw):
            if tok is None:
                return
            k, v = tok
            if k == "E_" + eng and (eng == "pe" or not SAME_ENGINE_SYNC or not raw):
                return
            if need.get(k, 0) < v:
                need[k] = v

        for b in reads:
            add(b.w, True)
        for b in writes:
            add(b.w, b.excl)
            for t in b.r:
                add(t, False)
        out = []
        kn = self.known[eng]
        for k, v in need.items():
            if kn.get(k, 0) < v:
                kn[k] = v
                out.append((k, v))
        return out

    def _mark(self, tok, reads, writes):
        for b in reads:
            b.r.append(tok)
        for b in writes:
            b.w = tok
            b.r = []

    def op(self, eng, fn, reads=(), writes=(), inc=True):
        ex = [b for b in reads if b.excl]
        if ex:
            reads = [b for b in reads if not b.excl]
            writes = list(writes) + ex
        waits = self._waits(eng, reads, writes)
        key = "E_" + eng
        if inc:
            self.cnt[key] += 1
            tok = (key, self.cnt[key])
            self.ops[eng].append((waits, fn, (key, 1)))
        else:
            tok = (key, self.cnt[key] + 1)
            self.ops[eng].append((waits, fn, None))
        self._mark(tok, reads, writes)
        return tok

    def dma(self, eng, fn, semkey, reads=(), writes=()):
        waits = self._waits(eng, reads, writes)
        self.cnt[semkey] += 16
        tok = (semkey, self.cnt[semkey])
        self.ops[eng].append((waits, fn, (semkey, 16)))
        self._mark(tok, reads, writes)
        return tok

    def wait_all(self, eng, toks):
        waits = []
        kn = self.known[eng]
        for k, v in toks:
            if kn.get(k, 0) < v:
                kn[k] = v
                waits.append((k, v))
        self.ops[eng].append((waits, None, None))

    def emit(self):
        sems = self.sems
        with self.nc.Block() as block:
            def run(engname):
                def body(e):
                    for waits, fn, inc in self.ops[engname]:
                        for k, v in waits:
                            e.wait_ge(sems[k], v)
                        if fn is not None:
                            ins = fn(e)
                            if inc is not None:
                                ins.then_inc(sems[inc[0]], inc[1])
                return body
            block.tensor(run("pe"))
            block.scalar(run("act"))
            block.vector(run("dve"))
            block.gpsimd(run("pool"))
            block.sync(run("sp"))


def build_program():
    nc = bass.Bass("TRN2", target_bir_lowering=False)
    din = lambda n, s: nc.dram_tensor(n, s, F32, kind="ExternalInput")
    dout = lambda n, s: nc.dram_tensor(n, s, F32, kind="ExternalOutput")
    xin = din("xin", [HALO + SEG + NS, D])
    pin = din("pin", [SEG + NS, 256])
    cak = din("cak", [4, 512, 512])
    cav = din("cav", [4, 512, 512])
    cbkd = din("cbkd", [4, 128, 256])
    cbk = din("cbk", [4, 128, 128])
    cbv = din("cbv", [4, 128, 128])
    w32 = din("w32", [128, FTOT])
    wr_d = din("wr", [128, 8 * 36])
    rb_d = din("rb", [1, 36])
    lnp_d = din("lnp", [1, 4 * D])
    sinks_d = din("sinks", [1, 8])
    tabA_d = din("tabA", [128, 3 * 8])
    ohA_d = din("ohA", [128, 3 * 768])
    tabB_d = din("tabB", [32, 8])
    ohB_d = din("ohB", [32, 384])
    mA_d = din("maskA", [128, 640])
    mAs_d = din("maskAs", [128, 128])
    mB_d = din("maskB", [128, 256])
    mBs_d = din("maskBs", [128, 128])
    hm_d = din("hm", [1, 128])

    y_p = dout("y_p", [SEG, D])
    y_s = dout("y_s", [NS, D])
    ak_p = dout("ak_p", [512, 512])
    av_p = dout("av_p", [512, 512])
    bk_p = dout("bk_p", [128, 128])
    bv_p = dout("bv_p", [128, 128])
    ak_s = dout("ak_s", [4, 512, 512])
    av_s = dout("av_s", [4, 512, 512])
    bk_s = dout("bk_s", [4, 128, 128])
    bv_s = dout("bv_s", [4, 128, 128])

    wbf = nc.dram_tensor("wbf", [128, FTOT], BF16, kind="Internal")
    GA_d = nc.dram_tensor("GA", [8, 768], F32, kind="Internal")
    GB_d = nc.dram_tensor("GB", [8, 384], F32, kind="Internal")

    with ExitStack() as ctx:
        S = Sched(nc, ctx)

        def sb(name, shape, dt):
            return ctx.enter_context(nc.sbuf_tensor(name, shape, dt))

        slots = [sb("slot%d" % i, [128, SLOT], BF16) for i in range(NSLOT)]
        slotB = [Buf("slot%d" % i) for i in range(NSLOT)]
        xf = [sb("xf%d" % i, [128, D], F32) for i in range(2)]
        xfB = [Buf("xf0"), Buf("xf1")]
        xb = sb("xb", [128, D], BF16)
        xbB = Buf("xb")
        xT = sb("xT", [128, 8, 512], BF16)
        xTB = [Buf("xT%d" % i) for i in range(4)]
        qAT = sb("qAT", [128, 4, 512], BF16)
        qATB = [Buf("qAT%d" % i) for i in range(4)]
        qBT = sb("qBT", [128, 4, 512], BF16)
        qBTB = [Buf("qBT%d" % i) for i in range(4)]
        kAT = sb("kAT", [128, 4, 1024], BF16)
        kATB = [Buf("kAT%d" % i) for i in range(8)]
        kBT = sb("kBT", [128, 2, 1024], BF16)
        kBTB = [Buf("kBT%d" % i) for i in range(8)]
        VA = sb("VA", [128, 8, 8, 65], BF16)
        VAB = [Buf("VA%d" % i) for i in range(8)]
        VB = sb("VB", [128, 8, 2, 65], BF16)
        VBB = [Buf("VB%d" % i) for i in range(8)]
        PTA = sb("PTA", [128, 2, 5, 512], BF16)
        PTAB = [[Buf("PTA%d%d" % (a, b)) for b in range(5)] for a in range(2)]
        PTB = sb("PTB", [128, 2, 2, 512], BF16)
        PTBB = [[Buf("PTB%d%d" % (a, b)) for b in range(2)] for a in range(2)]
        BTA = sb("BTA", [128, 8, 640], BF16)
        BTAs = sb("BTAs", [128, 8, 128], BF16)
        BTB = sb("BTB", [128, 8, 256], BF16)
        BTBs = sb("BTBs", [128, 8, 128], BF16)
        constB = Buf("const")
        yab = sb("yab", [128, 2, 2, 512], BF16)
        yabB = [[Buf("ya0"), Buf("yb0")], [Buf("ya1"), Buf("yb1")]]
        ypar = [0]
        rsA = sb("rsA", [128, 4], F32)
        rsAB = Buf("rsA")
        yaT = sb("yaT", [128, 4, 512], BF16)
        yaTB = [Buf("yaT%d" % i) for i in range(4)]
        ybT = sb("ybT", [128, 4, 512], BF16)
        ybTB = [Buf("ybT%d" % i) for i in range(4)]
        sga = sb("sga", [128, 512], F32)
        sgaB = Buf("sga")
        sgb = sb("sgb", [128, 512], F32)
        sgbB = Buf("sgb")
        mgT = sb("mgT", [128, 8, 512], BF16)
        mgTB = [Buf("mgT%d" % i) for i in range(8)]
        ckb = mgT[:, 0:4, :]
        ckbb = mgT[:, 4, 0:256]
        xt1 = sb("xt1", [128, D], F32)
        xt1B = Buf("xt1")
        Yacc = sb("Yacc", [128, 4, D], F32)
        YaccB = [Buf("Yacc%d" % i) for i in range(4)]
        x1T = sb("x1T", [128, 8, 512], BF16)
        x1TB = [Buf("x1T%d" % i) for i in range(4)]

        lnst = sb("lnst", [128, 2, 6], F32)
        lnstB = Buf("lnst")
        lnmv = sb("lnmv", [128, 2], F32)
        lnmvB = Buf("lnmv")
        lnr = sb("lnr", [128, 1], F32)
        lnrB = Buf("lnr")
        rt = sb("rt", [128, 768], F32)
        rtB = Buf("rt")
        combT = sb("combT", [32, 512], BF16)
        combTB = [Buf("combT%d" % i) for i in range(4)]
        mt = sb("mt", [128, 4, 512], F32)
        s1 = [mt[:, 0, :], mt[:, 1, :]]
        s1B = [Buf("s1_0"), Buf("s1_1")]
        tm = [mt[:, 2, :], mt[:, 3, :]]
        tmB = [Buf("tm_0"), Buf("tm_1")]
        stgBL = [s1B[0], s1B[1], tmB[0]]
        x1Tf = mt[:, 2:4, :].rearrange("p a (b c) -> p (a b) c", c=128)
        hdn = sb("hdn", [128, 2, 2, 512], BF16)
        hdnB = [[Buf("hdn%d%d" % (a, b)) for b in range(2)] for a in range(2)]
        pf = sb("pf", [128, 256], F32)
        pfB = Buf("pf")
        pbt = sb("pbt", [128, 256], BF16)
        pbtB = Buf("pbt")
        pT = sb("pT", [128, 2, 512], BF16)
        pTB = [Buf("pT%d" % i) for i in range(4)]
        stg = mt[:, :, :].rearrange("p a b -> p (a b)")[:, 0:1280]
        lnpb = sb("lnpb", [128, 2, D], F32)
        lnpB = Buf("lnp")
        cm = sb("cm", [32, 2, 512], BF16)
        cmB = [Buf("cm0"), Buf("cm1")]
        ones32 = sb("ones32", [32, 128], BF16)
        wr = sb("wr_sb", [128, 8, 36], F32)
        rb = sb("rb_sb", [128, 36], F32)
        es = sb("es_sb", [128, 8], F32)
        identf = sb("identf", [128, 128], F32)
        identb = sb("identb", [128, 128], BF16)
        Jb = sb("Jb", [128, 128], BF16)
        ones_r = sb("ones_r", [1, 128], BF16)
        hm_b = sb("hm_b", [1, 128], BF16)
        hm_f = sb("hm_f", [1, 128], F32)

        banks = [ctx.enter_context(nc.psum_tensor("bank%d" % i, [128, 512], F32)) for i in range(8)]
        bankB = [Buf("bank%d" % i, excl=True) for i in range(8)]
        bank_rr = [0]

        def nbank():
            i = bank_rr[0] % 8
            bank_rr[0] += 1
            return i

        def bbf(i):
            return banks[i][:, :].bitcast(BF16)

        def mm(out, lhsT, rhs, start, stop, reads, writes, inc=None):
            if inc is None:
                inc = stop
            S.op("pe", lambda e: e.matmul(out, lhsT=lhsT, rhs=rhs, start=start, stop=stop),
                 reads=reads, writes=writes, inc=inc)

        def tr(out, in_, ident, reads, writes, inc=True):
            S.op("pe", lambda e: e.transpose(out=out, in_=in_, identity=ident), reads=reads, writes=writes, inc=inc)

        def act(out, in_, func, reads, writes, scale=1.0, bias=None):
            if bias is None:
                S.op("act", lambda e: e.activation(out=out, in_=in_, func=func, scale=scale), reads=reads, writes=writes)
            else:
                S.op("act", lambda e: e.activation(out=out, in_=in_, func=func, scale=scale, bias=bias), reads=reads, writes=writes)

        def tt(eng, out, in0, in1, op, reads, writes):
            S.op(eng, lambda e: e.tensor_tensor(out=out, in0=in0, in1=in1, op=op), reads=reads, writes=writes)

        def tcopy(eng, out, in_, reads, writes):
            S.op(eng, lambda e: e.tensor_copy(out=out, in_=in_), reads=reads, writes=writes)

        def ts(eng, out, in0, s1_, s2_, op0, op1, reads, writes):
            if s2_ is None:
                S.op(eng, lambda e: e.tensor_scalar(out=out, in0=in0, scalar1=s1_, scalar2=None, op0=op0), reads=reads, writes=writes)
            else:
                S.op(eng, lambda e: e.tensor_scalar(out=out, in0=in0, scalar1=s1_, scalar2=s2_, op0=op0, op1=op1), reads=reads, writes=writes)

        def memset(eng, ap, val, writes):
            S.op(eng, lambda e: e.memset(ap, val), writes=writes)

        def dma(eng, out, in_, sem, reads, writes):
            S.dma(eng, lambda e: e.dma_start(out=out, in_=in_), sem, reads=reads, writes=writes)

        ring_i = [0]
        for i in range(NSLOT):
            S.newsem("ring%d" % i)

        def wget(name):
            i = ring_i[0] % NSLOT
            ring_i[0] += 1
            off, size = CH_OFF[name], CH_SIZE[name]
            dma("sp", slots[i][:, 0:size], wbf.ap()[:, off:off + size], "ring%d" % i, [ppB[CH_PIECE[name]]], [slotB[i]])
            return slots[i], slotB[i]

        S.newsem("setup")
        setup_bufs = []

        def sload(out, in_):
            dma("sp", out, in_, "setup", [], [constB])

        sf = [s[:, :].bitcast(F32) for s in slots]
        ohA0 = sf[0][:, 0:1536].rearrange("p (c n) -> p c n", c=3)
        ohA1 = sf[1][:, 0:768].rearrange("p (c n) -> p c n", c=3)
        tabA = sf[1][:, 768:792].rearrange("p (c n) -> p c n", c=3)
        tabB = sf[1][0:32, 800:808]
        ohB = sf[1][0:32, 1024:1408]
        mA = sf[2][:, 0:640]
        mAs = sf[2][:, 640:768]
        mB = sf[2][:, 768:1024]
        mBs = sf[2][:, 1024:1152]
        toe = sf[3][:, 0:640]
        toeb = sf[3][:, 1024:1280]
        gsb = sf[4][0:8, 256:1024]
        gsbB = slotB[4]
        ohA_v = ohA_d.ap().rearrange("p (c n) -> p c n", c=3)
        S.newsem("su0")
        S.newsem("su1")
        S.newsem("su2")
        dma("sp", ohA0, ohA_v[:, :, 0:512], "su0", [], [slotB[0]])
        dma("sp", ohA1, ohA_v[:, :, 512:768], "su1", [], [slotB[1]])
        dma("sp", tabA, tabA_d.ap().rearrange("p (c n) -> p c n", c=3), "su1", [], [slotB[1]])
        dma("sp", tabB, tabB_d.ap(), "su1", [], [slotB[1]])
        dma("sp", ohB, ohB_d.ap(), "su1", [], [slotB[1]])
        dma("sp", mA, mA_d.ap(), "su2", [], [slotB[2]])
        dma("sp", mAs, mAs_d.ap(), "su2", [], [slotB[2]])
        dma("sp", mB, mB_d.ap(), "su2", [], [slotB[2]])
        dma("sp", mBs, mBs_d.ap(), "su2", [], [slotB[2]])
        sload(wr[:, :, :], wr_d.ap().rearrange("p (k n) -> p k n", k=8))
        sload(rb[:, :], rb_d.ap().partition_broadcast(128))
        sload(es[:, :], sinks_d.ap().partition_broadcast(128))
        sload(hm_f[:, :], hm_d.ap())
        constB.w = ("setup", S.cnt["setup"])

        memset("dve", identf[:, :], 0.0, [constB])
        S.op("pool", lambda e: e.affine_select(out=identf[:, :], in_=identf[:, :], pattern=[[-1, 128]],
                                               compare_op=ALU.not_equal, fill=1.0, base=0, channel_multiplier=1),
             reads=[constB], writes=[constB])
        tcopy("dve", identb[:, :], identf[:, :], [constB], [constB])
        jf = sf[4][:, 0:128]
        memset("dve", jf, 0.0, [slotB[4]])
        S.op("pool", lambda e: e.affine_select(out=jf, in_=jf, pattern=[[1, 128]], compare_op=ALU.not_equal,
                                               fill=1.0, base=-127, channel_multiplier=1),
             reads=[slotB[4]], writes=[slotB[4]])
        tcopy("dve", Jb[:, :], jf, [slotB[4]], [constB])

        memset("dve", ones32[:, :], 1.0, [constB])
        memset("dve", ones_r[:, :], 1.0, [constB])
        tcopy("dve", hm_b[:, :], hm_f[:, :], [constB], [constB])
        act(es[:, :], es[:, :], AF.Exp, [constB], [constB])
        memset("dve", VA[:, :, :, :], 1.0, VAB)
        memset("dve", xT[:, :, :], 0.0, xTB)
        memset("dve", qAT[:, :, :], 0.0, qATB)
        memset("dve", qBT[:, :, :], 0.0, qBTB)
        memset("dve", VB[:, :, :, :], 1.0, VBB)

        b0, b1 = nbank(), nbank()
        for c in range(3):
            mm(banks[b0][0:8, 0:512], tabA[:, c, :], ohA0[:, c, :], c == 0, c == 2, [slotB[0], slotB[1]], [bankB[b0]])
        for c in range(3):
            mm(banks[b1][0:8, 0:256], tabA[:, c, :], ohA1[:, c, :], c == 0, c == 2, [slotB[1]], [bankB[b1]])
        act(gsb[:, 0:512], banks[b0][0:8, 0:512], AF.Copy, [bankB[b0]], [gsbB])
        act(gsb[:, 512:768], banks[b1][0:8, 0:256], AF.Copy, [bankB[b1]], [gsbB])
        S.newsem("gA")
        GAB = Buf("GA")
        dma("sp", GA_d.ap(), gsb[:, :], "gA", [gsbB], [GAB])
        b2 = nbank()
        mm(banks[b2][0:8, 0:384], tabB, ohB, True, True, [slotB[1]], [bankB[b2]])
        act(gsb[:, 0:384], banks[b2][0:8, 0:384], AF.Copy, [bankB[b2]], [gsbB])
        S.newsem("gB")
        GBB = Buf("GB")
        dma("sp", GB_d.ap(), gsb[:, 0:384], "gB", [gsbB], [GBB])
        S.newsem("toe")
        S.newsem("toeb")

        def toeplitz_loads():
            dma("pool", BTA[:, :, :], bass.AP(GA_d, 0, [[1, 128], [768, 8], [1, 640]]), "toe", [GAB], [constB])
            dma("pool", BTB[:, :, :], bass.AP(GB_d, 0, [[1, 128], [384, 8], [1, 256]]), "toeb", [GBB], [constB])
            constB.w = ("toe", S.cnt["toe"])
            tt("dve", BTAs[:, :, :], BTA[:, :, 512:640], mAs.unsqueeze(1).broadcast_to([128, 8, 128]), ALU.add,
               [constB, slotB[2]], [constB])
            tt("dve", BTA[:, :, :], BTA[:, :, :], mA.unsqueeze(1).broadcast_to([128, 8, 640]), ALU.add,
               [constB, slotB[2]], [constB])
            constB.w = ("toeb", S.cnt["toeb"])
            tt("dve", BTBs[:, :, :], BTB[:, :, 128:256], mBs.unsqueeze(1).broadcast_to([128, 8, 128]), ALU.add,
               [constB, slotB[2]], [constB])
            tt("dve", BTB[:, :, :], BTB[:, :, :], mB.unsqueeze(1).broadcast_to([128, 8, 256]), ALU.add,
               [constB, slotB[2]], [constB])

        ppB = []
        for i, (a, b) in enumerate(PIECES):
            lo, hi = CH_OFF[a], CH_OFF[b] + CH_SIZE[b]
            S.newsem("pp%d" % i)
            bb = Buf("pp%d" % i)
            ppB.append(bb)
            prev = [ppB[i - 2]] if i >= 2 else []
            c = lo
            while c < hi:
                c2 = min(hi, c + 8192)
                dma("pool", wbf.ap()[:, c:c2], w32.ap()[:, c:c2], "pp%d" % i, prev, [bb])
                c = c2
            if i == 1:
                toeplitz_loads()

        S.newsem("xf0")
        S.newsem("xf1")
        S.newsem("pf")
        S.newsem("stg")
        S.newsem("cc")
        S.newsem("ckb")
        S.newsem("ckbb")
        S.newsem("ckvb")
        S.newsem("lnp")
        S.newsem("ckv")
        for i in range(4):
            S.newsem("yo%d" % i)
        out_toks = []
        xf_rr = [0]

        def load_x(row0):
            i = xf_rr[0] % 2
            xf_rr[0] += 1
            dma("sp", xf[i][:, :], xin.ap()[row0:row0 + 128, :], "xf%d" % i, [], [xfB[i]])
            return i

        def make_xT(row0, NT, tiles=None):
            for t in (range(NT) if tiles is None else tiles):
                i = load_x(row0 + t * 128)
                act(xb[:, :], xf[i][:, :], AF.Copy, [xfB[i]], [xbB])
                bk = nbank()
                for kc in range(8):
                    tr(bbf(bk)[:, kc * 128:(kc + 1) * 128], xb[:, kc * 128:(kc + 1) * 128], identb[:, :],
                       [xbB, constB], [bankB[bk]], inc=(kc == 7))
                act(xT[:, :, t * 128:(t + 1) * 128], bbf(bk).rearrange("p (k n) -> p k n", k=8), AF.Copy,
                    [bankB[bk]], [xTB[t]])

        def proj_fm(wname, ngrp, dst, dstB_of, col0, N, NT):
            sl, slB = wget(wname)
            wv = sl[:, 0:ngrp * 1024].rearrange("p (g k n) -> p g k n", g=ngrp, k=8)
            for g in range(ngrp):
                bk = nbank()
                for kc in range(8):
                    mm(banks[bk][:, 0:N], wv[:, g, kc, :], xT[:, kc, 0:N], kc == 0, kc == 7,
                       [slB] + xTB[:NT], [bankB[bk]])
                act(dst[:, g, col0:col0 + N], banks[bk][:, 0:N], AF.Copy, [bankB[bk]], dstB_of(g))

        def proj_v_make(pos_of, cacheout, after_tile):
            st = {}

            def tile(t):
                if not st:
                    st["a"] = wget("TVA")
                    st["b"] = wget("TVB")
                    if cacheout:
                        st["ka"] = wget("TKA")
                        st["kb"] = wget("TKB")
                slA, slAB = st["a"]
                slB_, slBB = st["b"]
                wa = slA[:, 0:4096].rearrange("p (k n) -> p k n", k=8)
                wb = slB_[:, 0:1024].rearrange("p (k n) -> p k n", k=8)
                pos = pos_of(t)
                ba, bb_ = nbank(), nbank()
                for kc in range(8):
                    mm(banks[ba][:, 0:512], xT[:, kc, t * 128:(t + 1) * 128], wa[:, kc, :], kc == 0, kc == 7,
                       [slAB, xTB[t]], [bankB[ba]])
                for kc in range(8):
                    mm(banks[bb_][:, 0:128], xT[:, kc, t * 128:(t + 1) * 128], wb[:, kc, :], kc == 0, kc == 7,
                       [slBB, xTB[t]], [bankB[bb_]])
                act(VA[:, pos, :, 0:64], banks[ba][:, 0:512].rearrange("p (h d) -> p h d", h=8), AF.Copy,
                    [bankB[ba]], [VAB[pos]])
                act(VB[:, pos, :, 0:64], banks[bb_][:, 0:128].rearrange("p (h d) -> p h d", h=2), AF.Copy,
                    [bankB[bb_]], [VBB[pos]])
                if cacheout:
                    slKA, slKAB = st["ka"]
                    slKB, slKBB = st["kb"]
                    wka = slKA[:, 0:4096].rearrange("p (k n) -> p k n", k=8)
                    wkb = slKB[:, 0:1024].rearrange("p (k n) -> p k n", k=8)
                    tcopy("dve", stg[:, 640:1152], banks[ba][:, 0:512], [bankB[ba]], stgBL)
                    tcopy("dve", stg[:, 1152:1280], banks[bb_][:, 0:128], [bankB[bb_]], stgBL)
                    bka, bkb = nbank(), nbank()
                    for kc in range(8):
                        mm(banks[bka][:, 0:512], xT[:, kc, t * 128:(t + 1) * 128], wka[:, kc, :], kc == 0, kc == 7,
                           [slKAB, xTB[t]], [bankB[bka]])
                    for kc in range(8):
                        mm(banks[bkb][:, 0:128], xT[:, kc, t * 128:(t + 1) * 128], wkb[:, kc, :], kc == 0, kc == 7,
                           [slKBB, xTB[t]], [bankB[bkb]])
                    tcopy("dve", stg[:, 0:512], banks[bka][:, 0:512], [bankB[bka]], stgBL)
                    tcopy("dve", stg[:, 512:640], banks[bkb][:, 0:128], [bankB[bkb]], stgBL)
                    after_tile(t)
            return tile

        def attn_pair(qa, ka, va, ta, qb, kb, vb, tb, haloA, haloB, rdA, rdB):
            def scoresA(hg):
                for kt in range(5):
                    bk = nbank()
                    for hh in range(4):
                        h = 4 * hg + hh
                        o = banks[bk][:, hh * 128:(hh + 1) * 128]
                        mm(o, ka(h, kt), qa(h), True, False, rdA(kt), [bankB[bk]])
                        if kt in haloA:
                            mm(o, hm_b[0:1, :], ones_r[0:1, :], False, False, [constB], [bankB[bk]])
                        mm(o, ta(h, kt), Jb[:, :], False, True, [constB], [bankB[bk]], inc=(hh == 3))
                    act(PTA[:, hg, kt, :], banks[bk][:, :], AF.Exp, [bankB[bk]], [PTAB[hg][kt]], scale=0.125)

            def scoresB():
                for kv in range(2):
                    for kt in range(2):
                        bk = nbank()
                        for half in range(2):
                            o2 = banks[bk][:, half * 256:(half + 1) * 256].rearrange("p (a b) -> p a b", a=2)
                            mm(o2, kb(kv, half, kt), qb(kv, half), True, False, rdB(kt), [bankB[bk]])
                            for j in range(2):
                                blk = half * 2 + j
                                o = banks[bk][:, blk * 128:(blk + 1) * 128]
                                if kt in haloB:
                                    mm(o, hm_b[0:1, :], ones_r[0:1, :], False, False, [constB], [bankB[bk]])
                                mm(o, tb(4 * kv + blk, kt), Jb[:, :], False, j == 1, [constB], [bankB[bk]],
                                   inc=(blk == 3))
                        act(PTB[:, kv, kt, :], banks[bk][:, :], AF.Exp, [bankB[bk]], [PTBB[kv][kt]], scale=0.125)

            def pvA(hg):
                bk = nbank()
                for hh in range(4):
                    h = 4 * hg + hh
                    for kt in range(5):
                        mm(banks[bk][:, hh * 65:(hh + 1) * 65], PTA[:, hg, kt, hh * 128:(hh + 1) * 128], va(kt, h),
                           kt == 0, kt == 4, [PTAB[hg][kt]] + rdA(kt), [bankB[bk]], inc=(kt == 4 and hh == 3))
                ov = banks[bk][:, 0:260].rearrange("p (h d) -> p h d", d=65)
                S.op("dve", lambda e, ov=ov: e.reciprocal(out=rsA[:, :], in_=ov[:, :, 64]), reads=[bankB[bk]], writes=[rsAB])
                tt("dve", ya[:, hg * 256:(hg + 1) * 256].rearrange("p (h d) -> p h d", h=4), ov[:, :, 0:64],
                   rsA[:, :].unsqueeze(2).broadcast_to([128, 4, 64]), ALU.mult, [bankB[bk], rsAB], [yaB])

            def pvB():
                for kv in range(2):
                    bk = nbank()
                    for blk in range(4):
                        for kt in range(2):
                            mm(banks[bk][:, blk * 65:(blk + 1) * 65], PTB[:, kv, kt, blk * 128:(blk + 1) * 128], vb(kt, kv),
                               kt == 0, kt == 1, [PTBB[kv][kt]] + rdB(kt), [bankB[bk]], inc=(kt == 1 and blk == 3))
                    ov = banks[bk][:, 0:260].rearrange("p (h d) -> p h d", d=65)
                    tt("dve", rsA[:, :], ov[:, :, 64], es[:, kv * 4:(kv + 1) * 4], ALU.add, [bankB[bk], constB], [rsAB])
                    S.op("dve", lambda e: e.reciprocal(out=rsA[:, :], in_=rsA[:, :]), reads=[rsAB], writes=[rsAB])
                    tt("dve", yb[:, kv * 256:(kv + 1) * 256].rearrange("p (h d) -> p h d", h=4), ov[:, :, 0:64],
                       rsA[:, :].unsqueeze(2).broadcast_to([128, 4, 64]), ALU.mult, [bankB[bk], rsAB], [ybB])

            par = ypar[0] % 2
            ypar[0] += 1
            ya, yb = yab[:, par, 0, :], yab[:, par, 1, :]
            yaB, ybB = yabB[par]
            scoresA(0)
            scoresA(1)
            scoresB()
            pvA(0)
            pvA(1)
            pvB()
            return ya, yaB, yb, ybB

        def y_transposes(yy, dstA, dstB_, dstAB, dstBB, ncols):
            ya, yaB, yb, ybB = yy
            for (src, srcB, dst, dB) in ((ya, yaB, dstA, dstAB), (yb, ybB, dstB_, dstBB)):
                bk = nbank()
                for c in range(4):
                    tr(bbf(bk)[:, c * 128:(c + 1) * 128], src[:, c * 128:(c + 1) * 128], identb[:, :],
                       [srcB, constB], [bankB[bk]], inc=(c == 3))
                act(dst, bbf(bk)[:, 0:512].rearrange("p (c t) -> p c t", c=4)[:, :, 0:ncols], AF.Copy,
                    [bankB[bk]], dB)

        def load_ln(which):
            dma("pool", lnpb[:, :, :],
                lnp_d.ap()[:, which * 2048:(which + 1) * 2048].partition_broadcast(128).rearrange("p o (a n) -> p (o a) n", a=2),
                "lnp", [], [lnpB])

        def layer_norm(xap, xB, gi, out_ap, outB):
            for h in range(2):
                S.op("dve", lambda e, h=h: e.bn_stats(out=lnst[:, h, :], in_=xap[:, h * 512:(h + 1) * 512]),
                     reads=[xB], writes=[lnstB])
            S.op("dve", lambda e: e.bn_aggr(out=lnmv[:, :], in_=lnst[:, :, :]), reads=[lnstB], writes=[lnmvB])
            ts("dve", lnr[:, :], lnmv[:, 1:2], 1e-5, None, ALU.add, None, [lnmvB], [lnrB])
            act(lnr[:, :], lnr[:, :], AF.Ln, [lnrB], [lnrB])
            act(lnr[:, :], lnr[:, :], AF.Exp, [lnrB], [lnrB], scale=-0.5)
            ts("dve", xap, xap, lnmv[:, 0:1], lnr[:, 0:1], ALU.subtract, ALU.mult, [xB, lnmvB, lnrB], [xB])
            tt("dve", xap, xap, lnpb[:, 0, :], ALU.mult, [xB, lnpB], [xB])
            tt("dve", out_ap, xap, lnpb[:, 1, :], ALU.add, [xB, lnpB], [outB] if outB is not xB else [xB])

        def blk(kind, bi):
            sample = kind == "sample"
            NT = 1 if sample else 4
            if kind == "halo":
                row0 = 0
            elif kind == "main":
                row0 = HALO + bi * 512
            else:
                row0 = HALO + SEG
            gbase = 0 if kind == "halo" else (4 + 4 * bi if kind == "main" else 7)
            pos_of = lambda t: (gbase + t) % 8
            kcol0 = 896 if sample else (pos_of(0) * 128)
            cacheout = sample or (kind == "main" and bi == NBLK - 1)
            return sample, NT, NT * 128, row0, gbase, pos_of, kcol0, cacheout

        def stageB_items(kind, bi):
            sample, NT, N, row0, gbase, pos_of, kcol0, cacheout = blk(kind, bi)
            items = []
            for t in range(NT):
                items.append(lambda t=t: make_xT(row0, NT, [t]))
            items.append(lambda: proj_fm("K1", 4, kAT, lambda g: [kATB[pos_of(t)] for t in range(NT)], kcol0, N, NT))
            items.append(lambda: proj_fm("K2", 2, kBT, lambda g: [kBTB[pos_of(t)] for t in range(NT)], kcol0, N, NT))
            if kind != "halo":
                items.append(lambda: proj_fm("Q1", 4, qAT, lambda g: [qATB[g]], 0, N, NT))
                items.append(lambda: proj_fm("Q2", 4, qBT, lambda g: [qBTB[g]], 0, N, NT))

            def after_tile(t):
                if kind == "main":
                    r = t * 128
                    dma("pool", ak_p.ap()[r:r + 128, :], stg[:, 0:512], "stg", stgBL, [])
                    dma("pool", av_p.ap()[r:r + 128, :], stg[:, 640:1152], "stg", stgBL, [])
                    if t == 3:
                        dma("pool", bk_p.ap(), stg[:, 512:640], "stg", stgBL, [])
                        dma("pool", bv_p.ap(), stg[:, 1152:1280], "stg", stgBL, [])
                else:
                    for s_ in range(4):
                        ps_ = slice(32 * s_, 32 * s_ + 32)
                        dma("pool", ak_s.ap()[s_, 480:512, :], stg[ps_, 0:512], "stg", stgBL, [])
                        dma("pool", av_s.ap()[s_, 480:512, :], stg[ps_, 640:1152], "stg", stgBL, [])
                        dma("pool", bk_s.ap()[s_, 96:128, :], stg[ps_, 512:640], "stg", stgBL, [])
                        dma("pool", bv_s.ap()[s_, 96:128, :], stg[ps_, 1152:1280], "stg", stgBL, [])

            pv = proj_v_make(pos_of, cacheout, after_tile)
            for t in range(NT):
                items.append(lambda t=t: pv(t))
            return items

        def do_rest(kind, bi, next_items):
            sample, NT, N, row0, gbase, pos_of, kcol0, cacheout = blk(kind, bi)
            load_ln(0)

            if not sample:
                for pi in range(4):
                    g = gbase + pi
                    qc = slice(pi * 128, (pi + 1) * 128)

                    def kcolsA(kt, g=g):
                        p = (g - 4 + kt) % 8
                        return slice(p * 128, (p + 1) * 128), p

                    def kcolsB(kt, g=g):
                        p = (g - 1 + kt) % 8
                        return slice(p * 128, (p + 1) * 128), p

                    haloA = set(kt for kt in range(5) if bi == 0 and pi + kt < 4)
                    haloB = set(kt for kt in range(2) if bi == 0 and 3 + pi + kt < 4)
                    yy = attn_pair(
                        qa=lambda h, qc=qc: qAT[(h % 2) * 64:(h % 2) * 64 + 64, h // 2, qc],
                        ka=lambda h, kt, f=kcolsA: kAT[(h % 2) * 64:(h % 2) * 64 + 64, h // 2, f(kt)[0]],
                        va=lambda kt, h, f=kcolsA: VA[:, f(kt)[1], h, :],
                        ta=lambda h, kt: BTA[:, h, kt * 128:(kt + 1) * 128],
                        qb=lambda kv, half, qc=qc: qBT[half * 64:half * 64 + 64, 2 * kv:2 * kv + 2, qc],
                        kb=lambda kv, half, kt, f=kcolsB: kBT[half * 64:half * 64 + 64, kv, f(kt)[0]],
                        vb=lambda kt, kv, f=kcolsB: VB[:, f(kt)[1], kv, :],
                        tb=lambda h, kt: BTB[:, h, kt * 128:(kt + 1) * 128],
                        haloA=haloA, haloB=haloB,
                        rdA=lambda kt, f=kcolsA: [kATB[f(kt)[1]], VAB[f(kt)[1]]] + qATB,
                        rdB=lambda kt, f=kcolsB: [kBTB[f(kt)[1]], VBB[f(kt)[1]]] + qBTB,
                    )
                    y_transposes(yy, yaT[:, :, qc], ybT[:, :, qc], [yaTB[pi]], [ybTB[pi]], 128)
            else:
                slA, slAB = wget("TVA")
                slB_, slBB = wget("TVB")
                wa = slA[:, 0:4096].rearrange("p (k n) -> p k n", k=8)
                wb = slB_[:, 0:1024].rearrange("p (k n) -> p k n", k=8)
                memset("dve", kAT[:, :, 544:640], 0.0, [kATB[4]])
                memset("dve", kBT[:, :, 160:256], 0.0, [kBTB[1]])
                for s in range(4):
                    dma("pool", ckb[:, :, :], cak.ap()[s].rearrange("(t p) f -> p t f", p=128), "ckb", [], mgTB[0:4])
                    dma("pool", ckbb[:, :], cbkd.ap()[s], "ckbb", [], [mgTB[4]])
                    for c2 in range(2):
                        bk = nbank()
                        for cc in range(2):
                            c = 2 * c2 + cc
                            for t in range(4):
                                tr(bbf(bk)[:, (cc * 4 + t) * 128:(cc * 4 + t + 1) * 128], ckb[:, t, c * 128:(c + 1) * 128],
                                   identb[:, :], mgTB[0:4] + [constB], [bankB[bk]], inc=(cc == 1 and t == 3))
                        act(kAT[:, 2 * c2:2 * c2 + 2, 0:512], bbf(bk).rearrange("p (c n) -> p c n", c=2), AF.Copy,
                            [bankB[bk]], kATB[0:4])
                    bk = nbank()
                    for kv in range(2):
                        tr(bbf(bk)[:, kv * 128:(kv + 1) * 128], ckbb[:, kv * 128:(kv + 1) * 128], identb[:, :],
                           [mgTB[4], constB], [bankB[bk]], inc=(kv == 1))
                    act(kBT[:, :, 0:128], bbf(bk)[:, 0:256].rearrange("p (c n) -> p c n", c=2), AF.Copy,
                        [bankB[bk]], [kBTB[0]])
                    act(kAT[:, :, 512:544], kAT[:, :, 896 + 32 * s:928 + 32 * s], AF.Copy, [kATB[7]], [kATB[4]])
                    act(kBT[:, :, 128:160], kBT[:, :, 896 + 32 * s:928 + 32 * s], AF.Copy, [kBTB[7]], [kBTB[1]])
                    for t4 in range(4):
                        dma("pool", VA[:, t4, :, 0:64],
                            cav.ap()[s, t4 * 128:(t4 + 1) * 128, :].rearrange("p (h d) -> p h d", h=8),
                            "ckv", [], VAB[0:4])
                    dma("pool", VB[:, 0, :, 0:64], cbv.ap()[s].rearrange("p (h d) -> p h d", h=2), "ckvb", [], [VBB[0]])
                    ba, bb_ = nbank(), nbank()
                    for kc in range(8):
                        mm(banks[ba][:, 0:512], xT[:, kc, 32 * s:32 * s + 128], wa[:, kc, :], kc == 0, kc == 7,
                           [slAB] + xTB[0:2], [bankB[ba]])
                    for kc in range(8):
                        mm(banks[bb_][:, 0:128], xT[:, kc, 32 * s:32 * s + 128], wb[:, kc, :], kc == 0, kc == 7,
                           [slBB] + xTB[0:2], [bankB[bb_]])
                    act(VA[:, 4, :, 0:64], banks[ba][:, 0:512].rearrange("p (h d) -> p h d", h=8), AF.Copy,
                        [bankB[ba]], [VAB[4]])
                    act(VB[:, 1, :, 0:64], banks[bb_][:, 0:128].rearrange("p (h d) -> p h d", h=2), AF.Copy,
                        [bankB[bb_]], [VBB[1]])
                    qc = slice(32 * s, 32 * s + 128)
                    yy = attn_pair(
                        qa=lambda h, qc=qc: qAT[(h % 2) * 64:(h % 2) * 64 + 64, h // 2, qc],
                        ka=lambda h, kt: kAT[(h % 2) * 64:(h % 2) * 64 + 64, h // 2, kt * 128:(kt + 1) * 128],
                        va=lambda kt, h: VA[:, kt, h, :],
                        ta=lambda h, kt: (BTA[:, h, kt * 128:(kt + 1) * 128] if kt < 4 else BTAs[:, h, :]),
                        qb=lambda kv, half, qc=qc: qBT[half * 64:half * 64 + 64, 2 * kv:2 * kv + 2, qc],
                        kb=lambda kv, half, kt: kBT[half * 64:half * 64 + 64, kv, kt * 128:(kt + 1) * 128],
                        vb=lambda kt, kv: VB[:, kt, kv, :],
                        tb=lambda h, kt: (BTB[:, h, 0:128] if kt == 0 else BTBs[:, h, :]),
                        haloA=set(), haloB=set(),
                        rdA=lambda kt: [kATB[kt], VAB[kt]] + qATB,
                        rdB=lambda kt: [kBTB[kt], VBB[kt]] + qBTB,
                    )
                    y_transposes(yy, yaT[:, :, 32 * s:32 * s + 32], ybT[:, :, 32 * s:32 * s + 32], [yaTB[0]], [ybTB[0]], 32)

            for j in range(8):
                sl, slB = wget("D%d" % j)
                gaw = sl[:, 0:1024].rearrange("p (k n) -> p k n", k=8)
                gbw = sl[:, 1024:2048].rearrange("p (k n) -> p k n", k=8)
                paw = sl[:, 2048:2560].rearrange("p (k n) -> p k n", k=4)
                pbw = sl[:, 2560:3072].rearrange("p (k n) -> p k n", k=4)
                bga, bgb, bpa, bpb = nbank(), nbank(), nbank(), nbank()
                for kc in range(8):
                    mm(banks[bga][:, 0:N], gaw[:, kc, :], xT[:, kc, 0:N], kc == 0, kc == 7, [slB] + xTB[:NT], [bankB[bga]])
                for kc in range(8):
                    mm(banks[bgb][:, 0:N], gbw[:, kc, :], xT[:, kc, 0:N], kc == 0, kc == 7, [slB] + xTB[:NT], [bankB[bgb]])
                for kc in range(4):
                    mm(banks[bpa][:, 0:N], paw[:, kc, :], yaT[:, kc, 0:N], kc == 0, kc == 3, [slB] + yaTB[:NT], [bankB[bpa]])
                for kc in range(4):
                    mm(banks[bpb][:, 0:N], pbw[:, kc, :], ybT[:, kc, 0:N], kc == 0, kc == 3, [slB] + ybTB[:NT], [bankB[bpb]])
                act(sga[:, 0:N], banks[bga][:, 0:N], AF.Sigmoid, [bankB[bga]], [sgaB])
                act(sgb[:, 0:N], banks[bgb][:, 0:N], AF.Sigmoid, [bankB[bgb]], [sgbB])
                tt("dve", sga[:, 0:N], sga[:, 0:N], banks[bpa][:, 0:N], ALU.mult, [sgaB, bankB[bpa]], [sgaB])
                tt("dve", sgb[:, 0:N], sgb[:, 0:N], banks[bpb][:, 0:N], ALU.mult, [sgbB, bankB[bpb]], [sgbB])
                tt("dve", mgT[:, j, 0:N], sga[:, 0:N], sgb[:, 0:N], ALU.add, [sgaB, sgbB], [mgTB[j]])

            wo0, wo0B = wget("WO0")
            wo1, wo1B = wget("WO1")
            wov = [wo0[:, :].rearrange("p (k n) -> p k n", k=4), wo1[:, :].rearrange("p (k n) -> p k n", k=4)]
            AXX = mybir.AxisListType.X
            lgall = rt[:, 0:NT * 36].rearrange("p (t n) -> p t n", t=NT)
            wo_banks = {}

            def wo_mm(t):
                tok = slice(t * 128, (t + 1) * 128)
                bo = [nbank(), nbank()]
                wo_banks[t] = bo
                for nh in range(2):
                    for kc in range(8):
                        mm(banks[bo[nh]][:, :], mgT[:, kc, tok], wov[kc // 4][:, kc % 4, nh * 512:(nh + 1) * 512],
                           kc == 0, kc == 7, [wo0B, wo1B] + mgTB, [bankB[bo[nh]]])

            def ln_part(t):
                bo = wo_banks[t]
                xi = load_x(row0 + t * 128)
                for nh in range(2):
                    S.op("dve", lambda e, nh=nh, xi=xi, bo=bo: e.scalar_tensor_tensor(
                        out=xt1[:, nh * 512:(nh + 1) * 512], in0=xf[xi][:, nh * 512:(nh + 1) * 512], scalar=ALPHA,
                        in1=banks[bo[nh]][:, :], op0=ALU.mult, op1=ALU.add),
                        reads=[xfB[xi], bankB[bo[nh]]], writes=[xt1B])
                layer_norm(xt1[:, :], xt1B, 0, xt1[:, :], xt1B)
                act(Yacc[:, t, :], xt1[:, :], AF.Copy, [xt1B], [YaccB[t]], scale=ALPHA)

            def part2(t):
                tok = slice(t * 128, (t + 1) * 128)
                bt = [nbank(), nbank()]
                for kc in range(8):
                    tr(banks[bt[kc // 4]][:, (kc % 4) * 128:(kc % 4 + 1) * 128], xt1[:, kc * 128:(kc + 1) * 128],
                       identf[:, :], [xt1B, constB], [bankB[bt[kc // 4]]], inc=(kc % 4 == 3))
                for hb in range(2):
                    bv = banks[bt[hb]][:, :].rearrange("p (k n) -> p k n", k=4)
                    act(x1T[:, hb * 4:(hb + 1) * 4, tok], bv, AF.Copy, [bankB[bt[hb]]], [x1TB[t]])
                    tcopy("dve", x1Tf[:, hb * 4:(hb + 1) * 4, :], bv, [bankB[bt[hb]]], [tmB[0], tmB[1]])
                bl = nbank()
                for kc in range(8):
                    mm(banks[bl][:, 0:36], x1Tf[:, kc, :], wr[:, kc, :], kc == 0, kc == 7, [tmB[0], tmB[1], constB], [bankB[bl]])
                tt("dve", lgall[:, t, :], banks[bl][:, 0:36], rb[:, :], ALU.add, [bankB[bl], constB], [rtB])

            wo_mm(0)
            for t in range(NT):
                ln_part(t)
                if t + 1 < NT:
                    wo_mm(t + 1)
                part2(t)

            o = NT * 36

            def rsl(n):
                nonlocal o
                v = rt[:, o:o + n]
                o += n
                return v

            G = lgall[:, :, 0:4]
            E = lgall[:, :, 4:36]
            gmax = rsl(NT)
            goh = rsl(NT * 4).rearrange("p (t n) -> p t n", t=NT)
            gex = rsl(NT * 4).rearrange("p (t n) -> p t n", t=NT)
            gsum = rsl(NT)
            gw = rsl(NT)
            gpen = rsl(NT * 4)
            em = rsl(NT * 32)
            m8 = rsl(NT * 8).rearrange("p (t n) -> p t n", t=NT)
            oh1 = rsl(NT * 32).rearrange("p (t n) -> p t n", t=NT)
            oh2 = rsl(NT * 32).rearrange("p (t n) -> p t n", t=NT)
            dlt = rsl(NT)
            w1_ = rsl(NT)
            w2_ = rsl(NT)
            R = [rtB]
            bc4 = lambda v: v.unsqueeze(2).broadcast_to([128, NT, 4])
            bc32 = lambda v: v.unsqueeze(2).broadcast_to([128, NT, 32])
            S.op("dve", lambda e: e.tensor_reduce(out=gmax, in_=G, axis=AXX, op=ALU.max), reads=R, writes=R)
            tt("dve", goh, G, bc4(gmax), ALU.is_equal, R, R)
            tt("dve", gex, G, bc4(gmax), ALU.subtract, R, R)
            act(gex, gex, AF.Exp, R, R)
            S.op("dve", lambda e: e.tensor_reduce(out=gsum, in_=gex, axis=AXX, op=ALU.add), reads=R, writes=R)
            S.op("dve", lambda e: e.reciprocal(out=gw, in_=gsum), reads=R, writes=R)
            ts("dve", gpen, goh.rearrange("p t n -> p (t n)"), -1.0, 1e30, ALU.add, ALU.mult, R, R)
            for t in range(NT):
                tt("dve", em[:, t * 32:(t + 1) * 32].rearrange("p (g e) -> p g e", g=4),
                   lgall[:, t, 4:36].rearrange("p (g e) -> p g e", g=4),
                   gpen[:, t * 4:(t + 1) * 4].unsqueeze(2).broadcast_to([128, 4, 8]), ALU.add, R, R)
            for t in range(NT):
                S.op("dve", lambda e, t=t: e.max(out=m8[:, t, :], in_=em[:, t * 32:(t + 1) * 32]), reads=R, writes=R)
            emv = em.rearrange("p (t n) -> p t n", t=NT)
            tt("dve", oh1, emv, bc32(m8[:, :, 0]), ALU.is_equal, R, R)
            tt("dve", oh2, emv, bc32(m8[:, :, 1]), ALU.is_equal, R, R)
            tt("dve", dlt, m8[:, :, 1], m8[:, :, 0], ALU.subtract, R, R)
            act(dlt, dlt, AF.Exp, R, R)
            ts("dve", dlt, dlt, 1.0, None, ALU.add, None, R, R)
            S.op("dve", lambda e: e.reciprocal(out=w1_, in_=dlt), reads=R, writes=R)
            tt("dve", w1_, w1_, gw, ALU.mult, R, R)
            tt("dve", w2_, gw, w1_, ALU.subtract, R, R)
            tt("dve", oh1, oh1, bc32(w1_), ALU.mult, R, R)
            tt("dve", oh2, oh2, bc32(w2_), ALU.mult, R, R)
            tt("dve", oh1, oh1, oh2, ALU.add, R, R)
            for t in range(NT):
                bc_ = nbank()
                tr(banks[bc_][0:32, 0:128], oh1[:, t, :], identf[:, :], [rtB, constB], [bankB[bc_]])
                act(combT[:, t * 128:(t + 1) * 128], banks[bc_][0:32, 0:128], AF.Copy, [bankB[bc_]], [combTB[t]])

            load_ln(1)
            prow0 = (bi * 512) if kind == "main" else SEG
            for t in range(NT):
                dma("sp", pf[:, :], pin.ap()[prow0 + t * 128:prow0 + (t + 1) * 128, :], "pf", [], [pfB])
                act(pbt[:, :], pf[:, :], AF.Copy, [pfB], [pbtB])
                bk = nbank()
                for c in range(2):
                    tr(bbf(bk)[:, c * 128:(c + 1) * 128], pbt[:, c * 128:(c + 1) * 128], identb[:, :], [pbtB, constB],
                       [bankB[bk]], inc=(c == 1))
                act(pT[:, :, t * 128:(t + 1) * 128], bbf(bk)[:, 0:256].rearrange("p (c n) -> p c n", c=2), AF.Copy,
                    [bankB[bk]], [pTB[t]])
            for eg in range(16):
                wslots = []
                for ee in range(2):
                    e_ = 2 * eg + ee
                    ea, eaB = wget("EA%d" % e_)
                    eb, ebB = wget("EB%d" % e_)
                    wslots.append((eb, ebB))
                    w1v = ea[:, 0:2048].rearrange("p (k n) -> p k n", k=8)
                    w3v = ea[:, 2048:4096].rearrange("p (k n) -> p k n", k=8)
                    ci = e_ % 2
                    hb_ = []
                    for hc in range(2):
                        bh1, bh3 = nbank(), nbank()
                        hb_.append((bh1, bh3))
                        for kc in range(8):
                            mm(banks[bh1][:, 0:N], w1v[:, kc, hc * 128:(hc + 1) * 128], x1T[:, kc, 0:N], kc == 0, kc == 7,
                               [eaB] + x1TB[:NT], [bankB[bh1]])
                        for kc in range(8):
                            mm(banks[bh3][:, 0:N], w3v[:, kc, hc * 128:(hc + 1) * 128], x1T[:, kc, 0:N], kc == 0, kc == 7,
                               [eaB] + x1TB[:NT], [bankB[bh3]])
                    bcb = nbank()
                    act(cm[:, ci, 0:N], combT[:, 0:N], AF.Copy, combTB[:NT] + [constB], [cmB[ci]], scale=identf[0:32, e_:e_ + 1])
                    mm(banks[bcb][:, 0:N], ones32[:, :], cm[:, ci, 0:N], True, True, [cmB[ci], constB], [bankB[bcb]])
                    for hc in range(2):
                        bh1, bh3 = hb_[hc]
                        act(s1[hc][:, 0:N], banks[bh1][:, 0:N], AF.Silu, [bankB[bh1]], [s1B[hc]])
                        tt("dve", tm[hc][:, 0:N], s1[hc][:, 0:N], banks[bh3][:, 0:N], ALU.mult, [s1B[hc], bankB[bh3]], [tmB[hc]])
                        tt("dve", hdn[:, ee, hc, 0:N], tm[hc][:, 0:N], banks[bcb][:, 0:N], ALU.mult,
                           [tmB[hc], bankB[bcb]], [hdnB[ee][hc]])
                for t in range(NT):
                    tok = slice(t * 128, (t + 1) * 128)
                    for nh in range(2):
                        by = nbank()
                        i = 0
                        for ee in range(2):
                            w2v = wslots[ee][0][:, 0:2048].rearrange("p (k n) -> p k n", k=2)
                            for hc in range(2):
                                mm(banks[by][:, :], hdn[:, ee, hc, tok], w2v[:, hc, nh * 512:(nh + 1) * 512], i == 0, i == 3,
                                   [hdnB[ee][hc], wslots[ee][1]], [bankB[by]])
                                i += 1
                        tt("dve", Yacc[:, t, nh * 512:(nh + 1) * 512], Yacc[:, t, nh * 512:(nh + 1) * 512], banks[by][:, :],
                           ALU.add, [YaccB[t], bankB[by]], [YaccB[t]])

            pg0, pg0B = wget("PG0")
            pg1, pg1B = wget("PG1")
            pe_, peB = wget("PE")
            pgv = [pg0[:, :].rearrange("p (k n) -> p k n", k=4), pg1[:, :].rearrange("p (k n) -> p k n", k=4)]
            pev = pe_[:, 0:2048].rearrange("p (k n) -> p k n", k=2)
            for t in range(NT):
                tok = slice(t * 128, (t + 1) * 128)
                for nh in range(2):
                    bg, bp = nbank(), nbank()
                    for kc in range(8):
                        mm(banks[bg][:, :], x1T[:, kc, tok], pgv[kc // 4][:, kc % 4, nh * 512:(nh + 1) * 512], kc == 0, kc == 7,
                           [pg0B, pg1B, x1TB[t]], [bankB[bg]])
                    for kc in range(2):
                        mm(banks[bp][:, :], pT[:, kc, tok], pev[:, kc, nh * 512:(nh + 1) * 512], kc == 0, kc == 1,
                           [peB, pTB[t]], [bankB[bp]])
                    act(sga[:, :], banks[bg][:, :], AF.Sigmoid, [bankB[bg]], [sgaB])
                    tt("dve", sga[:, :], sga[:, :], banks[bp][:, :], ALU.mult, [sgaB, bankB[bp]], [sgaB])
                    tt("dve", Yacc[:, t, nh * 512:(nh + 1) * 512], Yacc[:, t, nh * 512:(nh + 1) * 512], sga[:, :], ALU.add,
                       [YaccB[t], sgaB], [YaccB[t]])

            def ln2_tile(t):
                layer_norm(Yacc[:, t, :], YaccB[t], 2, Yacc[:, t, :], YaccB[t])
                if kind == "main":
                    dst = y_p.ap()[bi * 512 + t * 128:bi * 512 + (t + 1) * 128, :]
                else:
                    dst = y_s.ap()
                dma("pool", dst, Yacc[:, t, :], "yo%d" % t, [YaccB[t]], [])

            items = list(next_items)
            tl = list(range(NT))
            while items or tl:
                for _ in range(2):
                    if items:
                        items.pop(0)()
                if tl:
                    ln2_tile(tl.pop(0))

        for it in stageB_items("halo", -1):
            it()
        for it in stageB_items("main", 0):
            it()
        for bi in range(NBLK):
            nxt = stageB_items("main", bi + 1) if bi + 1 < NBLK else stageB_items("sample", 0)
            do_rest("main", bi, nxt)
            if bi == 1:
                dma("sp", ak_s.ap()[:, 0:480, :], cak.ap()[:, 32:512, :], "cc", [], [])
                dma("sp", av_s.ap()[:, 0:480, :], cav.ap()[:, 32:512, :], "cc", [], [])
                dma("sp", bk_s.ap()[:, 0:96, :], cbk.ap()[:, 32:128, :], "cc", [], [])
                dma("sp", bv_s.ap()[:, 0:96, :], cbv.ap()[:, 32:128, :], "cc", [], [])
        do_rest("sample", 0, [])
        fin = [(k, S.cnt[k]) for k in ["cc", "stg", "yo0", "yo1", "yo2", "yo3"]]
        S.wait_all("pool", fin)
        S.emit()
    return nc


def _consts():
    ar = np.arange
    m = ar(768)
    idxA = np.clip(639 - m, -128, 128) + 128
    ohA = np.zeros((384, 768), np.float32)
    ohA[idxA[:767], m[:767]] = 8.0
    mb = ar(384)
    idxB = _t5_bucket(255 - mb)
    ohB = np.zeros((32, 384), np.float32)
    ohB[idxB[:383], mb[:383]] = 8.0
    qp = 127 - ar(128)[:, None]
    j = ar(640)[None, :]
    validA = np.where(qp < 64, j < 576, j >= 64)
    maskA = np.where(validA, 0.0, MASKV).astype(np.float32)
    maskAs = np.broadcast_to(np.where(ar(128)[None, :] < 32, 0.0, MASKV), (128, 128)).astype(np.float32)
    jb = ar(256)[None, :]
    validB = np.where(qp < 64, jb < 192, jb >= 64)
    maskB = np.where(validB, 0.0, MASKV).astype(np.float32)
    maskBs = maskAs.copy()
    return dict(ohA=np.ascontiguousarray(ohA.reshape(3, 128, 768).transpose(1, 0, 2).reshape(128, 3 * 768)),
                ohB=ohB, maskA=maskA, maskAs=np.ascontiguousarray(maskAs), maskB=maskB, maskBs=maskBs)


def kernel(x_prompt, x_sample, p_prompt, p_sample, cache_a_k, cache_a_v, cache_b_k, cache_b_v, w_in,
           a_rel_table, b_sinks, w_pa, w_pb, w_o, ln1_g, ln1_b, w_rg, b_rg, w_re, b_re, w1, w3, w2, w_pe,
           w_pg, ln2_g, ln2_b, t5_table):
    f = lambda a: np.ascontiguousarray(np.asarray(a, dtype=np.float32))
    x_prompt, x_sample, p_prompt, p_sample = f(x_prompt), f(x_sample), f(p_prompt), f(p_sample)
    cache_a_k, cache_a_v, cache_b_k, cache_b_v = f(cache_a_k), f(cache_a_v), f(cache_b_k), f(cache_b_v)
    W = _pack_weights(f(w_in)[0], f(w_pa)[0], f(w_pb)[0], f(w_o)[0], f(w1)[0], f(w3)[0], f(w2)[0], f(w_pe)[0], f(w_pg)[0])
    cst = _consts()
    wr = np.concatenate([f(w_rg)[0], f(w_re)[0]], axis=1)
    wr = np.ascontiguousarray(wr.reshape(8, 128, 36).transpose(1, 0, 2).reshape(128, 8 * 36))
    rb = np.concatenate([f(b_rg)[0], f(b_re)[0]])[None, :]
    lnp = np.concatenate([f(ln1_g)[0], f(ln1_b)[0], f(ln2_g)[0], f(ln2_b)[0]])[None, :]
    tabA = np.zeros((384, 8), np.float32)
    tabA[:257] = f(a_rel_table)[0]
    tabA = np.ascontiguousarray(tabA.reshape(3, 128, 8).transpose(1, 0, 2).reshape(128, 24))
    shared = dict(w32=W, wr=wr, rb=np.ascontiguousarray(rb), lnp=np.ascontiguousarray(lnp), sinks=f(b_sinks),
                  tabA=tabA, ohA=cst["ohA"], tabB=f(t5_table), ohB=cst["ohB"], maskA=cst["maskA"],
                  maskAs=cst["maskAs"], maskB=cst["maskB"], maskBs=cst["maskBs"])
    in_maps = []
    for c in range(NCORES):
        b, sgm = c // 4, c % 4
        t0 = sgm * SEG
        xin = np.zeros((HALO + SEG + NS, D), np.float32)
        if sgm > 0:
            xin[0:HALO] = x_prompt[b, t0 - HALO:t0]
        xin[HALO:HALO + SEG] = x_prompt[b, t0:t0 + SEG]
        xin[HALO + SEG:] = x_sample[4 * c:4 * c + 4].reshape(NS, D)
        pin = np.concatenate([p_prompt[0, b, t0:t0 + SEG], p_sample[0, 4 * c:4 * c + 4].reshape(NS, 256)], 0)
        cbk_ = cache_b_k[0, 4 * c:4 * c + 4].reshape(4, 128, 2, 64)
        cbkd = np.concatenate([cbk_[:, :, 0], cbk_[:, :, 0], cbk_[:, :, 1], cbk_[:, :, 1]], axis=-1)
        hm = np.full((1, 128), MASKV if sgm == 0 else 0.0, np.float32)
        m = dict(shared)
        m.update(xin=xin, pin=np.ascontiguousarray(pin),
                 cak=np.ascontiguousarray(cache_a_k[0, 4 * c:4 * c + 4].reshape(4, 512, 512)),
                 cav=np.ascontiguousarray(cache_a_v[0, 4 * c:4 * c + 4].reshape(4, 512, 512)),
                 cbkd=np.ascontiguousarray(cbkd),
                 cbk=np.ascontiguousarray(cache_b_k[0, 4 * c:4 * c + 4].reshape(4, 128, 128)),
                 cbv=np.ascontiguousarray(cache_b_v[0, 4 * c:4 * c + 4].reshape(4, 128, 128)), hm=hm)
        in_maps.append(m)
    nc = build_program()
    res = run_bass_kernel_spmd(nc, in_maps, core_ids=list(range(NCORES)))
    R = res.results
    y_prompt = np.stack([np.concatenate([R[4 * b + s]["y_p"] for s in range(4)], 0) for b in range(2)], 0)
    y_sample = np.concatenate([R[c]["y_s"].reshape(4, 32, D) for c in range(NCORES)], 0)
    pak = np.stack([R[4 * b + 3]["ak_p"].reshape(512, 8, 64) for b in range(2)], 0)[None]
    pav = np.stack([R[4 * b + 3]["av_p"].reshape(512, 8, 64) for b in range(2)], 0)[None]
    pbk = np.stack([R[4 * b + 3]["bk_p"].reshape(128, 2, 64) for b in range(2)], 0)[None]
    pbv = np.stack([R[4 * b + 3]["bv_p"].reshape(128, 2, 64) for b in range(2)], 0)[None]
    sak = np.concatenate([R[c]["ak_s"].reshape(4, 512, 8, 64) for c in range(NCORES)], 0)[None]
    sav = np.concatenate([R[c]["av_s"].reshape(4, 512, 8, 64) for c in range(NCORES)], 0)[None]
    sbk = np.concatenate([R[c]["bk_s"].reshape(4, 128, 2, 64) for c in range(NCORES)], 0)[None]
    sbv = np.concatenate([R[c]["bv_s"].reshape(4, 128, 2, 64) for c in range(NCORES)], 0)[None]
    out = (y_prompt, y_sample, pak, pav, pbk, pbv, sak, sav, sbk, sbv)
    return tuple(np.ascontiguousarray(o, dtype=np.float32) for o in out)
```

```python
import math
from contextlib import ExitStack

import numpy as np

import concourse.bass as bass
import concourse.mybir as mybir
from concourse.bass_utils import run_bass_kernel_spmd

F32 = mybir.dt.float32
BF16 = mybir.dt.bfloat16
AF = mybir.ActivationFunctionType
ALU = mybir.AluOpType

NCORES = 8
D = 1024
SEG = 4096
HALO = 512
NS = 128
NBLK = 8
ALPHA = 2.0 ** 0.25
MASKV = -240000.0
NSLOT = 5
SLOT = 4096
SAME_ENGINE_SYNC = True
SAME_ENGINE_WAR = True

CH_ORDER = (["K1", "K2", "Q1", "Q2", "TVA", "TVB", "TKA", "TKB"] + ["D%d" % j for j in range(8)]
            + ["WO0", "WO1"])
for _e in range(32):
    CH_ORDER += ["EA%d" % _e, "EB%d" % _e]
CH_ORDER += ["PG0", "PG1", "PE"]
CH_SIZE = {"K1": 4096, "K2": 2048, "Q1": 4096, "Q2": 4096, "TVA": 4096, "TVB": 1024, "TKA": 4096,
           "TKB": 1024, "WO0": 4096, "WO1": 4096, "PG0": 4096, "PG1": 4096, "PE": 2048}
for _j in range(8):
    CH_SIZE["D%d" % _j] = 3072
for _e in range(32):
    CH_SIZE["EA%d" % _e] = 4096
    CH_SIZE["EB%d" % _e] = 2048
CH_OFF = {}
_o = 0
for _n in CH_ORDER:
    CH_OFF[_n] = _o
    _o += CH_SIZE[_n]
FTOT = _o
PIECES = ([("K1", "TKB"), ("D0", "D7"), ("WO0", "WO1")]
          + [("EA%d" % (2 * _i), "EB%d" % (2 * _i + 1)) for _i in range(16)] + [("PG0", "PE")])
CH_PIECE = {}
for _pi, (_a, _b) in enumerate(PIECES):
    for _n in CH_ORDER[CH_ORDER.index(_a):CH_ORDER.index(_b) + 1]:
        CH_PIECE[_n] = _pi


def _t5_bucket(rel):
    m = -rel
    base = np.where(m > 0, 16, 0)
    n = np.abs(m)
    nf = np.maximum(n, 1).astype(np.float32)
    v = np.log(nf / np.float32(8)) / np.float32(math.log(16.0)) * np.float32(8)
    large = 8 + v.astype(np.int32)
    large = np.minimum(large, 15)
    return base + np.where(n < 8, n, large)


def _pack_weights(w_in, w_pa, w_pb, w_o, w1, w3, w2, w_pe, w_pg):
    W = np.empty((128, FTOT), np.float32)

    def put(name, arr, off=0):
        a = arr.reshape(128, -1)
        W[:, CH_OFF[name] + off:CH_OFF[name] + off + a.shape[1]] = a

    def fm(cols):
        return w_in[:, cols].reshape(8, 128, len(cols)).transpose(1, 0, 2)

    def kmaj(w, nk):
        return w.reshape(nk, 128, w.shape[1]).transpose(1, 0, 2)

    ar = np.arange
    put("K1", np.stack([fm(512 + g * 128 + ar(128)) for g in range(4)], 1))
    put("K2", np.stack([fm(np.concatenate([2048 + kv * 64 + ar(64)] * 2)) for kv in range(2)], 1))
    put("Q1", np.stack([fm(g * 128 + ar(128)) for g in range(4)], 1))
    qb = []
    for g in range(4):
        kv, a = g // 2, g % 2
        qb.append(fm(np.concatenate([1536 + (4 * kv + a) * 64 + ar(64), 1536 + (4 * kv + a + 2) * 64 + ar(64)])))
    put("Q2", np.stack(qb, 1))
    put("TVA", fm(1024 + ar(512)))
    put("TVB", fm(2176 + ar(128)))
    put("TKA", fm(512 + ar(512)))
    put("TKB", fm(2048 + ar(128)))
    pa = kmaj(w_pa, 4)
    pb = kmaj(w_pb, 4)
    for j in range(8):
        n = "D%d" % j
        put(n, fm(2304 + j * 128 + ar(128)), 0)
        put(n, fm(3328 + j * 128 + ar(128)), 1024)
        put(n, pa[:, :, j * 128:(j + 1) * 128], 2048)
        put(n, pb[:, :, j * 128:(j + 1) * 128], 2560)
    wo = kmaj(w_o, 8)
    put("WO0", wo[:, 0:4])
    put("WO1", wo[:, 4:8])
    for e in range(32):
        put("EA%d" % e, kmaj(w1[e], 8), 0)
        put("EA%d" % e, kmaj(w3[e], 8), 2048)
        put("EB%d" % e, kmaj(w2[e], 2))
    pg = kmaj(w_pg, 8)
    put("PG0", pg[:, 0:4])
    put("PG1", pg[:, 4:8])
    put("PE", kmaj(w_pe, 2))
    return W


class Buf:
    __slots__ = ("name", "w", "r", "excl")

    def __init__(self, name="", excl=False):
        self.name = name
        self.w = None
        self.r = []
        self.excl = excl


class Sched:
    ENGS = ("pe", "act", "dve", "pool", "sp")

    def __init__(self, nc, ctx):
        self.nc = nc
        self.ctx = ctx
        self.ops = {e: [] for e in self.ENGS}
        self.sems = {}
        self.cnt = {}
        self.known = {e: {} for e in self.ENGS}
        for e in self.ENGS:
            self.newsem("E_" + e)

    def newsem(self, key):
        self.sems[key] = self.ctx.enter_context(self.nc.semaphore(key))
        self.cnt[key] = 0
        return key

    def _waits(self, eng, reads, writes):
        need = {}

        def add(tok, raw):
            if tok is None:
                return
            k, v = tok
            if k == "E_" + eng and (eng == "pe" or not SAME_ENGINE_SYNC or not (raw or SAME_ENGINE_WAR)):
                return
            if need.get(k, 0) < v:
                need[k] = v

        for b in reads:
            add(b.w, True)
        for b in writes:
            add(b.w, b.excl)
            for t in b.r:
                add(t, False)
        out = []
        kn = self.known[eng]
        for k, v in need.items():
            if kn.get(k, 0) < v:
                kn[k] = v
                out.append((k, v))
        return out

    def _mark(self, tok, reads, writes):
        for b in reads:
            b.r.append(tok)
        for b in writes:
            b.w = tok
            b.r = []

    def op(self, eng, fn, reads=(), writes=(), inc=True):
        ex = [b for b in reads if b.excl]
        if ex:
            reads = [b for b in reads if not b.excl]
            writes = list(writes) + ex
        waits = self._waits(eng, reads, writes)
        key = "E_" + eng
        if inc:
            self.cnt[key] += 1
            tok = (key, self.cnt[key])
            self.ops[eng].append((waits, fn, (key, 1)))
        else:
            tok = (key, self.cnt[key] + 1)
            self.ops[eng].append((waits, fn, None))
        self._mark(tok, reads, writes)
        return tok

    def dma(self, eng, fn, semkey, reads=(), writes=()):
        waits = self._waits(eng, reads, writes)
        self.cnt[semkey] += 16
        tok = (semkey, self.cnt[semkey])
        self.ops[eng].append((waits, fn, (semkey, 16)))
        self._mark(tok, reads, writes)
        return tok

    def wait_all(self, eng, toks):
        waits = []
        kn = self.known[eng]
        for k, v in toks:
            if kn.get(k, 0) < v:
                kn[k] = v
                waits.append((k, v))
        self.ops[eng].append((waits, None, None))

    def emit(self):
        sems = self.sems
        with self.nc.Block() as block:
            def run(engname):
                def body(e):
                    for waits, fn, inc in self.ops[engname]:
                        for k, v in waits:
                            e.wait_ge(sems[k], v)
                        if fn is not None:
                            ins = fn(e)
                            if inc is not None:
                                ins.then_inc(sems[inc[0]], inc[1])
                return body
            block.tensor(run("pe"))
            block.scalar(run("act"))
            block.vector(run("dve"))
            block.gpsimd(run("pool"))
            block.sync(run("sp"))


def build_program():
    nc = bass.Bass("TRN2", target_bir_lowering=False)
    din = lambda n, s: nc.dram_tensor(n, s, F32, kind="ExternalInput")
    dout = lambda n, s: nc.dram_tensor(n, s, F32, kind="ExternalOutput")
    xin = din("xin", [HALO + SEG + NS, D])
    pin = din("pin", [SEG + NS, 256])
    cak = din("cak", [4, 512, 512])
    cav = din("cav", [4, 512, 512])
    cbkd = din("cbkd", [4, 128, 256])
    cbk = din("cbk", [4, 128, 128])
    cbv = din("cbv", [4, 128, 128])
    w32 = din("w32", [128, FTOT])
    wr_d = din("wr", [128, 8 * 36])
    rb_d = din("rb", [1, 36])
    lnp_d = din("lnp", [1, 4 * D])
    sinks_d = din("sinks", [1, 8])
    tabA_d = din("tabA", [128, 3 * 8])
    ohA_d = din("ohA", [128, 3 * 768])
    tabB_d = din("tabB", [32, 8])
    ohB_d = din("ohB", [32, 384])
    mA_d = din("maskA", [128, 640])
    mAs_d = din("maskAs", [128, 128])
    mB_d = din("maskB", [128, 256])
    mBs_d = din("maskBs", [128, 128])
    hm_d = din("hm", [1, 128])

    y_p = dout("y_p", [SEG, D])
    y_s = dout("y_s", [NS, D])
    ak_p = dout("ak_p", [512, 512])
    av_p = dout("av_p", [512, 512])
    bk_p = dout("bk_p", [128, 128])
    bv_p = dout("bv_p", [128, 128])
    ak_s = dout("ak_s", [4, 512, 512])
    av_s = dout("av_s", [4, 512, 512])
    bk_s = dout("bk_s", [4, 128, 128])
    bv_s = dout("bv_s", [4, 128, 128])

    wbf = nc.dram_tensor("wbf", [128, FTOT], BF16, kind="Internal")
    GA_d = nc.dram_tensor("GA", [8, 768], F32, kind="Internal")
    GB_d = nc.dram_tensor("GB", [8, 384], F32, kind="Internal")

    with ExitStack() as ctx:
        S = Sched(nc, ctx)

        def sb(name, shape, dt):
            return ctx.enter_context(nc.sbuf_tensor(name, shape, dt))

        slots = [sb("slot%d" % i, [128, SLOT], BF16) for i in range(NSLOT)]
        slotB = [Buf("slot%d" % i) for i in range(NSLOT)]
        xf = [sb("xf%d" % i, [128, D], F32) for i in range(2)]
        xfB = [Buf("xf0"), Buf("xf1")]
        xb = sb("xb", [128, D], BF16)
        xbB = Buf("xb")
        xT = sb("xT", [128, 8, 512], BF16)
        xTB = [Buf("xT%d" % i) for i in range(4)]
        qAT = sb("qAT", [128, 4, 512], BF16)
        qATB = [Buf("qAT%d" % i) for i in range(4)]
        qBT = sb("qBT", [128, 4, 512], BF16)
        qBTB = [Buf("qBT%d" % i) for i in range(4)]
        kAT = sb("kAT", [128, 4, 1024], BF16)
        kATB = [Buf("kAT%d" % i) for i in range(8)]
        kBT = sb("kBT", [128, 2, 1024], BF16)
        kBTB = [Buf("kBT%d" % i) for i in range(8)]
        VA = sb("VA", [128, 8, 8, 65], BF16)
        VAB = [Buf("VA%d" % i) for i in range(8)]
        VB = sb("VB", [128, 8, 2, 65], BF16)
        VBB = [Buf("VB%d" % i) for i in range(8)]
        PTA = sb("PTA", [128, 2, 5, 512], BF16)
        PTAB = [[Buf("PTA%d%d" % (a, b)) for b in range(5)] for a in range(2)]
        PTB = sb("PTB", [128, 2, 2, 512], BF16)
        PTBB = [[Buf("PTB%d%d" % (a, b)) for b in range(2)] for a in range(2)]
        BTA = sb("BTA", [128, 8, 640], BF16)
        BTAs = sb("BTAs", [128, 8, 128], BF16)
        BTB = sb("BTB", [128, 8, 256], BF16)
        BTBs = sb("BTBs", [128, 8, 128], BF16)
        constB = Buf("const")
        yab = sb("yab", [128, 2, 2, 512], BF16)
        yabB = [[Buf("ya0"), Buf("yb0")], [Buf("ya1"), Buf("yb1")]]
        ypar = [0]
        rsA = sb("rsA", [128, 4], F32)
        rsAB = Buf("rsA")
        yaT = sb("yaT", [128, 4, 512], BF16)
        yaTB = [Buf("yaT%d" % i) for i in range(4)]
        ybT = sb("ybT", [128, 4, 512], BF16)
        ybTB = [Buf("ybT%d" % i) for i in range(4)]
        sga = sb("sga", [128, 512], F32)
        sgaB = Buf("sga")
        sgb = sb("sgb", [128, 512], F32)
        sgbB = Buf("sgb")
        mgT = sb("mgT", [128, 8, 512], BF16)
        mgTB = [Buf("mgT%d" % i) for i in range(8)]
        ckb = mgT[:, 0:4, :]
        ckbb = mgT[:, 4, 0:256]
        xt1 = sb("xt1", [128, D], F32)
        xt1B = Buf("xt1")
        Yacc = sb("Yacc", [128, 4, D], F32)
        YaccB = [Buf("Yacc%d" % i) for i in range(4)]
        x1T = sb("x1T", [128, 8, 512], BF16)
        x1TB = [Buf("x1T%d" % i) for i in range(4)]

        lnst = sb("lnst", [128, 2, 6], F32)
        lnstB = Buf("lnst")
        lnmv = sb("lnmv", [128, 2], F32)
        lnmvB = Buf("lnmv")
        lnr = sb("lnr", [128, 1], F32)
        lnrB = Buf("lnr")
        rt = sb("rt", [128, 768], F32)
        rtB = Buf("rt")
        combT = sb("combT", [32, 512], BF16)
        combTB = [Buf("combT%d" % i) for i in range(4)]
        mt = sb("mt", [128, 4, 512], F32)
        s1 = [mt[:, 0, :], mt[:, 1, :]]
        s1B = [Buf("s1_0"), Buf("s1_1")]
        tm = [mt[:, 2, :], mt[:, 3, :]]
        tmB = [Buf("tm_0"), Buf("tm_1")]
        stgBL = [s1B[0], s1B[1], tmB[0]]
        x1Tf = mt[:, 2:4, :].rearrange("p a (b c) -> p (a b) c", c=128)
        hdn = sb("hdn", [128, 2, 2, 512], BF16)
        hdnB = [[Buf("hdn%d%d" % (a, b)) for b in range(2)] for a in range(2)]
        pf = sb("pf", [128, 256], F32)
        pfB = Buf("pf")
        pbt = sb("pbt", [128, 256], BF16)
        pbtB = Buf("pbt")
        pT = sb("pT", [128, 2, 512], BF16)
        pTB = [Buf("pT%d" % i) for i in range(4)]
        stg = mt[:, :, :].rearrange("p a b -> p (a b)")[:, 0:1280]
        lnpb = sb("lnpb", [128, 2, D], F32)
        lnpB = Buf("lnp")
        cm = sb("cm", [32, 2, 512], BF16)
        cmB = [Buf("cm0"), Buf("cm1")]
        ones32 = sb("ones32", [32, 128], BF16)
        wr = sb("wr_sb", [128, 8, 36], F32)
        rb = sb("rb_sb", [128, 36], F32)
        es = sb("es_sb", [128, 8], F32)
        identf = sb("identf", [128, 128], F32)
        identb = sb("identb", [128, 128], BF16)
        Jb = sb("Jb", [128, 128], BF16)
        ones_r = sb("ones_r", [1, 128], BF16)
        hm_b = sb("hm_b", [1, 128], BF16)
        hm_f = sb("hm_f", [1, 128], F32)

        banks = [ctx.enter_context(nc.psum_tensor("bank%d" % i, [128, 512], F32)) for i in range(8)]
        bankB = [Buf("bank%d" % i, excl=True) for i in range(8)]
        bank_rr = [0]

        def nbank():
            i = bank_rr[0] % 8
            bank_rr[0] += 1
            return i

        def bbf(i):
            return banks[i][:, :].bitcast(BF16)

        def mm(out, lhsT, rhs, start, stop, reads, writes, inc=None):
            if inc is None:
                inc = stop
            S.op("pe", lambda e: e.matmul(out, lhsT=lhsT, rhs=rhs, start=start, stop=stop),
                 reads=reads, writes=writes, inc=inc)

        def tr(out, in_, ident, reads, writes, inc=True):
            S.op("pe", lambda e: e.transpose(out=out, in_=in_, identity=ident), reads=reads, writes=writes, inc=inc)

        def act(out, in_, func, reads, writes, scale=1.0, bias=None):
            if bias is None:
                S.op("act", lambda e: e.activation(out=out, in_=in_, func=func, scale=scale), reads=reads, writes=writes)
            else:
                S.op("act", lambda e: e.activation(out=out, in_=in_, func=func, scale=scale, bias=bias), reads=reads, writes=writes)

        def tt(eng, out, in0, in1, op, reads, writes):
            S.op(eng, lambda e: e.tensor_tensor(out=out, in0=in0, in1=in1, op=op), reads=reads, writes=writes)

        def tcopy(eng, out, in_, reads, writes):
            S.op(eng, lambda e: e.tensor_copy(out=out, in_=in_), reads=reads, writes=writes)

        def ts(eng, out, in0, s1_, s2_, op0, op1, reads, writes):
            if s2_ is None:
                S.op(eng, lambda e: e.tensor_scalar(out=out, in0=in0, scalar1=s1_, scalar2=None, op0=op0), reads=reads, writes=writes)
            else:
                S.op(eng, lambda e: e.tensor_scalar(out=out, in0=in0, scalar1=s1_, scalar2=s2_, op0=op0, op1=op1), reads=reads, writes=writes)

        def memset(eng, ap, val, writes):
            S.op(eng, lambda e: e.memset(ap, val), writes=writes)

        def dma(eng, out, in_, sem, reads, writes):
            S.dma(eng, lambda e: e.dma_start(out=out, in_=in_), sem, reads=reads, writes=writes)

        ring_i = [0]
        for i in range(NSLOT):
            S.newsem("ring%d" % i)

        def wget(name):
            i = ring_i[0] % NSLOT
            ring_i[0] += 1
            off, size = CH_OFF[name], CH_SIZE[name]
            dma("sp", slots[i][:, 0:size], wbf.ap()[:, off:off + size], "ring%d" % i, [ppB[CH_PIECE[name]]], [slotB[i]])
            return slots[i], slotB[i]

        S.newsem("setup")
        setup_bufs = []

        def sload(out, in_):
            dma("sp", out, in_, "setup", [], [constB])

        sf = [s[:, :].bitcast(F32) for s in slots]
        ohA0 = sf[0][:, 0:1536].rearrange("p (c n) -> p c n", c=3)
        ohA1 = sf[1][:, 0:768].rearrange("p (c n) -> p c n", c=3)
        tabA = sf[1][:, 768:792].rearrange("p (c n) -> p c n", c=3)
        tabB = sf[1][0:32, 800:808]
        ohB = sf[1][0:32, 1024:1408]
        mA = sf[2][:, 0:640]
        mAs = sf[2][:, 640:768]
        mB = sf[2][:, 768:1024]
        mBs = sf[2][:, 1024:1152]
        toe = sf[3][:, 0:640]
        toeb = sf[3][:, 1024:1280]
        gsb = sf[4][0:8, 256:1024]
        gsbB = slotB[4]
        ohA_v = ohA_d.ap().rearrange("p (c n) -> p c n", c=3)
        S.newsem("su0")
        S.newsem("su1")
        S.newsem("su2")
        dma("sp", ohA0, ohA_v[:, :, 0:512], "su0", [], [slotB[0]])
        dma("sp", ohA1, ohA_v[:, :, 512:768], "su1", [], [slotB[1]])
        dma("sp", tabA, tabA_d.ap().rearrange("p (c n) -> p c n", c=3), "su1", [], [slotB[1]])
        dma("sp", tabB, tabB_d.ap(), "su1", [], [slotB[1]])
        dma("sp", ohB, ohB_d.ap(), "su1", [], [slotB[1]])
        dma("sp", mA, mA_d.ap(), "su2", [], [slotB[2]])
        dma("sp", mAs, mAs_d.ap(), "su2", [], [slotB[2]])
        dma("sp", mB, mB_d.ap(), "su2", [], [slotB[2]])
        dma("sp", mBs, mBs_d.ap(), "su2", [], [slotB[2]])
        sload(wr[:, :, :], wr_d.ap().rearrange("p (k n) -> p k n", k=8))
        sload(rb[:, :], rb_d.ap().partition_broadcast(128))
        sload(es[:, :], sinks_d.ap().partition_broadcast(128))
        sload(hm_f[:, :], hm_d.ap())
        constB.w = ("setup", S.cnt["setup"])

        memset("dve", identf[:, :], 0.0, [constB])
        S.op("pool", lambda e: e.affine_select(out=identf[:, :], in_=identf[:, :], pattern=[[-1, 128]],
                                               compare_op=ALU.not_equal, fill=1.0, base=0, channel_multiplier=1),
             reads=[constB], writes=[constB])
        tcopy("dve", identb[:, :], identf[:, :], [constB], [constB])
        jf = sf[4][:, 0:128]
        memset("dve", jf, 0.0, [slotB[4]])
        S.op("pool", lambda e: e.affine_select(out=jf, in_=jf, pattern=[[1, 128]], compare_op=ALU.not_equal,
                                               fill=1.0, base=-127, channel_multiplier=1),
             reads=[slotB[4]], writes=[slotB[4]])
        tcopy("dve", Jb[:, :], jf, [slotB[4]], [constB])

        memset("dve", ones32[:, :], 1.0, [constB])
        memset("dve", ones_r[:, :], 1.0, [constB])
        tcopy("dve", hm_b[:, :], hm_f[:, :], [constB], [constB])
        act(es[:, :], es[:, :], AF.Exp, [constB], [constB])
        memset("dve", VA[:, :, :, :], 1.0, VAB)
        memset("dve", xT[:, :, :], 0.0, xTB)
        memset("dve", qAT[:, :, :], 0.0, qATB)
        memset("dve", qBT[:, :, :], 0.0, qBTB)
        memset("dve", VB[:, :, :, :], 1.0, VBB)

        b0, b1 = nbank(), nbank()
        for c in range(3):
            mm(banks[b0][0:8, 0:512], tabA[:, c, :], ohA0[:, c, :], c == 0, c == 2, [slotB[0], slotB[1]], [bankB[b0]])
        for c in range(3):
            mm(banks[b1][0:8, 0:256], tabA[:, c, :], ohA1[:, c, :], c == 0, c == 2, [slotB[1]], [bankB[b1]])
        act(gsb[:, 0:512], banks[b0][0:8, 0:512], AF.Copy, [bankB[b0]], [gsbB])
        act(gsb[:, 512:768], banks[b1][0:8, 0:256], AF.Copy, [bankB[b1]], [gsbB])
        S.newsem("gA")
        GAB = Buf("GA")
        dma("sp", GA_d.ap(), gsb[:, :], "gA", [gsbB], [GAB])
        b2 = nbank()
        mm(banks[b2][0:8, 0:384], tabB, ohB, True, True, [slotB[1]], [bankB[b2]])
        act(gsb[:, 0:384], banks[b2][0:8, 0:384], AF.Copy, [bankB[b2]], [gsbB])
        S.newsem("gB")
        GBB = Buf("GB")
        dma("sp", GB_d.ap(), gsb[:, 0:384], "gB", [gsbB], [GBB])
        S.newsem("toe")
        S.newsem("toeb")

        def toeplitz_loads():
            dma("pool", BTA[:, :, :], bass.AP(GA_d, 0, [[1, 128], [768, 8], [1, 640]]), "toe", [GAB], [constB])
            dma("pool", BTB[:, :, :], bass.AP(GB_d, 0, [[1, 128], [384, 8], [1, 256]]), "toeb", [GBB], [constB])
            constB.w = ("toe", S.cnt["toe"])
            tt("dve", BTAs[:, :, :], BTA[:, :, 512:640], mAs.unsqueeze(1).broadcast_to([128, 8, 128]), ALU.add,
               [constB, slotB[2]], [constB])
            tt("dve", BTA[:, :, :], BTA[:, :, :], mA.unsqueeze(1).broadcast_to([128, 8, 640]), ALU.add,
               [constB, slotB[2]], [constB])
            constB.w = ("toeb", S.cnt["toeb"])
            tt("dve", BTBs[:, :, :], BTB[:, :, 128:256], mBs.unsqueeze(1).broadcast_to([128, 8, 128]), ALU.add,
               [constB, slotB[2]], [constB])
            tt("dve", BTB[:, :, :], BTB[:, :, :], mB.unsqueeze(1).broadcast_to([128, 8, 256]), ALU.add,
               [constB, slotB[2]], [constB])

        ppB = []
        for i, (a, b) in enumerate(PIECES):
            lo, hi = CH_OFF[a], CH_OFF[b] + CH_SIZE[b]
            S.newsem("pp%d" % i)
            bb = Buf("pp%d" % i)
            ppB.append(bb)
            prev = [ppB[i - 2]] if i >= 2 else []
            c = lo
            while c < hi:
                c2 = min(hi, c + 8192)
                dma("pool", wbf.ap()[:, c:c2], w32.ap()[:, c:c2], "pp%d" % i, prev, [bb])
                c = c2
            if i == 1:
                toeplitz_loads()

        S.newsem("xf0")
        S.newsem("xf1")
        S.newsem("pf")
        S.newsem("stg")
        S.newsem("cc")
        S.newsem("ckb")
        S.newsem("ckbb")
        S.newsem("ckvb")
        S.newsem("lnp")
        S.newsem("ckv")
        for i in range(4):
            S.newsem("yo%d" % i)
        out_toks = []
        xf_rr = [0]

        def load_x(row0):
            i = xf_rr[0] % 2
            xf_rr[0] += 1
            dma("sp", xf[i][:, :], xin.ap()[row0:row0 + 128, :], "xf%d" % i, [], [xfB[i]])
            return i

        def make_xT(row0, NT, tiles=None):
            for t in (range(NT) if tiles is None else tiles):
                i = load_x(row0 + t * 128)
                act(xb[:, :], xf[i][:, :], AF.Copy, [xfB[i]], [xbB])
                bk = nbank()
                for kc in range(8):
                    tr(bbf(bk)[:, kc * 128:(kc + 1) * 128], xb[:, kc * 128:(kc + 1) * 128], identb[:, :],
                       [xbB, constB], [bankB[bk]], inc=(kc == 7))
                act(xT[:, :, t * 128:(t + 1) * 128], bbf(bk).rearrange("p (k n) -> p k n", k=8), AF.Copy,
                    [bankB[bk]], [xTB[t]])

        def proj_fm(wname, ngrp, dst, dstB_of, col0, N, NT):
            sl, slB = wget(wname)
            wv = sl[:, 0:ngrp * 1024].rearrange("p (g k n) -> p g k n", g=ngrp, k=8)
            for g in range(ngrp):
                bk = nbank()
                for kc in range(8):
                    mm(banks[bk][:, 0:N], wv[:, g, kc, :], xT[:, kc, 0:N], kc == 0, kc == 7,
                       [slB] + xTB[:NT], [bankB[bk]])
                act(dst[:, g, col0:col0 + N], banks[bk][:, 0:N], AF.Copy, [bankB[bk]], dstB_of(g))

        def proj_v_make(pos_of, cacheout, after_tile):
            st = {}

            def tile(t):
                if not st:
                    st["a"] = wget("TVA")
                    st["b"] = wget("TVB")
                    if cacheout:
                        st["ka"] = wget("TKA")
                        st["kb"] = wget("TKB")
                slA, slAB = st["a"]
                slB_, slBB = st["b"]
                wa = slA[:, 0:4096].rearrange("p (k n) -> p k n", k=8)
                wb = slB_[:, 0:1024].rearrange("p (k n) -> p k n", k=8)
                pos = pos_of(t)
                ba, bb_ = nbank(), nbank()
                for kc in range(8):
                    mm(banks[ba][:, 0:512], xT[:, kc, t * 128:(t + 1) * 128], wa[:, kc, :], kc == 0, kc == 7,
                       [slAB, xTB[t]], [bankB[ba]])
                for kc in range(8):
                    mm(banks[bb_][:, 0:128], xT[:, kc, t * 128:(t + 1) * 128], wb[:, kc, :], kc == 0, kc == 7,
                       [slBB, xTB[t]], [bankB[bb_]])
                act(VA[:, pos, :, 0:64], banks[ba][:, 0:512].rearrange("p (h d) -> p h d", h=8), AF.Copy,
                    [bankB[ba]], [VAB[pos]])
                act(VB[:, pos, :, 0:64], banks[bb_][:, 0:128].rearrange("p (h d) -> p h d", h=2), AF.Copy,
                    [bankB[bb_]], [VBB[pos]])
                if cacheout:
                    slKA, slKAB = st["ka"]
                    slKB, slKBB = st["kb"]
                    wka = slKA[:, 0:4096].rearrange("p (k n) -> p k n", k=8)
                    wkb = slKB[:, 0:1024].rearrange("p (k n) -> p k n", k=8)
                    tcopy("dve", stg[:, 640:1152], banks[ba][:, 0:512], [bankB[ba]], stgBL)
                    tcopy("dve", stg[:, 1152:1280], banks[bb_][:, 0:128], [bankB[bb_]], stgBL)
                    bka, bkb = nbank(), nbank()
                    for kc in range(8):
                        mm(banks[bka][:, 0:512], xT[:, kc, t * 128:(t + 1) * 128], wka[:, kc, :], kc == 0, kc == 7,
                           [slKAB, xTB[t]], [bankB[bka]])
                    for kc in range(8):
                        mm(banks[bkb][:, 0:128], xT[:, kc, t * 128:(t + 1) * 128], wkb[:, kc, :], kc == 0, kc == 7,
                           [slKBB, xTB[t]], [bankB[bkb]])
                    tcopy("dve", stg[:, 0:512], banks[bka][:, 0:512], [bankB[bka]], stgBL)
                    tcopy("dve", stg[:, 512:640], banks[bkb][:, 0:128], [bankB[bkb]], stgBL)
                    after_tile(t)
            return tile

        def attn_pair(qa, ka, va, ta, qb, kb, vb, tb, haloA, haloB, rdA, rdB):
            def scoresA(hg):
                for kt in range(5):
                    bk = nbank()
                    for hh in range(4):
                        h = 4 * hg + hh
                        o = banks[bk][:, hh * 128:(hh + 1) * 128]
                        mm(o, ka(h, kt), qa(h), True, False, rdA(kt), [bankB[bk]])
                        if kt in haloA:
                            mm(o, hm_b[0:1, :], ones_r[0:1, :], False, False, [constB], [bankB[bk]])
                        mm(o, ta(h, kt), Jb[:, :], False, True, [constB], [bankB[bk]], inc=(hh == 3))
                    act(PTA[:, hg, kt, :], banks[bk][:, :], AF.Exp, [bankB[bk]], [PTAB[hg][kt]], scale=0.125)

            def scoresB():
                for kv in range(2):
                    for kt in range(2):
                        bk = nbank()
                        for half in range(2):
                            o2 = banks[bk][:, half * 256:(half + 1) * 256].rearrange("p (a b) -> p a b", a=2)
                            mm(o2, kb(kv, half, kt), qb(kv, half), True, False, rdB(kt), [bankB[bk]])
                            for j in range(2):
                                blk = half * 2 + j
                                o = banks[bk][:, blk * 128:(blk + 1) * 128]
                                if kt in haloB:
                                    mm(o, hm_b[0:1, :], ones_r[0:1, :], False, False, [constB], [bankB[bk]])
                                mm(o, tb(4 * kv + blk, kt), Jb[:, :], False, j == 1, [constB], [bankB[bk]],
                                   inc=(blk == 3))
                        act(PTB[:, kv, kt, :], banks[bk][:, :], AF.Exp, [bankB[bk]], [PTBB[kv][kt]], scale=0.125)

            def pvA(hg):
                bk = nbank()
                for hh in range(4):
                    h = 4 * hg + hh
                    for kt in range(5):
                        mm(banks[bk][:, hh * 65:(hh + 1) * 65], PTA[:, hg, kt, hh * 128:(hh + 1) * 128], va(kt, h),
                           kt == 0, kt == 4, [PTAB[hg][kt]] + rdA(kt), [bankB[bk]], inc=(kt == 4 and hh == 3))
                ov = banks[bk][:, 0:260].rearrange("p (h d) -> p h d", d=65)
                S.op("dve", lambda e, ov=ov: e.reciprocal(out=rsA[:, :], in_=ov[:, :, 64]), reads=[bankB[bk]], writes=[rsAB])
                tt("dve", ya[:, hg * 256:(hg + 1) * 256].rearrange("p (h d) -> p h d", h=4), ov[:, :, 0:64],
                   rsA[:, :].unsqueeze(2).broadcast_to([128, 4, 64]), ALU.mult, [bankB[bk], rsAB], [yaB])

            def pvB():
                for kv in range(2):
                    bk = nbank()
                    for blk in range(4):
                        for kt in range(2):
                            mm(banks[bk][:, blk * 65:(blk + 1) * 65], PTB[:, kv, kt, blk * 128:(blk + 1) * 128], vb(kt, kv),
                               kt == 0, kt == 1, [PTBB[kv][kt]] + rdB(kt), [bankB[bk]], inc=(kt == 1 and blk == 3))
                    ov = banks[bk][:, 0:260].rearrange("p (h d) -> p h d", d=65)
                    tt("dve", rsA[:, :], ov[:, :, 64], es[:, kv * 4:(kv + 1) * 4], ALU.add, [bankB[bk], constB], [rsAB])
                    S.op("dve", lambda e: e.reciprocal(out=rsA[:, :], in_=rsA[:, :]), reads=[rsAB], writes=[rsAB])
                    tt("dve", yb[:, kv * 256:(kv + 1) * 256].rearrange("p (h d) -> p h d", h=4), ov[:, :, 0:64],
                       rsA[:, :].unsqueeze(2).broadcast_to([128, 4, 64]), ALU.mult, [bankB[bk], rsAB], [ybB])

            par = ypar[0] % 2
            ypar[0] += 1
            ya, yb = yab[:, par, 0, :], yab[:, par, 1, :]
            yaB, ybB = yabB[par]
            scoresA(0)
            scoresA(1)
            scoresB()
            pvA(0)
            pvA(1)
            pvB()
            return ya, yaB, yb, ybB

        def y_transposes(yy, dstA, dstB_, dstAB, dstBB, ncols):
            ya, yaB, yb, ybB = yy
            for (src, srcB, dst, dB) in ((ya, yaB, dstA, dstAB), (yb, ybB, dstB_, dstBB)):
                bk = nbank()
                for c in range(4):
                    tr(bbf(bk)[:, c * 128:(c + 1) * 128], src[:, c * 128:(c + 1) * 128], identb[:, :],
                       [srcB, constB], [bankB[bk]], inc=(c == 3))
                act(dst, bbf(bk)[:, 0:512].rearrange("p (c t) -> p c t", c=4)[:, :, 0:ncols], AF.Copy,
                    [bankB[bk]], dB)

        def load_ln(which):
            dma("pool", lnpb[:, :, :],
                lnp_d.ap()[:, which * 2048:(which + 1) * 2048].partition_broadcast(128).rearrange("p o (a n) -> p (o a) n", a=2),
                "lnp", [], [lnpB])

        def layer_norm(xap, xB, gi, out_ap, outB):
            for h in range(2):
                S.op("dve", lambda e, h=h: e.bn_stats(out=lnst[:, h, :], in_=xap[:, h * 512:(h + 1) * 512]),
                     reads=[xB], writes=[lnstB])
            S.op("dve", lambda e: e.bn_aggr(out=lnmv[:, :], in_=lnst[:, :, :]), reads=[lnstB], writes=[lnmvB])
            ts("dve", lnr[:, :], lnmv[:, 1:2], 1e-5, None, ALU.add, None, [lnmvB], [lnrB])
            act(lnr[:, :], lnr[:, :], AF.Ln, [lnrB], [lnrB])
            act(lnr[:, :], lnr[:, :], AF.Exp, [lnrB], [lnrB], scale=-0.5)
            ts("dve", xap, xap, lnmv[:, 0:1], lnr[:, 0:1], ALU.subtract, ALU.mult, [xB, lnmvB, lnrB], [xB])
            tt("dve", xap, xap, lnpb[:, 0, :], ALU.mult, [xB, lnpB], [xB])
            tt("dve", out_ap, xap, lnpb[:, 1, :], ALU.add, [xB, lnpB], [outB] if outB is not xB else [xB])

        def blk(kind, bi):
            sample = kind == "sample"
            NT = 1 if sample else 4
            if kind == "halo":
                row0 = 0
            elif kind == "main":
                row0 = HALO + bi * 512
            else:
                row0 = HALO + SEG
            gbase = 0 if kind == "halo" else (4 + 4 * bi if kind == "main" else 7)
            pos_of = lambda t: (gbase + t) % 8
            kcol0 = 896 if sample else (pos_of(0) * 128)
            cacheout = sample or (kind == "main" and bi == NBLK - 1)
            return sample, NT, NT * 128, row0, gbase, pos_of, kcol0, cacheout

        def stageB_items(kind, bi):
            sample, NT, N, row0, gbase, pos_of, kcol0, cacheout = blk(kind, bi)
            items = []
            for t in range(NT):
                items.append(lambda t=t: make_xT(row0, NT, [t]))
            items.append(lambda: proj_fm("K1", 4, kAT, lambda g: [kATB[pos_of(t)] for t in range(NT)], kcol0, N, NT))
            items.append(lambda: proj_fm("K2", 2, kBT, lambda g: [kBTB[pos_of(t)] for t in range(NT)], kcol0, N, NT))
            if kind != "halo":
                items.append(lambda: proj_fm("Q1", 4, qAT, lambda g: [qATB[g]], 0, N, NT))
                items.append(lambda: proj_fm("Q2", 4, qBT, lambda g: [qBTB[g]], 0, N, NT))

            def after_tile(t):
                if kind == "main":
                    r = t * 128
                    dma("pool", ak_p.ap()[r:r + 128, :], stg[:, 0:512], "stg", stgBL, [])
                    dma("pool", av_p.ap()[r:r + 128, :], stg[:, 640:1152], "stg", stgBL, [])
                    if t == 3:
                        dma("pool", bk_p.ap(), stg[:, 512:640], "stg", stgBL, [])
                        dma("pool", bv_p.ap(), stg[:, 1152:1280], "stg", stgBL, [])
                else:
                    for s_ in range(4):
                        ps_ = slice(32 * s_, 32 * s_ + 32)
                        dma("pool", ak_s.ap()[s_, 480:512, :], stg[ps_, 0:512], "stg", stgBL, [])
                        dma("pool", av_s.ap()[s_, 480:512, :], stg[ps_, 640:1152], "stg", stgBL, [])
                        dma("pool", bk_s.ap()[s_, 96:128, :], stg[ps_, 512:640], "stg", stgBL, [])
                        dma("pool", bv_s.ap()[s_, 96:128, :], stg[ps_, 1152:1280], "stg", stgBL, [])

            pv = proj_v_make(pos_of, cacheout, after_tile)
            for t in range(NT):
                items.append(lambda t=t: pv(t))
            return items

        def do_rest(kind, bi, next_items):
            sample, NT, N, row0, gbase, pos_of, kcol0, cacheout = blk(kind, bi)
            load_ln(0)

            if not sample:
                for pi in range(4):
                    g = gbase + pi
                    qc = slice(pi * 128, (pi + 1) * 128)

                    def kcolsA(kt, g=g):
                        p = (g - 4 + kt) % 8
                        return slice(p * 128, (p + 1) * 128), p

                    def kcolsB(kt, g=g):
                        p = (g - 1 + kt) % 8
                        return slice(p * 128, (p + 1) * 128), p

                    haloA = set(kt for kt in range(5) if bi == 0 and pi + kt < 4)
                    haloB = set(kt for kt in range(2) if bi == 0 and 3 + pi + kt < 4)
                    yy = attn_pair(
                        qa=lambda h, qc=qc: qAT[(h % 2) * 64:(h % 2) * 64 + 64, h // 2, qc],
                        ka=lambda h, kt, f=kcolsA: kAT[(h % 2) * 64:(h % 2) * 64 + 64, h // 2, f(kt)[0]],
                        va=lambda kt, h, f=kcolsA: VA[:, f(kt)[1], h, :],
                        ta=lambda h, kt: BTA[:, h, kt * 128:(kt + 1) * 128],
                        qb=lambda kv, half, qc=qc: qBT[half * 64:half * 64 + 64, 2 * kv:2 * kv + 2, qc],
                        kb=lambda kv, half, kt, f=kcolsB: kBT[half * 64:half * 64 + 64, kv, f(kt)[0]],
                        vb=lambda kt, kv, f=kcolsB: VB[:, f(kt)[1], kv, :],
                        tb=lambda h, kt: BTB[:, h, kt * 128:(kt + 1) * 128],
                        haloA=haloA, haloB=haloB,
                        rdA=lambda kt, f=kcolsA: [kATB[f(kt)[1]], VAB[f(kt)[1]]] + qATB,
                        rdB=lambda kt, f=kcolsB: [kBTB[f(kt)[1]], VBB[f(kt)[1]]] + qBTB,
                    )
                    y_transposes(yy, yaT[:, :, qc], ybT[:, :, qc], [yaTB[pi]], [ybTB[pi]], 128)
            else:
                slA, slAB = wget("TVA")
                slB_, slBB = wget("TVB")
                wa = slA[:, 0:4096].rearrange("p (k n) -> p k n", k=8)
                wb = slB_[:, 0:1024].rearrange("p (k n) -> p k n", k=8)
                memset("dve", kAT[:, :, 544:640], 0.0, [kATB[4]])
                memset("dve", kBT[:, :, 160:256], 0.0, [kBTB[1]])
                for s in range(4):
                    dma("pool", ckb[:, :, :], cak.ap()[s].rearrange("(t p) f -> p t f", p=128), "ckb", [], mgTB[0:4])
                    dma("pool", ckbb[:, :], cbkd.ap()[s], "ckbb", [], [mgTB[4]])
                    for c2 in range(2):
                        bk = nbank()
                        for cc in range(2):
                            c = 2 * c2 + cc
                            for t in range(4):
                                tr(bbf(bk)[:, (cc * 4 + t) * 128:(cc * 4 + t + 1) * 128], ckb[:, t, c * 128:(c + 1) * 128],
                                   identb[:, :], mgTB[0:4] + [constB], [bankB[bk]], inc=(cc == 1 and t == 3))
                        act(kAT[:, 2 * c2:2 * c2 + 2, 0:512], bbf(bk).rearrange("p (c n) -> p c n", c=2), AF.Copy,
                            [bankB[bk]], kATB[0:4])
                    bk = nbank()
                    for kv in range(2):
                        tr(bbf(bk)[:, kv * 128:(kv + 1) * 128], ckbb[:, kv * 128:(kv + 1) * 128], identb[:, :],
                           [mgTB[4], constB], [bankB[bk]], inc=(kv == 1))
                    act(kBT[:, :, 0:128], bbf(bk)[:, 0:256].rearrange("p (c n) -> p c n", c=2), AF.Copy,
                        [bankB[bk]], [kBTB[0]])
                    act(kAT[:, :, 512:544], kAT[:, :, 896 + 32 * s:928 + 32 * s], AF.Copy, [kATB[7]], [kATB[4]])
                    act(kBT[:, :, 128:160], kBT[:, :, 896 + 32 * s:928 + 32 * s], AF.Copy, [kBTB[7]], [kBTB[1]])
                    for t4 in range(4):
                        dma("pool", VA[:, t4, :, 0:64],
                            cav.ap()[s, t4 * 128:(t4 + 1) * 128, :].rearrange("p (h d) -> p h d", h=8),
                            "ckv", [], VAB[0:4])
                    dma("pool", VB[:, 0, :, 0:64], cbv.ap()[s].rearrange("p (h d) -> p h d", h=2), "ckvb", [], [VBB[0]])
                    ba, bb_ = nbank(), nbank()
                    for kc in range(8):
                        mm(banks[ba][:, 0:512], xT[:, kc, 32 * s:32 * s + 128], wa[:, kc, :], kc == 0, kc == 7,
                           [slAB] + xTB[0:2], [bankB[ba]])
                    for kc in range(8):
                        mm(banks[bb_][:, 0:128], xT[:, kc, 32 * s:32 * s + 128], wb[:, kc, :], kc == 0, kc == 7,
                           [slBB] + xTB[0:2], [bankB[bb_]])
                    act(VA[:, 4, :, 0:64], banks[ba][:, 0:512].rearrange("p (h d) -> p h d", h=8), AF.Copy,
                        [bankB[ba]], [VAB[4]])
                    act(VB[:, 1, :, 0:64], banks[bb_][:, 0:128].rearrange("p (h d) -> p h d", h=2), AF.Copy,
                        [bankB[bb_]], [VBB[1]])
                    qc = slice(32 * s, 32 * s + 128)
                    yy = attn_pair(
                        qa=lambda h, qc=qc: qAT[(h % 2) * 64:(h % 2) * 64 + 64, h // 2, qc],
                        ka=lambda h, kt: kAT[(h % 2) * 64:(h % 2) * 64 + 64, h // 2, kt * 128:(kt + 1) * 128],
                        va=lambda kt, h: VA[:, kt, h, :],
                        ta=lambda h, kt: (BTA[:, h, kt * 128:(kt + 1) * 128] if kt < 4 else BTAs[:, h, :]),
                        qb=lambda kv, half, qc=qc: qBT[half * 64:half * 64 + 64, 2 * kv:2 * kv + 2, qc],
                        kb=lambda kv, half, kt: kBT[half * 64:half * 64 + 64, kv, kt * 128:(kt + 1) * 128],
                        vb=lambda kt, kv: VB[:, kt, kv, :],
                        tb=lambda h, kt: (BTB[:, h, 0:128] if kt == 0 else BTBs[:, h, :]),
                        haloA=set(), haloB=set(),
                        rdA=lambda kt: [kATB[kt], VAB[kt]] + qATB,
                        rdB=lambda kt: [kBTB[kt], VBB[kt]] + qBTB,
                    )
                    y_transposes(yy, yaT[:, :, 32 * s:32 * s + 32], ybT[:, :, 32 * s:32 * s + 32], [yaTB[0]], [ybTB[0]], 32)

            for j in range(8):
                sl, slB = wget("D%d" % j)
                gaw = sl[:, 0:1024].rearrange("p (k n) -> p k n", k=8)
                gbw = sl[:, 1024:2048].rearrange("p (k n) -> p k n", k=8)
                paw = sl[:, 2048:2560].rearrange("p (k n) -> p k n", k=4)
                pbw = sl[:, 2560:3072].rearrange("p (k n) -> p k n", k=4)
                bga, bgb, bpa, bpb = nbank(), nbank(), nbank(), nbank()
                for kc in range(8):
                    mm(banks[bga][:, 0:N], gaw[:, kc, :], xT[:, kc, 0:N], kc == 0, kc == 7, [slB] + xTB[:NT], [bankB[bga]])
                for kc in range(8):
                    mm(banks[bgb][:, 0:N], gbw[:, kc, :], xT[:, kc, 0:N], kc == 0, kc == 7, [slB] + xTB[:NT], [bankB[bgb]])
                for kc in range(4):
                    mm(banks[bpa][:, 0:N], paw[:, kc, :], yaT[:, kc, 0:N], kc == 0, kc == 3, [slB] + yaTB[:NT], [bankB[bpa]])
                for kc in range(4):
                    mm(banks[bpb][:, 0:N], pbw[:, kc, :], ybT[:, kc, 0:N], kc == 0, kc == 3, [slB] + ybTB[:NT], [bankB[bpb]])
                act(sga[:, 0:N], banks[bga][:, 0:N], AF.Sigmoid, [bankB[bga]], [sgaB])
                act(sgb[:, 0:N], banks[bgb][:, 0:N], AF.Sigmoid, [bankB[bgb]], [sgbB])
                tt("dve", sga[:, 0:N], sga[:, 0:N], banks[bpa][:, 0:N], ALU.mult, [sgaB, bankB[bpa]], [sgaB])
                tt("dve", sgb[:, 0:N], sgb[:, 0:N], banks[bpb][:, 0:N], ALU.mult, [sgbB, bankB[bpb]], [sgbB])
                tt("dve", mgT[:, j, 0:N], sga[:, 0:N], sgb[:, 0:N], ALU.add, [sgaB, sgbB], [mgTB[j]])

            wo0, wo0B = wget("WO0")
            wo1, wo1B = wget("WO1")
            wov = [wo0[:, :].rearrange("p (k n) -> p k n", k=4), wo1[:, :].rearrange("p (k n) -> p k n", k=4)]
            AXX = mybir.AxisListType.X
            lgall = rt[:, 0:NT * 36].rearrange("p (t n) -> p t n", t=NT)
            wo_banks = {}

            def wo_mm(t):
                tok = slice(t * 128, (t + 1) * 128)
                bo = [nbank(), nbank()]
                wo_banks[t] = bo
                for nh in range(2):
                    for kc in range(8):
                        mm(banks[bo[nh]][:, :], mgT[:, kc, tok], wov[kc // 4][:, kc % 4, nh * 512:(nh + 1) * 512],
                           kc == 0, kc == 7, [wo0B, wo1B] + mgTB, [bankB[bo[nh]]])

            def ln_part(t):
                bo = wo_banks[t]
                xi = load_x(row0 + t * 128)
                for nh in range(2):
                    S.op("dve", lambda e, nh=nh, xi=xi, bo=bo: e.scalar_tensor_tensor(
                        out=xt1[:, nh * 512:(nh + 1) * 512], in0=xf[xi][:, nh * 512:(nh + 1) * 512], scalar=ALPHA,
                        in1=banks[bo[nh]][:, :], op0=ALU.mult, op1=ALU.add),
                        reads=[xfB[xi], bankB[bo[nh]]], writes=[xt1B])
                layer_norm(xt1[:, :], xt1B, 0, xt1[:, :], xt1B)
                act(Yacc[:, t, :], xt1[:, :], AF.Copy, [xt1B], [YaccB[t]], scale=ALPHA)

            def part2(t):
                tok = slice(t * 128, (t + 1) * 128)
                bt = [nbank(), nbank()]
                for kc in range(8):
                    tr(banks[bt[kc // 4]][:, (kc % 4) * 128:(kc % 4 + 1) * 128], xt1[:, kc * 128:(kc + 1) * 128],
                       identf[:, :], [xt1B, constB], [bankB[bt[kc // 4]]], inc=(kc % 4 == 3))
                for hb in range(2):
                    bv = banks[bt[hb]][:, :].rearrange("p (k n) -> p k n", k=4)
                    act(x1T[:, hb * 4:(hb + 1) * 4, tok], bv, AF.Copy, [bankB[bt[hb]]], [x1TB[t]])
                    tcopy("dve", x1Tf[:, hb * 4:(hb + 1) * 4, :], bv, [bankB[bt[hb]]], [tmB[0], tmB[1]])
                bl = nbank()
                for kc in range(8):
                    mm(banks[bl][:, 0:36], x1Tf[:, kc, :], wr[:, kc, :], kc == 0, kc == 7, [tmB[0], tmB[1], constB], [bankB[bl]])
                tt("dve", lgall[:, t, :], banks[bl][:, 0:36], rb[:, :], ALU.add, [bankB[bl], constB], [rtB])

            wo_mm(0)
            for t in range(NT):
                ln_part(t)
                if t + 1 < NT:
                    wo_mm(t + 1)
                part2(t)

            o = NT * 36

            def rsl(n):
                nonlocal o
                v = rt[:, o:o + n]
                o += n
                return v

            G = lgall[:, :, 0:4]
            E = lgall[:, :, 4:36]
            gmax = rsl(NT)
            goh = rsl(NT * 4).rearrange("p (t n) -> p t n", t=NT)
            gex = rsl(NT * 4).rearrange("p (t n) -> p t n", t=NT)
            gsum = rsl(NT)
            gw = rsl(NT)
            gpen = rsl(NT * 4)
            em = rsl(NT * 32)
            m8 = rsl(NT * 8).rearrange("p (t n) -> p t n", t=NT)
            oh1 = rsl(NT * 32).rearrange("p (t n) -> p t n", t=NT)
            oh2 = rsl(NT * 32).rearrange("p (t n) -> p t n", t=NT)
            dlt = rsl(NT)
            w1_ = rsl(NT)
            w2_ = rsl(NT)
            R = [rtB]
            bc4 = lambda v: v.unsqueeze(2).broadcast_to([128, NT, 4])
            bc32 = lambda v: v.unsqueeze(2).broadcast_to([128, NT, 32])
            S.op("dve", lambda e: e.tensor_reduce(out=gmax, in_=G, axis=AXX, op=ALU.max), reads=R, writes=R)
            tt("dve", goh, G, bc4(gmax), ALU.is_equal, R, R)
            tt("dve", gex, G, bc4(gmax), ALU.subtract, R, R)
            act(gex, gex, AF.Exp, R, R)
            S.op("dve", lambda e: e.tensor_reduce(out=gsum, in_=gex, axis=AXX, op=ALU.add), reads=R, writes=R)
            S.op("dve", lambda e: e.reciprocal(out=gw, in_=gsum), reads=R, writes=R)
            ts("dve", gpen, goh.rearrange("p t n -> p (t n)"), -1.0, 1e30, ALU.add, ALU.mult, R, R)
            for t in range(NT):
                tt("dve", em[:, t * 32:(t + 1) * 32].rearrange("p (g e) -> p g e", g=4),
                   lgall[:, t, 4:36].rearrange("p (g e) -> p g e", g=4),
                   gpen[:, t * 4:(t + 1) * 4].unsqueeze(2).broadcast_to([128, 4, 8]), ALU.add, R, R)
            for t in range(NT):
                S.op("dve", lambda e, t=t: e.max(out=m8[:, t, :], in_=em[:, t * 32:(t + 1) * 32]), reads=R, writes=R)
            emv = em.rearrange("p (t n) -> p t n", t=NT)
            tt("dve", oh1, emv, bc32(m8[:, :, 0]), ALU.is_equal, R, R)
            tt("dve", oh2, emv, bc32(m8[:, :, 1]), ALU.is_equal, R, R)
            tt("dve", dlt, m8[:, :, 1], m8[:, :, 0], ALU.subtract, R, R)
            act(dlt, dlt, AF.Exp, R, R)
            ts("dve", dlt, dlt, 1.0, None, ALU.add, None, R, R)
            S.op("dve", lambda e: e.reciprocal(out=w1_, in_=dlt), reads=R, writes=R)
            tt("dve", w1_, w1_, gw, ALU.mult, R, R)
            tt("dve", w2_, gw, w1_, ALU.subtract, R, R)
            tt("dve", oh1, oh1, bc32(w1_), ALU.mult, R, R)
            tt("dve", oh2, oh2, bc32(w2_), ALU.mult, R, R)
            tt("dve", oh1, oh1, oh2, ALU.add, R, R)
            for t in range(NT):
                bc_ = nbank()
                tr(banks[bc_][0:32, 0:128], oh1[:, t, :], identf[:, :], [rtB, constB], [bankB[bc_]])
                act(combT[:, t * 128:(t + 1) * 128], banks[bc_][0:32, 0:128], AF.Copy, [bankB[bc_]], [combTB[t]])

            load_ln(1)
            prow0 = (bi * 512) if kind == "main" else SEG
            for t in range(NT):
                dma("sp", pf[:, :], pin.ap()[prow0 + t * 128:prow0 + (t + 1) * 128, :], "pf", [], [pfB])
                act(pbt[:, :], pf[:, :], AF.Copy, [pfB], [pbtB])
                bk = nbank()
                for c in range(2):
                    tr(bbf(bk)[:, c * 128:(c + 1) * 128], pbt[:, c * 128:(c + 1) * 128], identb[:, :], [pbtB, constB],
                       [bankB[bk]], inc=(c == 1))
                act(pT[:, :, t * 128:(t + 1) * 128], bbf(bk)[:, 0:256].rearrange("p (c n) -> p c n", c=2), AF.Copy,
                    [bankB[bk]], [pTB[t]])
            for eg in range(16):
                wslots = []
                for ee in range(2):
                    e_ = 2 * eg + ee
                    ea, eaB = wget("EA%d" % e_)
                    eb, ebB = wget("EB%d" % e_)
                    wslots.append((eb, ebB))
                    w1v = ea[:, 0:2048].rearrange("p (k n) -> p k n", k=8)
                    w3v = ea[:, 2048:4096].rearrange("p (k n) -> p k n", k=8)
                    ci = e_ % 2
                    hb_ = []
                    for hc in range(2):
                        bh1, bh3 = nbank(), nbank()
                        hb_.append((bh1, bh3))
                        for kc in range(8):
                            mm(banks[bh1][:, 0:N], w1v[:, kc, hc * 128:(hc + 1) * 128], x1T[:, kc, 0:N], kc == 0, kc == 7,
                               [eaB] + x1TB[:NT], [bankB[bh1]])
                        for kc in range(8):
                            mm(banks[bh3][:, 0:N], w3v[:, kc, hc * 128:(hc + 1) * 128], x1T[:, kc, 0:N], kc == 0, kc == 7,
                               [eaB] + x1TB[:NT], [bankB[bh3]])
                    bcb = nbank()
                    act(cm[:, ci, 0:N], combT[:, 0:N], AF.Copy, combTB[:NT] + [constB], [cmB[ci]], scale=identf[0:32, e_:e_ + 1])
                    mm(banks[bcb][:, 0:N], ones32[:, :], cm[:, ci, 0:N], True, True, [cmB[ci], constB], [bankB[bcb]])
                    for hc in range(2):
                        bh1, bh3 = hb_[hc]
                        act(s1[hc][:, 0:N], banks[bh1][:, 0:N], AF.Silu, [bankB[bh1]], [s1B[hc]])
                        tt("dve", tm[hc][:, 0:N], s1[hc][:, 0:N], banks[bh3][:, 0:N], ALU.mult, [s1B[hc], bankB[bh3]], [tmB[hc]])
                        tt("dve", hdn[:, ee, hc, 0:N], tm[hc][:, 0:N], banks[bcb][:, 0:N], ALU.mult,
                           [tmB[hc], bankB[bcb]], [hdnB[ee][hc]])
                for t in range(NT):
                    tok = slice(t * 128, (t + 1) * 128)
                    for nh in range(2):
                        by = nbank()
                        i = 0
                        for ee in range(2):
                            w2v = wslots[ee][0][:, 0:2048].rearrange("p (k n) -> p k n", k=2)
                            for hc in range(2):
                                mm(banks[by][:, :], hdn[:, ee, hc, tok], w2v[:, hc, nh * 512:(nh + 1) * 512], i == 0, i == 3,
                                   [hdnB[ee][hc], wslots[ee][1]], [bankB[by]])
                                i += 1
                        tt("dve", Yacc[:, t, nh * 512:(nh + 1) * 512], Yacc[:, t, nh * 512:(nh + 1) * 512], banks[by][:, :],
                           ALU.add, [YaccB[t], bankB[by]], [YaccB[t]])

            pg0, pg0B = wget("PG0")
            pg1, pg1B = wget("PG1")
            pe_, peB = wget("PE")
            pgv = [pg0[:, :].rearrange("p (k n) -> p k n", k=4), pg1[:, :].rearrange("p (k n) -> p k n", k=4)]
            pev = pe_[:, 0:2048].rearrange("p (k n) -> p k n", k=2)
            for t in range(NT):
                tok = slice(t * 128, (t + 1) * 128)
                for nh in range(2):
                    bg, bp = nbank(), nbank()
                    for kc in range(8):
                        mm(banks[bg][:, :], x1T[:, kc, tok], pgv[kc // 4][:, kc % 4, nh * 512:(nh + 1) * 512], kc == 0, kc == 7,
                           [pg0B, pg1B, x1TB[t]], [bankB[bg]])
                    for kc in range(2):
                        mm(banks[bp][:, :], pT[:, kc, tok], pev[:, kc, nh * 512:(nh + 1) * 512], kc == 0, kc == 1,
                           [peB, pTB[t]], [bankB[bp]])
                    act(sga[:, :], banks[bg][:, :], AF.Sigmoid, [bankB[bg]], [sgaB])
                    tt("dve", sga[:, :], sga[:, :], banks[bp][:, :], ALU.mult, [sgaB, bankB[bp]], [sgaB])
                    tt("dve", Yacc[:, t, nh * 512:(nh + 1) * 512], Yacc[:, t, nh * 512:(nh + 1) * 512], sga[:, :], ALU.add,
                       [YaccB[t], sgaB], [YaccB[t]])

            def ln2_tile(t):
                layer_norm(Yacc[:, t, :], YaccB[t], 2, Yacc[:, t, :], YaccB[t])
                if kind == "main":
                    dst = y_p.ap()[bi * 512 + t * 128:bi * 512 + (t + 1) * 128, :]
                else:
                    dst = y_s.ap()
                dma("pool", dst, Yacc[:, t, :], "yo%d" % t, [YaccB[t]], [])

            items = list(next_items)
            tl = list(range(NT))
            while items or tl:
                for _ in range(2):
                    if items:
                        items.pop(0)()
                if tl:
                    ln2_tile(tl.pop(0))

        for it in stageB_items("halo", -1):
            it()
        for it in stageB_items("main", 0):
            it()
        for bi in range(NBLK):
            nxt = stageB_items("main", bi + 1) if bi + 1 < NBLK else stageB_items("sample", 0)
            do_rest("main", bi, nxt)
            if bi == 1:
                dma("sp", ak_s.ap()[:, 0:480, :], cak.ap()[:, 32:512, :], "cc", [], [])
                dma("sp", av_s.ap()[:, 0:480, :], cav.ap()[:, 32:512, :], "cc", [], [])
                dma("sp", bk_s.ap()[:, 0:96, :], cbk.ap()[:, 32:128, :], "cc", [], [])
                dma("sp", bv_s.ap()[:, 0:96, :], cbv.ap()[:, 32:128, :], "cc", [], [])
        do_rest("sample", 0, [])
        fin = [(k, S.cnt[k]) for k in ["cc", "stg", "yo0", "yo1", "yo2", "yo3"]]
        S.wait_all("pool", fin)
        S.emit()
    return nc


def _consts():
    ar = np.arange
    m = ar(768)
    idxA = np.clip(639 - m, -128, 128) + 128
    ohA = np.zeros((384, 768), np.float32)
    ohA[idxA[:767], m[:767]] = 8.0
    mb = ar(384)
    idxB = _t5_bucket(255 - mb)
    ohB = np.zeros((32, 384), np.float32)
    ohB[idxB[:383], mb[:383]] = 8.0
    qp = 127 - ar(128)[:, None]
    j = ar(640)[None, :]
    validA = np.where(qp < 64, j < 576, j >= 64)
    maskA = np.where(validA, 0.0, MASKV).astype(np.float32)
    maskAs = np.broadcast_to(np.where(ar(128)[None, :] < 32, 0.0, MASKV), (128, 128)).astype(np.float32)
    jb = ar(256)[None, :]
    validB = np.where(qp < 64, jb < 192, jb >= 64)
    maskB = np.where(validB, 0.0, MASKV).astype(np.float32)
    maskBs = maskAs.copy()
    return dict(ohA=np.ascontiguousarray(ohA.reshape(3, 128, 768).transpose(1, 0, 2).reshape(128, 3 * 768)),
                ohB=ohB, maskA=maskA, maskAs=np.ascontiguousarray(maskAs), maskB=maskB, maskBs=maskBs)


def kernel(x_prompt, x_sample, p_prompt, p_sample, cache_a_k, cache_a_v, cache_b_k, cache_b_v, w_in,
           a_rel_table, b_sinks, w_pa, w_pb, w_o, ln1_g, ln1_b, w_rg, b_rg, w_re, b_re, w1, w3, w2, w_pe,
           w_pg, ln2_g, ln2_b, t5_table):
    f = lambda a: np.ascontiguousarray(np.asarray(a, dtype=np.float32))
    x_prompt, x_sample, p_prompt, p_sample = f(x_prompt), f(x_sample), f(p_prompt), f(p_sample)
    cache_a_k, cache_a_v, cache_b_k, cache_b_v = f(cache_a_k), f(cache_a_v), f(cache_b_k), f(cache_b_v)
    W = _pack_weights(f(w_in)[0], f(w_pa)[0], f(w_pb)[0], f(w_o)[0], f(w1)[0], f(w3)[0], f(w2)[0], f(w_pe)[0], f(w_pg)[0])
    cst = _consts()
    wr = np.concatenate([f(w_rg)[0], f(w_re)[0]], axis=1)
    wr = np.ascontiguousarray(wr.reshape(8, 128, 36).transpose(1, 0, 2).reshape(128, 8 * 36))
    rb = np.concatenate([f(b_rg)[0], f(b_re)[0]])[None, :]
    lnp = np.concatenate([f(ln1_g)[0], f(ln1_b)[0], f(ln2_g)[0], f(ln2_b)[0]])[None, :]
    tabA = np.zeros((384, 8), np.float32)
    tabA[:257] = f(a_rel_table)[0]
    tabA = np.ascontiguousarray(tabA.reshape(3, 128, 8).transpose(1, 0, 2).reshape(128, 24))
    shared = dict(w32=W, wr=wr, rb=np.ascontiguousarray(rb), lnp=np.ascontiguousarray(lnp), sinks=f(b_sinks),
                  tabA=tabA, ohA=cst["ohA"], tabB=f(t5_table), ohB=cst["ohB"], maskA=cst["maskA"],
                  maskAs=cst["maskAs"], maskB=cst["maskB"], maskBs=cst["maskBs"])
    in_maps = []
    for c in range(NCORES):
        b, sgm = c // 4, c % 4
        t0 = sgm * SEG
        xin = np.zeros((HALO + SEG + NS, D), np.float32)
        if sgm > 0:
            xin[0:HALO] = x_prompt[b, t0 - HALO:t0]
        xin[HALO:HALO + SEG] = x_prompt[b, t0:t0 + SEG]
        xin[HALO + SEG:] = x_sample[4 * c:4 * c + 4].reshape(NS, D)
        pin = np.concatenate([p_prompt[0, b, t0:t0 + SEG], p_sample[0, 4 * c:4 * c + 4].reshape(NS, 256)], 0)
        cbk_ = cache_b_k[0, 4 * c:4 * c + 4].reshape(4, 128, 2, 64)
        cbkd = np.concatenate([cbk_[:, :, 0], cbk_[:, :, 0], cbk_[:, :, 1], cbk_[:, :, 1]], axis=-1)
        hm = np.full((1, 128), MASKV if sgm == 0 else 0.0, np.float32)
        m = dict(shared)
        m.update(xin=xin, pin=np.ascontiguousarray(pin),
                 cak=np.ascontiguousarray(cache_a_k[0, 4 * c:4 * c + 4].reshape(4, 512, 512)),
                 cav=np.ascontiguousarray(cache_a_v[0, 4 * c:4 * c + 4].reshape(4, 512, 512)),
                 cbkd=np.ascontiguousarray(cbkd),
                 cbk=np.ascontiguousarray(cache_b_k[0, 4 * c:4 * c + 4].reshape(4, 128, 128)),
                 cbv=np.ascontiguousarray(cache_b_v[0, 4 * c:4 * c + 4].reshape(4, 128, 128)), hm=hm)
        in_maps.append(m)
    nc = build_program()
    res = run_bass_kernel_spmd(nc, in_maps, core_ids=list(range(NCORES)))
    R = res.results
    y_prompt = np.stack([np.concatenate([R[4 * b + s]["y_p"] for s in range(4)], 0) for b in range(2)], 0)
    y_sample = np.concatenate([R[c]["y_s"].reshape(4, 32, D) for c in range(NCORES)], 0)
    pak = np.stack([R[4 * b + 3]["ak_p"].reshape(512, 8, 64) for b in range(2)], 0)[None]
    pav = np.stack([R[4 * b + 3]["av_p"].reshape(512, 8, 64) for b in range(2)], 0)[None]
    pbk = np.stack([R[4 * b + 3]["bk_p"].reshape(128, 2, 64) for b in range(2)], 0)[None]
    pbv = np.stack([R[4 * b + 3]["bv_p"].reshape(128, 2, 64) for b in range(2)], 0)[None]
    sak = np.concatenate([R[c]["ak_s"].reshape(4, 512, 8, 64) for c in range(NCORES)], 0)[None]
    sav = np.concatenate([R[c]["av_s"].reshape(4, 512, 8, 64) for c in range(NCORES)], 0)[None]
    sbk = np.concatenate([R[c]["bk_s"].reshape(4, 128, 2, 64) for c in range(NCORES)], 0)[None]
    sbv = np.concatenate([R[c]["bv_s"].reshape(4, 128, 2, 64) for c in range(NCORES)], 0)[None]
    out = (y_prompt, y_sample, pak, pav, pbk, pbv, sak, sav, sbk, sbv)
    return tuple(np.ascontiguousarray(o, dtype=np.float32) for o in out)
```

```python
import math
from contextlib import ExitStack

import numpy as np

import concourse.bass as bass
import concourse.mybir as mybir
from concourse.bass_utils import run_bass_kernel_spmd

F32 = mybir.dt.float32
BF16 = mybir.dt.bfloat16
AF = mybir.ActivationFunctionType
ALU = mybir.AluOpType

NCORES = 8
D = 1024
SEG = 4096
HALO = 512
NS = 128
NBLK = 8
ALPHA = 2.0 ** 0.25
MASKV = -240000.0
NSLOT = 5
SLOT = 4096
SAME_ENGINE_SYNC = True
SAME_ENGINE_WAR = True

CH_ORDER = (["K1", "K2", "Q1", "Q2", "TVA", "TVB", "TKA", "TKB"] + ["D%d" % j for j in range(8)]
            + ["WO0", "WO1"])
for _e in range(32):
    CH_ORDER += ["EA%d" % _e, "EB%d" % _e]
CH_ORDER += ["PG0", "PG1", "PE"]
CH_SIZE = {"K1": 4096, "K2": 2048, "Q1": 4096, "Q2": 4096, "TVA": 4096, "TVB": 1024, "TKA": 4096,
           "TKB": 1024, "WO0": 4096, "WO1": 4096, "PG0": 4096, "PG1": 4096, "PE": 2048}
for _j in range(8):
    CH_SIZE["D%d" % _j] = 3072
for _e in range(32):
    CH_SIZE["EA%d" % _e] = 4096
    CH_SIZE["EB%d" % _e] = 2048
CH_OFF = {}
_o = 0
for _n in CH_ORDER:
    CH_OFF[_n] = _o
    _o += CH_SIZE[_n]
FTOT = _o
PIECES = ([("K1", "TKB"), ("D0", "D7"), ("WO0", "WO1")]
          + [("EA%d" % (2 * _i), "EB%d" % (2 * _i + 1)) for _i in range(16)] + [("PG0", "PE")])
CH_PIECE = {}
for _pi, (_a, _b) in enumerate(PIECES):
    for _n in CH_ORDER[CH_ORDER.index(_a):CH_ORDER.index(_b) + 1]:
        CH_PIECE[_n] = _pi


def _t5_bucket(rel):
    m = -rel
    base = np.where(m > 0, 16, 0)
    n = np.abs(m)
    nf = np.maximum(n, 1).astype(np.float32)
    v = np.log(nf / np.float32(8)) / np.float32(math.log(16.0)) * np.float32(8)
    large = 8 + v.astype(np.int32)
    large = np.minimum(large, 15)
    return base + np.where(n < 8, n, large)


def _pack_weights(w_in, w_pa, w_pb, w_o, w1, w3, w2, w_pe, w_pg):
    W = np.empty((128, FTOT), np.float32)

    def put(name, arr, off=0):
        a = arr.reshape(128, -1)
        W[:, CH_OFF[name] + off:CH_OFF[name] + off + a.shape[1]] = a

    def fm(cols):
        return w_in[:, cols].reshape(8, 128, len(cols)).transpose(1, 0, 2)

    def kmaj(w, nk):
        return w.reshape(nk, 128, w.shape[1]).transpose(1, 0, 2)

    ar = np.arange
    put("K1", np.stack([fm(512 + g * 128 + ar(128)) for g in range(4)], 1))
    put("K2", np.stack([fm(np.concatenate([2048 + kv * 64 + ar(64)] * 2)) for kv in range(2)], 1))
    put("Q1", np.stack([fm(g * 128 + ar(128)) for g in range(4)], 1))
    qb = []
    for g in range(4):
        kv, a = g // 2, g % 2
        qb.append(fm(np.concatenate([1536 + (4 * kv + a) * 64 + ar(64), 1536 + (4 * kv + a + 2) * 64 + ar(64)])))
    put("Q2", np.stack(qb, 1))
    put("TVA", fm(1024 + ar(512)))
    put("TVB", fm(2176 + ar(128)))
    put("TKA", fm(512 + ar(512)))
    put("TKB", fm(2048 + ar(128)))
    pa = kmaj(w_pa, 4)
    pb = kmaj(w_pb, 4)
    for j in range(8):
        n = "D%d" % j
        put(n, fm(2304 + j * 128 + ar(128)), 0)
        put(n, fm(3328 + j * 128 + ar(128)), 1024)
        put(n, pa[:, :, j * 128:(j + 1) * 128], 2048)
        put(n, pb[:, :, j * 128:(j + 1) * 128], 2560)
    wo = kmaj(w_o, 8)
    put("WO0", wo[:, 0:4])
    put("WO1", wo[:, 4:8])
    for e in range(32):
        put("EA%d" % e, kmaj(w1[e], 8), 0)
        put("EA%d" % e, kmaj(w3[e], 8), 2048)
        put("EB%d" % e, kmaj(w2[e], 2))
    pg = kmaj(w_pg, 8)
    put("PG0", pg[:, 0:4])
    put("PG1", pg[:, 4:8])
    put("PE", kmaj(w_pe, 2))
    return W


class Buf:
    __slots__ = ("name", "w", "r", "excl")

    def __init__(self, name="", excl=False):
        self.name = name
        self.w = None
        self.r = []
        self.excl = excl


class Sched:
    ENGS = ("pe", "act", "dve", "pool", "sp")

    def __init__(self, nc, ctx):
        self.nc = nc
        self.ctx = ctx
        self.ops = {e: [] for e in self.ENGS}
        self.sems = {}
        self.cnt = {}
        self.known = {e: {} for e in self.ENGS}
        for e in self.ENGS:
            self.newsem("E_" + e)

    def newsem(self, key):
        self.sems[key] = self.ctx.enter_context(self.nc.semaphore(key))
        self.cnt[key] = 0
        return key

    def _waits(self, eng, reads, writes):
        need = {}

        def add(tok, raw):
            if tok is None:
                return
            k, v = tok
            if k == "E_" + eng and (eng == "pe" or not SAME_ENGINE_SYNC or not (raw or SAME_ENGINE_WAR)):
                return
            if need.get(k, 0) < v:
                need[k] = v

        for b in reads:
            add(b.w, True)
        for b in writes:
            add(b.w, b.excl)
            for t in b.r:
                add(t, False)
        out = []
        kn = self.known[eng]
        for k, v in need.items():
            if kn.get(k, 0) < v:
                kn[k] = v
                out.append((k, v))
        return out

    def _mark(self, tok, reads, writes):
        for b in reads:
            b.r.append(tok)
        for b in writes:
            b.w = tok
            b.r = []

    def op(self, eng, fn, reads=(), writes=(), inc=True):
        ex = [b for b in reads if b.excl]
        if ex:
            reads = [b for b in reads if not b.excl]
            writes = list(writes) + ex
        waits = self._waits(eng, reads, writes)
        key = "E_" + eng
        if inc:
            self.cnt[key] += 1
            tok = (key, self.cnt[key])
            self.ops[eng].append((waits, fn, (key, 1)))
        else:
            tok = (key, self.cnt[key] + 1)
            self.ops[eng].append((waits, fn, None))
        self._mark(tok, reads, writes)
        return tok

    def dma(self, eng, fn, semkey, reads=(), writes=()):
        waits = self._waits(eng, reads, writes)
        self.cnt[semkey] += 16
        tok = (semkey, self.cnt[semkey])
        self.ops[eng].append((waits, fn, (semkey, 16)))
        self._mark(tok, reads, writes)
        return tok

    def wait_all(self, eng, toks):
        waits = []
        kn = self.known[eng]
        for k, v in toks:
            if kn.get(k, 0) < v:
                kn[k] = v
                waits.append((k, v))
        self.ops[eng].append((waits, None, None))

    def emit(self):
        sems = self.sems
        with self.nc.Block() as block:
            def run(engname):
                def body(e):
                    for waits, fn, inc in self.ops[engname]:
                        for k, v in waits:
                            e.wait_ge(sems[k], v)
                        if fn is not None:
                            ins = fn(e)
                            if inc is not None:
                                ins.then_inc(sems[inc[0]], inc[1])
                return body
            block.tensor(run("pe"))
            block.scalar(run("act"))
            block.vector(run("dve"))
            block.gpsimd(run("pool"))
            block.sync(run("sp"))


def build_program():
    nc = bass.Bass("TRN2", target_bir_lowering=False)
    din = lambda n, s: nc.dram_tensor(n, s, F32, kind="ExternalInput")
    dout = lambda n, s: nc.dram_tensor(n, s, F32, kind="ExternalOutput")
    xin = din("xin", [HALO + SEG + NS, D])
    pin = din("pin", [SEG + NS, 256])
    cak = din("cak", [4, 512, 512])
    cav = din("cav", [4, 512, 512])
    cbkd = din("cbkd", [4, 128, 256])
    cbk = din("cbk", [4, 128, 128])
    cbv = din("cbv", [4, 128, 128])
    w32 = din("w32", [128, FTOT])
    wr_d = din("wr", [128, 8 * 36])
    rb_d = din("rb", [1, 36])
    lnp_d = din("lnp", [1, 4 * D])
    sinks_d = din("sinks", [1, 8])
    tabA_d = din("tabA", [128, 3 * 8])
    ohA_d = din("ohA", [128, 3 * 768])
    tabB_d = din("tabB", [32, 8])
    ohB_d = din("ohB", [32, 384])
    mA_d = din("maskA", [128, 640])
    mAs_d = din("maskAs", [128, 128])
    mB_d = din("maskB", [128, 256])
    mBs_d = din("maskBs", [128, 128])
    hm_d = din("hm", [1, 128])

    y_p = dout("y_p", [SEG, D])
    y_s = dout("y_s", [NS, D])
    ak_p = dout("ak_p", [512, 512])
    av_p = dout("av_p", [512, 512])
    bk_p = dout("bk_p", [128, 128])
    bv_p = dout("bv_p", [128, 128])
    ak_s = dout("ak_s", [4, 512, 512])
    av_s = dout("av_s", [4, 512, 512])
    bk_s = dout("bk_s", [4, 128, 128])
    bv_s = dout("bv_s", [4, 128, 128])

    wbf = nc.dram_tensor("wbf", [128, FTOT], BF16, kind="Internal")
    GA_d = nc.dram_tensor("GA", [8, 768], F32, kind="Internal")
    GB_d = nc.dram_tensor("GB", [8, 384], F32, kind="Internal")

    with ExitStack() as ctx:
        S = Sched(nc, ctx)

        def sb(name, shape, dt):
            return ctx.enter_context(nc.sbuf_tensor(name, shape, dt))

        slots = [sb("slot%d" % i, [128, SLOT], BF16) for i in range(NSLOT)]
        slotB = [Buf("slot%d" % i) for i in range(NSLOT)]
        xf = [sb("xf%d" % i, [128, D], F32) for i in range(2)]
        xfB = [Buf("xf0"), Buf("xf1")]
        xb = sb("xb", [128, D], BF16)
        xbB = Buf("xb")
        xT = sb("xT", [128, 8, 512], BF16)
        xTB = [Buf("xT%d" % i) for i in range(4)]
        qAT = sb("qAT", [128, 4, 512], BF16)
        qATB = [Buf("qAT%d" % i) for i in range(4)]
        qBT = sb("qBT", [128, 4, 512], BF16)
        qBTB = [Buf("qBT%d" % i) for i in range(4)]
        kAT = sb("kAT", [128, 4, 1024], BF16)
        kATB = [Buf("kAT%d" % i) for i in range(8)]
        kBT = sb("kBT", [128, 2, 1024], BF16)
        kBTB = [Buf("kBT%d" % i) for i in range(8)]
        VA = sb("VA", [128, 8, 8, 65], BF16)
        VAB = [Buf("VA%d" % i) for i in range(8)]
        VB = sb("VB", [128, 8, 2, 65], BF16)
        VBB = [Buf("VB%d" % i) for i in range(8)]
        PTA = sb("PTA", [128, 2, 5, 512], BF16)
        PTAB = [[Buf("PTA%d%d" % (a, b)) for b in range(5)] for a in range(2)]
        PTB = sb("PTB", [128, 2, 2, 512], BF16)
        PTBB = [[Buf("PTB%d%d" % (a, b)) for b in range(2)] for a in range(2)]
        EBA = sb("EBA", [128, 5, 8, 128], BF16)
        EBAs = sb("EBAs", [128, 8, 128], BF16)
        EBB = sb("EBB", [128, 2, 8, 128], BF16)
        EBBs = sb("EBBs", [128, 8, 128], BF16)
        constB = Buf("const")
        yab = sb("yab", [128, 2, 2, 512], BF16)
        yabB = [[Buf("ya0"), Buf("yb0")], [Buf("ya1"), Buf("yb1")]]
        ypar = [0]
        rsA = sb("rsA", [128, 4], F32)
        rsAB = Buf("rsA")
        yaT = sb("yaT", [128, 4, 512], BF16)
        yaTB = [Buf("yaT%d" % i) for i in range(4)]
        ybT = sb("ybT", [128, 4, 512], BF16)
        ybTB = [Buf("ybT%d" % i) for i in range(4)]
        sga = sb("sga", [128, 512], F32)
        sgaB = Buf("sga")
        sgb = sb("sgb", [128, 512], F32)
        sgbB = Buf("sgb")
        mgT = sb("mgT", [128, 8, 512], BF16)
        mgTB = [Buf("mgT%d" % i) for i in range(8)]
        ckb = mgT[:, 0:4, :]
        ckbb = mgT[:, 4, 0:256]
        xt1 = sb("xt1", [128, D], F32)
        xt1B = Buf("xt1")
        Yacc = sb("Yacc", [128, 4, D], F32)
        YaccB = [Buf("Yacc%d" % i) for i in range(4)]
        x1T = sb("x1T", [128, 8, 512], BF16)
        x1TB = [Buf("x1T%d" % i) for i in range(4)]

        lnst = sb("lnst", [128, 2, 6], F32)
        lnstB = Buf("lnst")
        lnmv = sb("lnmv", [128, 2], F32)
        lnmvB = Buf("lnmv")
        lnr = sb("lnr", [128, 1], F32)
        lnrB = Buf("lnr")
        rt = sb("rt", [128, 768], F32)
        rtB = Buf("rt")
        combT = sb("combT", [32, 512], BF16)
        combTB = [Buf("combT%d" % i) for i in range(4)]
        mt = sb("mt", [128, 4, 512], F32)
        s1 = [mt[:, 0, :], mt[:, 1, :]]
        s1B = [Buf("s1_0"), Buf("s1_1")]
        tm = [mt[:, 2, :], mt[:, 3, :]]
        tmB = [Buf("tm_0"), Buf("tm_1")]
        stgBL = [s1B[0], s1B[1], tmB[0]]
        x1Tf = mt[:, 2:4, :].rearrange("p a (b c) -> p (a b) c", c=128)
        hdn = sb("hdn", [128, 2, 2, 512], BF16)
        hdnB = [[Buf("hdn%d%d" % (a, b)) for b in range(2)] for a in range(2)]
        pf = sb("pf", [128, 256], F32)
        pfB = Buf("pf")
        pbt = sb("pbt", [128, 256], BF16)
        pbtB = Buf("pbt")
        pT = sb("pT", [128, 2, 512], BF16)
        pTB = [Buf("pT%d" % i) for i in range(4)]
        stg = mt[:, :, :].rearrange("p a b -> p (a b)")[:, 0:1280]
        lnpb = sb("lnpb", [128, 2, D], F32)
        lnpB = Buf("lnp")
        cm = sb("cm", [32, 2, 512], BF16)
        cmB = [Buf("cm0"), Buf("cm1")]
        ones32 = sb("ones32", [32, 128], BF16)
        wr = sb("wr_sb", [128, 8, 36], F32)
        rb = sb("rb_sb", [128, 36], F32)
        es = sb("es_sb", [128, 8], F32)
        identf = sb("identf", [128, 128], F32)
        identb = sb("identb", [128, 128], BF16)
        Jb = sb("Jb", [128, 128], BF16)
        ones_r = sb("ones_r", [1, 128], BF16)
        hmx = sb("hmx", [128, 1], F32)

        banks = [ctx.enter_context(nc.psum_tensor("bank%d" % i, [128, 512], F32)) for i in range(8)]
        bankB = [Buf("bank%d" % i, excl=True) for i in range(8)]
        bank_rr = [0]

        def nbank():
            i = bank_rr[0] % 8
            bank_rr[0] += 1
            return i

        def bbf(i):
            return banks[i][:, :].bitcast(BF16)

        def mm(out, lhsT, rhs, start, stop, reads, writes, inc=None):
            if inc is None:
                inc = stop
            S.op("pe", lambda e: e.matmul(out, lhsT=lhsT, rhs=rhs, start=start, stop=stop),
                 reads=reads, writes=writes, inc=inc)

        def tr(out, in_, ident, reads, writes, inc=True):
            S.op("pe", lambda e: e.transpose(out=out, in_=in_, identity=ident), reads=reads, writes=writes, inc=inc)

        def act(out, in_, func, reads, writes, scale=1.0, bias=None):
            if bias is None:
                S.op("act", lambda e: e.activation(out=out, in_=in_, func=func, scale=scale), reads=reads, writes=writes)
            else:
                S.op("act", lambda e: e.activation(out=out, in_=in_, func=func, scale=scale, bias=bias), reads=reads, writes=writes)

        def tt(eng, out, in0, in1, op, reads, writes):
            S.op(eng, lambda e: e.tensor_tensor(out=out, in0=in0, in1=in1, op=op), reads=reads, writes=writes)

        def tcopy(eng, out, in_, reads, writes):
            S.op(eng, lambda e: e.tensor_copy(out=out, in_=in_), reads=reads, writes=writes)

        def ts(eng, out, in0, s1_, s2_, op0, op1, reads, writes):
            if s2_ is None:
                S.op(eng, lambda e: e.tensor_scalar(out=out, in0=in0, scalar1=s1_, scalar2=None, op0=op0), reads=reads, writes=writes)
            else:
                S.op(eng, lambda e: e.tensor_scalar(out=out, in0=in0, scalar1=s1_, scalar2=s2_, op0=op0, op1=op1), reads=reads, writes=writes)

        def memset(eng, ap, val, writes):
            S.op(eng, lambda e: e.memset(ap, val), writes=writes)

        def dma(eng, out, in_, sem, reads, writes):
            S.dma(eng, lambda e: e.dma_start(out=out, in_=in_), sem, reads=reads, writes=writes)

        ring_i = [0]
        for i in range(NSLOT):
            S.newsem("ring%d" % i)

        def wget(name):
            i = ring_i[0] % NSLOT
            ring_i[0] += 1
            off, size = CH_OFF[name], CH_SIZE[name]
            dma("sp", slots[i][:, 0:size], wbf.ap()[:, off:off + size], "ring%d" % i, [ppB[CH_PIECE[name]]], [slotB[i]])
            return slots[i], slotB[i]

        S.newsem("setup")
        setup_bufs = []

        def sload(out, in_):
            dma("sp", out, in_, "setup", [], [constB])

        sf = [s[:, :].bitcast(F32) for s in slots]
        ohA0 = sf[0][:, 0:1536].rearrange("p (c n) -> p c n", c=3)
        ohA1 = sf[1][:, 0:768].rearrange("p (c n) -> p c n", c=3)
        tabA = sf[1][:, 768:792].rearrange("p (c n) -> p c n", c=3)
        tabB = sf[1][0:32, 800:808]
        ohB = sf[1][0:32, 1024:1408]
        mA = sf[2][:, 0:640]
        mAs = sf[2][:, 640:768]
        mB = sf[2][:, 768:1024]
        mBs = sf[2][:, 1024:1152]
        toe = sf[3][:, 0:640]
        toeb = sf[3][:, 1024:1280]
        gsb = sf[4][0:8, 256:1024]
        gsbB = slotB[4]
        ohA_v = ohA_d.ap().rearrange("p (c n) -> p c n", c=3)
        S.newsem("su0")
        S.newsem("su1")
        S.newsem("su2")
        dma("sp", ohA0, ohA_v[:, :, 0:512], "su0", [], [slotB[0]])
        dma("sp", ohA1, ohA_v[:, :, 512:768], "su1", [], [slotB[1]])
        dma("sp", tabA, tabA_d.ap().rearrange("p (c n) -> p c n", c=3), "su1", [], [slotB[1]])
        dma("sp", tabB, tabB_d.ap(), "su1", [], [slotB[1]])
        dma("sp", ohB, ohB_d.ap(), "su1", [], [slotB[1]])
        dma("sp", mA, mA_d.ap(), "su2", [], [slotB[2]])
        dma("sp", mAs, mAs_d.ap(), "su2", [], [slotB[2]])
        dma("sp", mB, mB_d.ap(), "su2", [], [slotB[2]])
        dma("sp", mBs, mBs_d.ap(), "su2", [], [slotB[2]])
        sload(wr[:, :, :], wr_d.ap().rearrange("p (k n) -> p k n", k=8))
        sload(rb[:, :], rb_d.ap().partition_broadcast(128))
        sload(es[:, :], sinks_d.ap().partition_broadcast(128))
        sload(hmx[:, :], hm_d.ap()[:, 0:1].partition_broadcast(128))
        constB.w = ("setup", S.cnt["setup"])

        memset("dve", identf[:, :], 0.0, [constB])
        S.op("pool", lambda e: e.affine_select(out=identf[:, :], in_=identf[:, :], pattern=[[-1, 128]],
                                               compare_op=ALU.not_equal, fill=1.0, base=0, channel_multiplier=1),
             reads=[constB], writes=[constB])
        tcopy("dve", identb[:, :], identf[:, :], [constB], [constB])
        jf = sf[4][:, 0:128]
        memset("dve", jf, 0.0, [slotB[4]])
        S.op("pool", lambda e: e.affine_select(out=jf, in_=jf, pattern=[[1, 128]], compare_op=ALU.not_equal,
                                               fill=1.0, base=-127, channel_multiplier=1),
             reads=[slotB[4]], writes=[slotB[4]])
        tcopy("dve", Jb[:, :], jf, [slotB[4]], [constB])

        memset("dve", ones32[:, :], 1.0, [constB])
        memset("dve", ones_r[:, :], 1.0, [constB])
        act(hmx[:, :], hmx[:, :], AF.Exp, [constB], [constB], scale=0.125)
        act(es[:, :], es[:, :], AF.Exp, [constB], [constB])
        memset("dve", VA[:, :, :, :], 1.0, VAB)
        memset("dve", xT[:, :, :], 0.0, xTB)
        memset("dve", qAT[:, :, :], 0.0, qATB)
        memset("dve", qBT[:, :, :], 0.0, qBTB)
        memset("dve", VB[:, :, :, :], 1.0, VBB)

        b0, b1 = nbank(), nbank()
        for c in range(3):
            mm(banks[b0][0:8, 0:512], tabA[:, c, :], ohA0[:, c, :], c == 0, c == 2, [slotB[0], slotB[1]], [bankB[b0]])
        for c in range(3):
            mm(banks[b1][0:8, 0:256], tabA[:, c, :], ohA1[:, c, :], c == 0, c == 2, [slotB[1]], [bankB[b1]])
        act(gsb[:, 0:512], banks[b0][0:8, 0:512], AF.Copy, [bankB[b0]], [gsbB])
        act(gsb[:, 512:768], banks[b1][0:8, 0:256], AF.Copy, [bankB[b1]], [gsbB])
        S.newsem("gA")
        GAB = Buf("GA")
        dma("sp", GA_d.ap(), gsb[:, :], "gA", [gsbB], [GAB])
        b2 = nbank()
        mm(banks[b2][0:8, 0:384], tabB, ohB, True, True, [slotB[1]], [bankB[b2]])
        act(gsb[:, 0:384], banks[b2][0:8, 0:384], AF.Copy, [bankB[b2]], [gsbB])
        S.newsem("gB")
        GBB = Buf("GB")
        dma("sp", GB_d.ap(), gsb[:, 0:384], "gB", [gsbB], [GBB])
        S.newsem("toe")
        S.newsem("toeb")

        sb16 = [s_[:, :] for s_ in slots]
        BTA_h = [sb16[3][:, 0:2560].rearrange("p (h j) -> p h j", h=4), sb16[0][:, 0:2560].rearrange("p (h j) -> p h j", h=4)]
        BTA_B = [slotB[3], slotB[0]]
        BTB = sb16[1][:, 0:2048].rearrange("p (h j) -> p h j", h=8)
        BTAs = sb16[4][:, 2048:3072].rearrange("p (h j) -> p h j", h=8)
        BTBs = sb16[4][:, 3072:4096].rearrange("p (h j) -> p h j", h=8)

        def toeplitz_loads():
            for half in range(2):
                dma("pool", BTA_h[half], bass.AP(GA_d, half * 4 * 768, [[1, 128], [768, 4], [1, 640]]), "toe", [GAB],
                    [BTA_B[half]])
            dma("pool", BTB, bass.AP(GB_d, 0, [[1, 128], [384, 8], [1, 256]]), "toeb", [GBB], [slotB[1]])
            for half in range(2):
                tt("dve", BTAs[:, half * 4:(half + 1) * 4, :], BTA_h[half][:, :, 512:640],
                   mAs.unsqueeze(1).broadcast_to([128, 4, 128]), ALU.add, [BTA_B[half], slotB[2]], [slotB[4]])
                tt("dve", BTA_h[half], BTA_h[half], mA.unsqueeze(1).broadcast_to([128, 4, 640]), ALU.add,
                   [BTA_B[half], slotB[2]], [BTA_B[half]])
            tt("dve", BTBs, BTB[:, :, 128:256], mBs.unsqueeze(1).broadcast_to([128, 8, 128]), ALU.add,
               [slotB[1], slotB[2]], [slotB[4]])
            tt("dve", BTB, BTB, mB.unsqueeze(1).broadcast_to([128, 8, 256]), ALU.add, [slotB[1], slotB[2]], [slotB[1]])

            def eb_tile(dst, srcs, rds):
                bk = nbank()
                for hh, src in enumerate(srcs):
                    mm(banks[bk][:, hh * 128:(hh + 1) * 128], src, Jb[:, :], True, True, rds + [constB], [bankB[bk]],
                       inc=(hh == 3))
                act(dst, banks[bk][:, :], AF.Exp, [bankB[bk]], [constB], scale=0.125)

            bta = lambda h: BTA_h[h // 4][:, h % 4, :]
            for kt in range(5):
                for hg in range(2):
                    eb_tile(EBA[:, kt, 4 * hg:4 * hg + 4, :].rearrange("p h q -> p (h q)"),
                            [bta(2 * hh + hg)[:, kt * 128:(kt + 1) * 128] for hh in range(4)], BTA_B)
            for hg in range(2):
                eb_tile(EBAs[:, 4 * hg:4 * hg + 4, :].rearrange("p h q -> p (h q)"),
                        [BTAs[:, 2 * hh + hg, :] for hh in range(4)], [slotB[4]])
            hB = lambda half, blk: 4 * (blk // 2) + 2 * half + blk % 2
            for kt in range(2):
                for half in range(2):
                    eb_tile(EBB[:, kt, 4 * half:4 * half + 4, :].rearrange("p h q -> p (h q)"),
                            [BTB[:, hB(half, blk), kt * 128:(kt + 1) * 128] for blk in range(4)], [slotB[1]])
            for half in range(2):
                eb_tile(EBBs[:, 4 * half:4 * half + 4, :].rearrange("p h q -> p (h q)"),
                        [BTBs[:, hB(half, blk), :] for blk in range(4)], [slotB[4]])

        ppB = []
        for i, (a, b) in enumerate(PIECES):
            lo, hi = CH_OFF[a], CH_OFF[b] + CH_SIZE[b]
            S.newsem("pp%d" % i)
            bb = Buf("pp%d" % i)
            ppB.append(bb)
            prev = [ppB[i - 2]] if i >= 2 else []
            c = lo
            while c < hi:
                c2 = min(hi, c + 8192)
                dma("pool", wbf.ap()[:, c:c2], w32.ap()[:, c:c2], "pp%d" % i, prev, [bb])
                c = c2
            if i == 1:
                toeplitz_loads()

        S.newsem("xf0")
        S.newsem("xf1")
        S.newsem("pf")
        S.newsem("stg")
        S.newsem("cc")
        S.newsem("ckb")
        S.newsem("ckbb")
        S.newsem("ckvb")
        S.newsem("lnp")
        S.newsem("ckv")
        for i in range(4):
            S.newsem("yo%d" % i)
        out_toks = []
        xf_rr = [0]

        def load_x(row0):
            i = xf_rr[0] % 2
            xf_rr[0] += 1
            dma("sp", xf[i][:, :], xin.ap()[row0:row0 + 128, :], "xf%d" % i, [], [xfB[i]])
            return i

        def make_xT(row0, NT, tiles=None):
            for t in (range(NT) if tiles is None else tiles):
                i = load_x(row0 + t * 128)
                act(xb[:, :], xf[i][:, :], AF.Copy, [xfB[i]], [xbB])
                bk = nbank()
                for kc in range(8):
                    tr(bbf(bk)[:, kc * 128:(kc + 1) * 128], xb[:, kc * 128:(kc + 1) * 128], identb[:, :],
                       [xbB, constB], [bankB[bk]], inc=(kc == 7))
                act(xT[:, :, t * 128:(t + 1) * 128], bbf(bk).rearrange("p (k n) -> p k n", k=8), AF.Copy,
                    [bankB[bk]], [xTB[t]])

        def proj_fm(wname, ngrp, dst, dstB_of, col0, N, NT):
            sl, slB = wget(wname)
            wv = sl[:, 0:ngrp * 1024].rearrange("p (g k n) -> p g k n", g=ngrp, k=8)
            for g in range(ngrp):
                bk = nbank()
                for kc in range(8):
                    mm(banks[bk][:, 0:N], wv[:, g, kc, :], xT[:, kc, 0:N], kc == 0, kc == 7,
                       [slB] + xTB[:NT], [bankB[bk]])
                act(dst[:, g, col0:col0 + N], banks[bk][:, 0:N], AF.Copy, [bankB[bk]], dstB_of(g))

        def proj_v_make(pos_of, cacheout, after_tile):
            st = {}

            def tile(t):
                if not st:
                    st["a"] = wget("TVA")
                    st["b"] = wget("TVB")
                    if cacheout:
                        st["ka"] = wget("TKA")
                        st["kb"] = wget("TKB")
                slA, slAB = st["a"]
                slB_, slBB = st["b"]
                wa = slA[:, 0:4096].rearrange("p (k n) -> p k n", k=8)
                wb = slB_[:, 0:1024].rearrange("p (k n) -> p k n", k=8)
                pos = pos_of(t)
                ba, bb_ = nbank(), nbank()
                for kc in range(8):
                    mm(banks[ba][:, 0:512], xT[:, kc, t * 128:(t + 1) * 128], wa[:, kc, :], kc == 0, kc == 7,
                       [slAB, xTB[t]], [bankB[ba]])
                for kc in range(8):
                    mm(banks[bb_][:, 0:128], xT[:, kc, t * 128:(t + 1) * 128], wb[:, kc, :], kc == 0, kc == 7,
                       [slBB, xTB[t]], [bankB[bb_]])
                act(VA[:, pos, :, 0:64], banks[ba][:, 0:512].rearrange("p (h d) -> p h d", h=8), AF.Copy,
                    [bankB[ba]], [VAB[pos]])
                act(VB[:, pos, :, 0:64], banks[bb_][:, 0:128].rearrange("p (h d) -> p h d", h=2), AF.Copy,
                    [bankB[bb_]], [VBB[pos]])
                if cacheout:
                    slKA, slKAB = st["ka"]
                    slKB, slKBB = st["kb"]
                    wka = slKA[:, 0:4096].rearrange("p (k n) -> p k n", k=8)
                    wkb = slKB[:, 0:1024].rearrange("p (k n) -> p k n", k=8)
                    tcopy("dve", stg[:, 640:1152], banks[ba][:, 0:512], [bankB[ba]], stgBL)
                    tcopy("dve", stg[:, 1152:1280], banks[bb_][:, 0:128], [bankB[bb_]], stgBL)
                    bka, bkb = nbank(), nbank()
                    for kc in range(8):
                        mm(banks[bka][:, 0:512], xT[:, kc, t * 128:(t + 1) * 128], wka[:, kc, :], kc == 0, kc == 7,
                           [slKAB, xTB[t]], [bankB[bka]])
                    for kc in range(8):
                        mm(banks[bkb][:, 0:128], xT[:, kc, t * 128:(t + 1) * 128], wkb[:, kc, :], kc == 0, kc == 7,
                           [slKBB, xTB[t]], [bankB[bkb]])
                    tcopy("dve", stg[:, 0:512], banks[bka][:, 0:512], [bankB[bka]], stgBL)
                    tcopy("dve", stg[:, 512:640], banks[bkb][:, 0:128], [bankB[bkb]], stgBL)
                    after_tile(t)
            return tile

        def attn_pair(qa, ka, va, ta, qb, kb, vb, tb, haloA, haloB, rdA, rdB):
            def pmul(pt, ptB, eb, hl):
                if hl:
                    S.op("dve", lambda e: e.scalar_tensor_tensor(out=pt, in0=pt, scalar=hmx[:, 0:1], in1=eb,
                                                                 op0=ALU.mult, op1=ALU.mult),
                         reads=[ptB, constB], writes=[ptB])
                else:
                    tt("dve", pt, pt, eb, ALU.mult, [ptB, constB], [ptB])

            def scoresA(hg):
                for kt in range(5):
                    bk = nbank()
                    for hh in range(4):
                        h = 2 * hh + hg
                        o = banks[bk][:, hh * 128:(hh + 1) * 128]
                        mm(o, ka(h, kt), qa(h), True, True, rdA(kt), [bankB[bk]], inc=(hh == 3))
                    act(PTA[:, hg, kt, :], banks[bk][:, :], AF.Exp, [bankB[bk]], [PTAB[hg][kt]], scale=0.125)
                    pmul(PTA[:, hg, kt, :], PTAB[hg][kt], ta(hg, kt), kt in haloA)

            def scoresB():
                for half in range(2):
                    for kt in range(2):
                        bk = nbank()
                        for kv in range(2):
                            o2 = banks[bk][:, kv * 256:(kv + 1) * 256].rearrange("p (a b) -> p a b", a=2)
                            mm(o2, kb(kv, half, kt), qb(kv, half), True, True, rdB(kt), [bankB[bk]], inc=(kv == 1))
                        act(PTB[:, half, kt, :], banks[bk][:, :], AF.Exp, [bankB[bk]], [PTBB[half][kt]], scale=0.125)
                        pmul(PTB[:, half, kt, :], PTBB[half][kt], tb(half, kt), kt in haloB)

            def pvA(hg):
                bk = nbank()
                for hh in range(4):
                    h = 2 * hh + hg
                    for kt in range(5):
                        mm(banks[bk][:, hh * 65:(hh + 1) * 65], PTA[:, hg, kt, hh * 128:(hh + 1) * 128], va(kt, h),
                           kt == 0, kt == 4, [PTAB[hg][kt]] + rdA(kt), [bankB[bk]], inc=(kt == 4 and hh == 3))
                ov = banks[bk][:, 0:260].rearrange("p (h d) -> p h d", d=65)
                S.op("dve", lambda e, ov=ov: e.reciprocal(out=rsA[:, :], in_=ov[:, :, 64]), reads=[bankB[bk]], writes=[rsAB])
                tt("dve", ya.rearrange("p (h d) -> p h d", h=8)[:, hg:8:2, :], ov[:, :, 0:64],
                   rsA[:, :].unsqueeze(2).broadcast_to([128, 4, 64]), ALU.mult, [bankB[bk], rsAB], [yaB])

            def pvB():
                for kv in range(2):
                    bk = nbank()
                    for blk in range(4):
                        half, j = blk // 2, blk % 2
                        c0 = (kv * 2 + j) * 128
                        for kt in range(2):
                            mm(banks[bk][:, blk * 65:(blk + 1) * 65], PTB[:, half, kt, c0:c0 + 128], vb(kt, kv),
                               kt == 0, kt == 1, [PTBB[half][kt]] + rdB(kt), [bankB[bk]], inc=(kt == 1 and blk == 3))
                    ov = banks[bk][:, 0:260].rearrange("p (h d) -> p h d", d=65)
                    tt("dve", rsA[:, :], ov[:, :, 64], es[:, kv * 4:(kv + 1) * 4], ALU.add, [bankB[bk], constB], [rsAB])
                    S.op("dve", lambda e: e.reciprocal(out=rsA[:, :], in_=rsA[:, :]), reads=[rsAB], writes=[rsAB])
                    tt("dve", yb[:, kv * 256:(kv + 1) * 256].rearrange("p (h d) -> p h d", h=4), ov[:, :, 0:64],
                       rsA[:, :].unsqueeze(2).broadcast_to([128, 4, 64]), ALU.mult, [bankB[bk], rsAB], [ybB])

            par = ypar[0] % 2
            ypar[0] += 1
            ya, yb = yab[:, par, 0, :], yab[:, par, 1, :]
            yaB, ybB = yabB[par]
            scoresA(0)
            scoresA(1)
            scoresB()
            pvA(0)
            pvA(1)
            pvB()
            return ya, yaB, yb, ybB

        def y_transposes(yy, dstA, dstB_, dstAB, dstBB, ncols):
            ya, yaB, yb, ybB = yy
            for (src, srcB, dst, dB) in ((ya, yaB, dstA, dstAB), (yb, ybB, dstB_, dstBB)):
                bk = nbank()
                for c in range(4):
                    tr(bbf(bk)[:, c * 128:(c + 1) * 128], src[:, c * 128:(c + 1) * 128], identb[:, :],
                       [srcB, constB], [bankB[bk]], inc=(c == 3))
                act(dst, bbf(bk)[:, 0:512].rearrange("p (c t) -> p c t", c=4)[:, :, 0:ncols], AF.Copy,
                    [bankB[bk]], dB)

        def load_ln(which):
            dma("pool", lnpb[:, :, :],
                lnp_d.ap()[:, which * 2048:(which + 1) * 2048].partition_broadcast(128).rearrange("p o (a n) -> p (o a) n", a=2),
                "lnp", [], [lnpB])

        def layer_norm(xap, xB, gi, out_ap, outB):
            for h in range(2):
                S.op("dve", lambda e, h=h: e.bn_stats(out=lnst[:, h, :], in_=xap[:, h * 512:(h + 1) * 512]),
                     reads=[xB], writes=[lnstB])
            S.op("dve", lambda e: e.bn_aggr(out=lnmv[:, :], in_=lnst[:, :, :]), reads=[lnstB], writes=[lnmvB])
            ts("dve", lnr[:, :], lnmv[:, 1:2], 1e-5, None, ALU.add, None, [lnmvB], [lnrB])
            act(lnr[:, :], lnr[:, :], AF.Ln, [lnrB], [lnrB])
            act(lnr[:, :], lnr[:, :], AF.Exp, [lnrB], [lnrB], scale=-0.5)
            ts("dve", xap, xap, lnmv[:, 0:1], lnr[:, 0:1], ALU.subtract, ALU.mult, [xB, lnmvB, lnrB], [xB])
            tt("dve", xap, xap, lnpb[:, 0, :], ALU.mult, [xB, lnpB], [xB])
            tt("dve", out_ap, xap, lnpb[:, 1, :], ALU.add, [xB, lnpB], [outB] if outB is not xB else [xB])

        def blk(kind, bi):
            sample = kind == "sample"
            NT = 1 if sample else 4
            if kind == "halo":
                row0 = 0
            elif kind == "main":
                row0 = HALO + bi * 512
            else:
                row0 = HALO + SEG
            gbase = 0 if kind == "halo" else (4 + 4 * bi if kind == "main" else 7)
            pos_of = lambda t: (gbase + t) % 8
            kcol0 = 896 if sample else (pos_of(0) * 128)
            cacheout = sample or (kind == "main" and bi == NBLK - 1)
            return sample, NT, NT * 128, row0, gbase, pos_of, kcol0, cacheout

        def stageB_items(kind, bi):
            sample, NT, N, row0, gbase, pos_of, kcol0, cacheout = blk(kind, bi)
            items = []
            for t in range(NT):
                items.append(lambda t=t: make_xT(row0, NT, [t]))
            items.append(lambda: proj_fm("K1", 4, kAT, lambda g: [kATB[pos_of(t)] for t in range(NT)], kcol0, N, NT))
            items.append(lambda: proj_fm("K2", 2, kBT, lambda g: [kBTB[pos_of(t)] for t in range(NT)], kcol0, N, NT))
            if kind != "halo":
                items.append(lambda: proj_fm("Q1", 4, qAT, lambda g: [qATB[g]], 0, N, NT))
                items.append(lambda: proj_fm("Q2", 4, qBT, lambda g: [qBTB[g]], 0, N, NT))

            def after_tile(t):
                if kind == "main":
                    r = t * 128
                    dma("pool", ak_p.ap()[r:r + 128, :], stg[:, 0:512], "stg", stgBL, [])
                    dma("pool", av_p.ap()[r:r + 128, :], stg[:, 640:1152], "stg", stgBL, [])
                    if t == 3:
                        dma("pool", bk_p.ap(), stg[:, 512:640], "stg", stgBL, [])
                        dma("pool", bv_p.ap(), stg[:, 1152:1280], "stg", stgBL, [])
                else:
                    for s_ in range(4):
                        ps_ = slice(32 * s_, 32 * s_ + 32)
                        dma("pool", ak_s.ap()[s_, 480:512, :], stg[ps_, 0:512], "stg", stgBL, [])
                        dma("pool", av_s.ap()[s_, 480:512, :], stg[ps_, 640:1152], "stg", stgBL, [])
                        dma("pool", bk_s.ap()[s_, 96:128, :], stg[ps_, 512:640], "stg", stgBL, [])
                        dma("pool", bv_s.ap()[s_, 96:128, :], stg[ps_, 1152:1280], "stg", stgBL, [])

            pv = proj_v_make(pos_of, cacheout, after_tile)
            for t in range(NT):
                items.append(lambda t=t: pv(t))
            return items

        def do_rest(kind, bi, next_items):
            sample, NT, N, row0, gbase, pos_of, kcol0, cacheout = blk(kind, bi)
            load_ln(0)

            if not sample:
                for pi in range(4):
                    g = gbase + pi
                    qc = slice(pi * 128, (pi + 1) * 128)

                    def kcolsA(kt, g=g):
                        p = (g - 4 + kt) % 8
                        return slice(p * 128, (p + 1) * 128), p

                    def kcolsB(kt, g=g):
                        p = (g - 1 + kt) % 8
                        return slice(p * 128, (p + 1) * 128), p

                    haloA = set(kt for kt in range(5) if bi == 0 and pi + kt < 4)
                    haloB = set(kt for kt in range(2) if bi == 0 and 3 + pi + kt < 4)
                    yy = attn_pair(
                        qa=lambda h, qc=qc: qAT[(h % 2) * 64:(h % 2) * 64 + 64, h // 2, qc],
                        ka=lambda h, kt, f=kcolsA: kAT[(h % 2) * 64:(h % 2) * 64 + 64, h // 2, f(kt)[0]],
                        va=lambda kt, h, f=kcolsA: VA[:, f(kt)[1], h, :],
                        ta=lambda hg, kt: EBA[:, kt, 4 * hg:4 * hg + 4, :].rearrange("p h q -> p (h q)"),
                        qb=lambda kv, half, qc=qc: qBT[half * 64:half * 64 + 64, 2 * kv:2 * kv + 2, qc],
                        kb=lambda kv, half, kt, f=kcolsB: kBT[half * 64:half * 64 + 64, kv, f(kt)[0]],
                        vb=lambda kt, kv, f=kcolsB: VB[:, f(kt)[1], kv, :],
                        tb=lambda hf, kt: EBB[:, kt, 4 * hf:4 * hf + 4, :].rearrange("p h q -> p (h q)"),
                        haloA=haloA, haloB=haloB,
                        rdA=lambda kt, f=kcolsA: [kATB[f(kt)[1]], VAB[f(kt)[1]]] + qATB,
                        rdB=lambda kt, f=kcolsB: [kBTB[f(kt)[1]], VBB[f(kt)[1]]] + qBTB,
                    )
                    y_transposes(yy, yaT[:, :, qc], ybT[:, :, qc], [yaTB[pi]], [ybTB[pi]], 128)
            else:
                slA, slAB = wget("TVA")
                slB_, slBB = wget("TVB")
                wa = slA[:, 0:4096].rearrange("p (k n) -> p k n", k=8)
                wb = slB_[:, 0:1024].rearrange("p (k n) -> p k n", k=8)
                memset("dve", kAT[:, :, 544:640], 0.0, [kATB[4]])
                memset("dve", kBT[:, :, 160:256], 0.0, [kBTB[1]])
                for s in range(4):
                    dma("pool", ckb[:, :, :], cak.ap()[s].rearrange("(t p) f -> p t f", p=128), "ckb", [], mgTB[0:4])
                    dma("pool", ckbb[:, :], cbkd.ap()[s], "ckbb", [], [mgTB[4]])
                    for c2 in range(2):
                        bk = nbank()
                        for cc in range(2):
                            c = 2 * c2 + cc
                            for t in range(4):
                                tr(bbf(bk)[:, (cc * 4 + t) * 128:(cc * 4 + t + 1) * 128], ckb[:, t, c * 128:(c + 1) * 128],
                                   identb[:, :], mgTB[0:4] + [constB], [bankB[bk]], inc=(cc == 1 and t == 3))
                        act(kAT[:, 2 * c2:2 * c2 + 2, 0:512], bbf(bk).rearrange("p (c n) -> p c n", c=2), AF.Copy,
                            [bankB[bk]], kATB[0:4])
                    bk = nbank()
                    for kv in range(2):
                        tr(bbf(bk)[:, kv * 128:(kv + 1) * 128], ckbb[:, kv * 128:(kv + 1) * 128], identb[:, :],
                           [mgTB[4], constB], [bankB[bk]], inc=(kv == 1))
                    act(kBT[:, :, 0:128], bbf(bk)[:, 0:256].rearrange("p (c n) -> p c n", c=2), AF.Copy,
                        [bankB[bk]], [kBTB[0]])
                    act(kAT[:, :, 512:544], kAT[:, :, 896 + 32 * s:928 + 32 * s], AF.Copy, [kATB[7]], [kATB[4]])
                    act(kBT[:, :, 128:160], kBT[:, :, 896 + 32 * s:928 + 32 * s], AF.Copy, [kBTB[7]], [kBTB[1]])
                    for t4 in range(4):
                        dma("pool", VA[:, t4, :, 0:64],
                            cav.ap()[s, t4 * 128:(t4 + 1) * 128, :].rearrange("p (h d) -> p h d", h=8),
                            "ckv", [], VAB[0:4])
                    dma("pool", VB[:, 0, :, 0:64], cbv.ap()[s].rearrange("p (h d) -> p h d", h=2), "ckvb", [], [VBB[0]])
                    ba, bb_ = nbank(), nbank()
                    for kc in range(8):
                        mm(banks[ba][:, 0:512], xT[:, kc, 32 * s:32 * s + 128], wa[:, kc, :], kc == 0, kc == 7,
                           [slAB] + xTB[0:2], [bankB[ba]])
                    for kc in range(8):
                        mm(banks[bb_][:, 0:128], xT[:, kc, 32 * s:32 * s + 128], wb[:, kc, :], kc == 0, kc == 7,
                           [slBB] + xTB[0:2], [bankB[bb_]])
                    act(VA[:, 4, :, 0:64], banks[ba][:, 0:512].rearrange("p (h d) -> p h d", h=8), AF.Copy,
                        [bankB[ba]], [VAB[4]])
                    act(VB[:, 1, :, 0:64], banks[bb_][:, 0:128].rearrange("p (h d) -> p h d", h=2), AF.Copy,
                        [bankB[bb_]], [VBB[1]])
                    qc = slice(32 * s, 32 * s + 128)
                    yy = attn_pair(
                        qa=lambda h, qc=qc: qAT[(h % 2) * 64:(h % 2) * 64 + 64, h // 2, qc],
                        ka=lambda h, kt: kAT[(h % 2) * 64:(h % 2) * 64 + 64, h // 2, kt * 128:(kt + 1) * 128],
                        va=lambda kt, h: VA[:, kt, h, :],
                        ta=lambda hg, kt: (EBA[:, kt, 4 * hg:4 * hg + 4, :] if kt < 4 else
                                           EBAs[:, 4 * hg:4 * hg + 4, :]).rearrange("p h q -> p (h q)"),
                        qb=lambda kv, half, qc=qc: qBT[half * 64:half * 64 + 64, 2 * kv:2 * kv + 2, qc],
                        kb=lambda kv, half, kt: kBT[half * 64:half * 64 + 64, kv, kt * 128:(kt + 1) * 128],
                        vb=lambda kt, kv: VB[:, kt, kv, :],
                        tb=lambda hf, kt: (EBB[:, 0, 4 * hf:4 * hf + 4, :] if kt == 0 else
                                           EBBs[:, 4 * hf:4 * hf + 4, :]).rearrange("p h q -> p (h q)"),
                        haloA=set(), haloB=set(),
                        rdA=lambda kt: [kATB[kt], VAB[kt]] + qATB,
                        rdB=lambda kt: [kBTB[kt], VBB[kt]] + qBTB,
                    )
                    y_transposes(yy, yaT[:, :, 32 * s:32 * s + 32], ybT[:, :, 32 * s:32 * s + 32], [yaTB[0]], [ybTB[0]], 32)

            for j in range(8):
                sl, slB = wget("D%d" % j)
                gaw = sl[:, 0:1024].rearrange("p (k n) -> p k n", k=8)
                gbw = sl[:, 1024:2048].rearrange("p (k n) -> p k n", k=8)
                paw = sl[:, 2048:2560].rearrange("p (k n) -> p k n", k=4)
                pbw = sl[:, 2560:3072].rearrange("p (k n) -> p k n", k=4)
                bga, bgb, bpa, bpb = nbank(), nbank(), nbank(), nbank()
                for kc in range(8):
                    mm(banks[bga][:, 0:N], gaw[:, kc, :], xT[:, kc, 0:N], kc == 0, kc == 7, [slB] + xTB[:NT], [bankB[bga]])
                for kc in range(8):
                    mm(banks[bgb][:, 0:N], gbw[:, kc, :], xT[:, kc, 0:N], kc == 0, kc == 7, [slB] + xTB[:NT], [bankB[bgb]])
                for kc in range(4):
                    mm(banks[bpa][:, 0:N], paw[:, kc, :], yaT[:, kc, 0:N], kc == 0, kc == 3, [slB] + yaTB[:NT], [bankB[bpa]])
                for kc in range(4):
                    mm(banks[bpb][:, 0:N], pbw[:, kc, :], ybT[:, kc, 0:N], kc == 0, kc == 3, [slB] + ybTB[:NT], [bankB[bpb]])
                act(sga[:, 0:N], banks[bga][:, 0:N], AF.Sigmoid, [bankB[bga]], [sgaB])
                act(sgb[:, 0:N], banks[bgb][:, 0:N], AF.Sigmoid, [bankB[bgb]], [sgbB])
                tt("dve", sga[:, 0:N], sga[:, 0:N], banks[bpa][:, 0:N], ALU.mult, [sgaB, bankB[bpa]], [sgaB])
                tt("dve", sgb[:, 0:N], sgb[:, 0:N], banks[bpb][:, 0:N], ALU.mult, [sgbB, bankB[bpb]], [sgbB])
                tt("dve", mgT[:, j, 0:N], sga[:, 0:N], sgb[:, 0:N], ALU.add, [sgaB, sgbB], [mgTB[j]])

            wo0, wo0B = wget("WO0")
            wo1, wo1B = wget("WO1")
            wov = [wo0[:, :].rearrange("p (k n) -> p k n", k=4), wo1[:, :].rearrange("p (k n) -> p k n", k=4)]
            AXX = mybir.AxisListType.X
            lgall = rt[:, 0:NT * 36].rearrange("p (t n) -> p t n", t=NT)
            wo_banks = {}

            def wo_mm(t):
                tok = slice(t * 128, (t + 1) * 128)
                bo = [nbank(), nbank()]
                wo_banks[t] = bo
                for nh in range(2):
                    for kc in range(8):
                        mm(banks[bo[nh]][:, :], mgT[:, kc, tok], wov[kc // 4][:, kc % 4, nh * 512:(nh + 1) * 512],
                           kc == 0, kc == 7, [wo0B, wo1B] + mgTB, [bankB[bo[nh]]])

            def ln_part(t):
                bo = wo_banks[t]
                xi = load_x(row0 + t * 128)
                for nh in range(2):
                    S.op("dve", lambda e, nh=nh, xi=xi, bo=bo: e.scalar_tensor_tensor(
                        out=xt1[:, nh * 512:(nh + 1) * 512], in0=xf[xi][:, nh * 512:(nh + 1) * 512], scalar=ALPHA,
                        in1=banks[bo[nh]][:, :], op0=ALU.mult, op1=ALU.add),
                        reads=[xfB[xi], bankB[bo[nh]]], writes=[xt1B])
                layer_norm(xt1[:, :], xt1B, 0, xt1[:, :], xt1B)
                act(Yacc[:, t, :], xt1[:, :], AF.Copy, [xt1B], [YaccB[t]], scale=ALPHA)

            def part2(t):
                tok = slice(t * 128, (t + 1) * 128)
                bt = [nbank(), nbank()]
                for kc in range(8):
                    tr(banks[bt[kc // 4]][:, (kc % 4) * 128:(kc % 4 + 1) * 128], xt1[:, kc * 128:(kc + 1) * 128],
                       identf[:, :], [xt1B, constB], [bankB[bt[kc // 4]]], inc=(kc % 4 == 3))
                for hb in range(2):
                    bv = banks[bt[hb]][:, :].rearrange("p (k n) -> p k n", k=4)
                    act(x1T[:, hb * 4:(hb + 1) * 4, tok], bv, AF.Copy, [bankB[bt[hb]]], [x1TB[t]])
                    tcopy("dve", x1Tf[:, hb * 4:(hb + 1) * 4, :], bv, [bankB[bt[hb]]], [tmB[0], tmB[1]])
                bl = nbank()
                for kc in range(8):
                    mm(banks[bl][:, 0:36], x1Tf[:, kc, :], wr[:, kc, :], kc == 0, kc == 7, [tmB[0], tmB[1], constB], [bankB[bl]])
                tt("dve", lgall[:, t, :], banks[bl][:, 0:36], rb[:, :], ALU.add, [bankB[bl], constB], [rtB])

            wo_mm(0)
            for t in range(NT):
                ln_part(t)
                if t + 1 < NT:
                    wo_mm(t + 1)
                part2(t)

            o = NT * 36

            def rsl(n):
                nonlocal o
                v = rt[:, o:o + n]
                o += n
                return v

            G = lgall[:, :, 0:4]
            E = lgall[:, :, 4:36]
            gmax = rsl(NT)
            goh = rsl(NT * 4).rearrange("p (t n) -> p t n", t=NT)
            gex = rsl(NT * 4).rearrange("p (t n) -> p t n", t=NT)
            gsum = rsl(NT)
            gw = rsl(NT)
            gpen = rsl(NT * 4)
            em = rsl(NT * 32)
            m8 = rsl(NT * 8).rearrange("p (t n) -> p t n", t=NT)
            oh1 = rsl(NT * 32).rearrange("p (t n) -> p t n", t=NT)
            oh2 = rsl(NT * 32).rearrange("p (t n) -> p t n", t=NT)
            dlt = rsl(NT)
            w1_ = rsl(NT)
            w2_ = rsl(NT)
            R = [rtB]
            bc4 = lambda v: v.unsqueeze(2).broadcast_to([128, NT, 4])
            bc32 = lambda v: v.unsqueeze(2).broadcast_to([128, NT, 32])
            S.op("dve", lambda e: e.tensor_reduce(out=gmax, in_=G, axis=AXX, op=ALU.max), reads=R, writes=R)
            tt("dve", goh, G, bc4(gmax), ALU.is_equal, R, R)
            tt("dve", gex, G, bc4(gmax), ALU.subtract, R, R)
            act(gex, gex, AF.Exp, R, R)
            S.op("dve", lambda e: e.tensor_reduce(out=gsum, in_=gex, axis=AXX, op=ALU.add), reads=R, writes=R)
            S.op("dve", lambda e: e.reciprocal(out=gw, in_=gsum), reads=R, writes=R)
            ts("dve", gpen, goh.rearrange("p t n -> p (t n)"), -1.0, 1e30, ALU.add, ALU.mult, R, R)
            for t in range(NT):
                tt("dve", em[:, t * 32:(t + 1) * 32].rearrange("p (g e) -> p g e", g=4),
                   lgall[:, t, 4:36].rearrange("p (g e) -> p g e", g=4),
                   gpen[:, t * 4:(t + 1) * 4].unsqueeze(2).broadcast_to([128, 4, 8]), ALU.add, R, R)
            for t in range(NT):
                S.op("dve", lambda e, t=t: e.max(out=m8[:, t, :], in_=em[:, t * 32:(t + 1) * 32]), reads=R, writes=R)
            emv = em.rearrange("p (t n) -> p t n", t=NT)
            tt("dve", oh1, emv, bc32(m8[:, :, 0]), ALU.is_equal, R, R)
            tt("dve", oh2, emv, bc32(m8[:, :, 1]), ALU.is_equal, R, R)
            tt("dve", dlt, m8[:, :, 1], m8[:, :, 0], ALU.subtract, R, R)
            act(dlt, dlt, AF.Exp, R, R)
            ts("dve", dlt, dlt, 1.0, None, ALU.add, None, R, R)
            S.op("dve", lambda e: e.reciprocal(out=w1_, in_=dlt), reads=R, writes=R)
            tt("dve", w1_, w1_, gw, ALU.mult, R, R)
            tt("dve", w2_, gw, w1_, ALU.subtract, R, R)
            tt("dve", oh1, oh1, bc32(w1_), ALU.mult, R, R)
            tt("dve", oh2, oh2, bc32(w2_), ALU.mult, R, R)
            tt("dve", oh1, oh1, oh2, ALU.add, R, R)
            for t in range(NT):
                bc_ = nbank()
                tr(banks[bc_][0:32, 0:128], oh1[:, t, :], identf[:, :], [rtB, constB], [bankB[bc_]])
                act(combT[:, t * 128:(t + 1) * 128], banks[bc_][0:32, 0:128], AF.Copy, [bankB[bc_]], [combTB[t]])

            load_ln(1)
            prow0 = (bi * 512) if kind == "main" else SEG
            for t in range(NT):
                dma("sp", pf[:, :], pin.ap()[prow0 + t * 128:prow0 + (t + 1) * 128, :], "pf", [], [pfB])
                act(pbt[:, :], pf[:, :], AF.Copy, [pfB], [pbtB])
                bk = nbank()
                for c in range(2):
                    tr(bbf(bk)[:, c * 128:(c + 1) * 128], pbt[:, c * 128:(c + 1) * 128], identb[:, :], [pbtB, constB],
                       [bankB[bk]], inc=(c == 1))
                act(pT[:, :, t * 128:(t + 1) * 128], bbf(bk)[:, 0:256].rearrange("p (c n) -> p c n", c=2), AF.Copy,
                    [bankB[bk]], [pTB[t]])
            for eg in range(16):
                wslots = []
                for ee in range(2):
                    e_ = 2 * eg + ee
                    ea, eaB = wget("EA%d" % e_)
                    eb, ebB = wget("EB%d" % e_)
                    wslots.append((eb, ebB))
                    w1v = ea[:, 0:2048].rearrange("p (k n) -> p k n", k=8)
                    w3v = ea[:, 2048:4096].rearrange("p (k n) -> p k n", k=8)
                    ci = e_ % 2
                    hb_ = []
                    for hc in range(2):
                        bh1, bh3 = nbank(), nbank()
                        hb_.append((bh1, bh3))
                        for kc in range(8):
                            mm(banks[bh1][:, 0:N], w1v[:, kc, hc * 128:(hc + 1) * 128], x1T[:, kc, 0:N], kc == 0, kc == 7,
                               [eaB] + x1TB[:NT], [bankB[bh1]])
                        for kc in range(8):
                            mm(banks[bh3][:, 0:N], w3v[:, kc, hc * 128:(hc + 1) * 128], x1T[:, kc, 0:N], kc == 0, kc == 7,
                               [eaB] + x1TB[:NT], [bankB[bh3]])
                    bcb = nbank()
                    act(cm[:, ci, 0:N], combT[:, 0:N], AF.Copy, combTB[:NT] + [constB], [cmB[ci]], scale=identf[0:32, e_:e_ + 1])
                    mm(banks[bcb][:, 0:N], ones32[:, :], cm[:, ci, 0:N], True, True, [cmB[ci], constB], [bankB[bcb]])
                    for hc in range(2):
                        bh1, bh3 = hb_[hc]
                        act(s1[hc][:, 0:N], banks[bh1][:, 0:N], AF.Silu, [bankB[bh1]], [s1B[hc]])
                        tt("dve", tm[hc][:, 0:N], s1[hc][:, 0:N], banks[bh3][:, 0:N], ALU.mult, [s1B[hc], bankB[bh3]], [tmB[hc]])
                        tt("dve", hdn[:, ee, hc, 0:N], tm[hc][:, 0:N], banks[bcb][:, 0:N], ALU.mult,
                           [tmB[hc], bankB[bcb]], [hdnB[ee][hc]])
                for t in range(NT):
                    tok = slice(t * 128, (t + 1) * 128)
                    for nh in range(2):
                        by = nbank()
                        i = 0
                        for ee in range(2):
                            w2v = wslots[ee][0][:, 0:2048].rearrange("p (k n) -> p k n", k=2)
                            for hc in range(2):
                                mm(banks[by][:, :], hdn[:, ee, hc, tok], w2v[:, hc, nh * 512:(nh + 1) * 512], i == 0, i == 3,
                                   [hdnB[ee][hc], wslots[ee][1]], [bankB[by]])
                                i += 1
                        tt("dve", Yacc[:, t, nh * 512:(nh + 1) * 512], Yacc[:, t, nh * 512:(nh + 1) * 512], banks[by][:, :],
                           ALU.add, [YaccB[t], bankB[by]], [YaccB[t]])

            pg0, pg0B = wget("PG0")
            pg1, pg1B = wget("PG1")
            pe_, peB = wget("PE")
            pgv = [pg0[:, :].rearrange("p (k n) -> p k n", k=4), pg1[:, :].rearrange("p (k n) -> p k n", k=4)]
            pev = pe_[:, 0:2048].rearrange("p (k n) -> p k n", k=2)
            for t in range(NT):
                tok = slice(t * 128, (t + 1) * 128)
                for nh in range(2):
                    bg, bp = nbank(), nbank()
                    for kc in range(8):
                        mm(banks[bg][:, :], x1T[:, kc, tok], pgv[kc // 4][:, kc % 4, nh * 512:(nh + 1) * 512], kc == 0, kc == 7,
                           [pg0B, pg1B, x1TB[t]], [bankB[bg]])
                    for kc in range(2):
                        mm(banks[bp][:, :], pT[:, kc, tok], pev[:, kc, nh * 512:(nh + 1) * 512], kc == 0, kc == 1,
                           [peB, pTB[t]], [bankB[bp]])
                    act(sga[:, :], banks[bg][:, :], AF.Sigmoid, [bankB[bg]], [sgaB])
                    tt("dve", sga[:, :], sga[:, :], banks[bp][:, :], ALU.mult, [sgaB, bankB[bp]], [sgaB])
                    tt("dve", Yacc[:, t, nh * 512:(nh + 1) * 512], Yacc[:, t, nh * 512:(nh + 1) * 512], sga[:, :], ALU.add,
                       [YaccB[t], sgaB], [YaccB[t]])

            def ln2_tile(t):
                layer_norm(Yacc[:, t, :], YaccB[t], 2, Yacc[:, t, :], YaccB[t])
                if kind == "main":
                    dst = y_p.ap()[bi * 512 + t * 128:bi * 512 + (t + 1) * 128, :]
                else:
                    dst = y_s.ap()
                dma("pool", dst, Yacc[:, t, :], "yo%d" % t, [YaccB[t]], [])

            items = list(next_items)
            tl = list(range(NT))
            while items or tl:
                for _ in range(2):
                    if items:
                        items.pop(0)()
                if tl:
                    ln2_tile(tl.pop(0))

        for it in stageB_items("halo", -1):
            it()
        for it in stageB_items("main", 0):
            it()
        for bi in range(NBLK):
            nxt = stageB_items("main", bi + 1) if bi + 1 < NBLK else stageB_items("sample", 0)
            do_rest("main", bi, nxt)
            if bi == 1:
                dma("sp", ak_s.ap()[:, 0:480, :], cak.ap()[:, 32:512, :], "cc", [], [])
                dma("sp", av_s.ap()[:, 0:480, :], cav.ap()[:, 32:512, :], "cc", [], [])
                dma("sp", bk_s.ap()[:, 0:96, :], cbk.ap()[:, 32:128, :], "cc", [], [])
                dma("sp", bv_s.ap()[:, 0:96, :], cbv.ap()[:, 32:128, :], "cc", [], [])
        do_rest("sample", 0, [])
        fin = [(k, S.cnt[k]) for k in ["cc", "stg", "yo0", "yo1", "yo2", "yo3"]]
        S.wait_all("pool", fin)
        S.emit()
    return nc


def _consts():
    ar = np.arange
    m = ar(768)
    idxA = np.clip(639 - m, -128, 128) + 128
    ohA = np.zeros((384, 768), np.float32)
    ohA[idxA[:767], m[:767]] = 8.0
    mb = ar(384)
    idxB = _t5_bucket(255 - mb)
    ohB = np.zeros((32, 384), np.float32)
    ohB[idxB[:383], mb[:383]] = 8.0
    qp = 127 - ar(128)[:, None]
    j = ar(640)[None, :]
    validA = np.where(qp < 64, j < 576, j >= 64)
    maskA = np.where(validA, 0.0, MASKV).astype(np.float32)
    maskAs = np.broadcast_to(np.where(ar(128)[None, :] < 32, 0.0, MASKV), (128, 128)).astype(np.float32)
    jb = ar(256)[None, :]
    validB = np.where(qp < 64, jb < 192, jb >= 64)
    maskB = np.where(validB, 0.0, MASKV).astype(np.float32)
    maskBs = maskAs.copy()
    return dict(ohA=np.ascontiguousarray(ohA.reshape(3, 128, 768).transpose(1, 0, 2).reshape(128, 3 * 768)),
                ohB=ohB, maskA=maskA, maskAs=np.ascontiguousarray(maskAs), maskB=maskB, maskBs=maskBs)


def kernel(x_prompt, x_sample, p_prompt, p_sample, cache_a_k, cache_a_v, cache_b_k, cache_b_v, w_in,
           a_rel_table, b_sinks, w_pa, w_pb, w_o, ln1_g, ln1_b, w_rg, b_rg, w_re, b_re, w1, w3, w2, w_pe,
           w_pg, ln2_g, ln2_b, t5_table):
    f = lambda a: np.ascontiguousarray(np.asarray(a, dtype=np.float32))
    x_prompt, x_sample, p_prompt, p_sample = f(x_prompt), f(x_sample), f(p_prompt), f(p_sample)
    cache_a_k, cache_a_v, cache_b_k, cache_b_v = f(cache_a_k), f(cache_a_v), f(cache_b_k), f(cache_b_v)
    W = _pack_weights(f(w_in)[0], f(w_pa)[0], f(w_pb)[0], f(w_o)[0], f(w1)[0], f(w3)[0], f(w2)[0], f(w_pe)[0], f(w_pg)[0])
    cst = _consts()
    wr = np.concatenate([f(w_rg)[0], f(w_re)[0]], axis=1)
    wr = np.ascontiguousarray(wr.reshape(8, 128, 36).transpose(1, 0, 2).reshape(128, 8 * 36))
    rb = np.concatenate([f(b_rg)[0], f(b_re)[0]])[None, :]
    lnp = np.concatenate([f(ln1_g)[0], f(ln1_b)[0], f(ln2_g)[0], f(ln2_b)[0]])[None, :]
    tabA = np.zeros((384, 8), np.float32)
    tabA[:257] = f(a_rel_table)[0]
    tabA = np.ascontiguousarray(tabA.reshape(3, 128, 8).transpose(1, 0, 2).reshape(128, 24))
    shared = dict(w32=W, wr=wr, rb=np.ascontiguousarray(rb), lnp=np.ascontiguousarray(lnp), sinks=f(b_sinks),
                  tabA=tabA, ohA=cst["ohA"], tabB=f(t5_table), ohB=cst["ohB"], maskA=cst["maskA"],
                  maskAs=cst["maskAs"], maskB=cst["maskB"], maskBs=cst["maskBs"])
    in_maps = []
    for c in range(NCORES):
        b, sgm = c // 4, c % 4
        t0 = sgm * SEG
        xin = np.zeros((HALO + SEG + NS, D), np.float32)
        if sgm > 0:
            xin[0:HALO] = x_prompt[b, t0 - HALO:t0]
        xin[HALO:HALO + SEG] = x_prompt[b, t0:t0 + SEG]
        xin[HALO + SEG:] = x_sample[4 * c:4 * c + 4].reshape(NS, D)
        pin = np.concatenate([p_prompt[0, b, t0:t0 + SEG], p_sample[0, 4 * c:4 * c + 4].reshape(NS, 256)], 0)
        cbk_ = cache_b_k[0, 4 * c:4 * c + 4].reshape(4, 128, 2, 64)
        cbkd = np.concatenate([cbk_[:, :, 0], cbk_[:, :, 0], cbk_[:, :, 1], cbk_[:, :, 1]], axis=-1)
        hm = np.full((1, 128), MASKV if sgm == 0 else 0.0, np.float32)
        m = dict(shared)
        m.update(xin=xin, pin=np.ascontiguousarray(pin),
                 cak=np.ascontiguousarray(cache_a_k[0, 4 * c:4 * c + 4].reshape(4, 512, 512)),
                 cav=np.ascontiguousarray(cache_a_v[0, 4 * c:4 * c + 4].reshape(4, 512, 512)),
                 cbkd=np.ascontiguousarray(cbkd),
                 cbk=np.ascontiguousarray(cache_b_k[0, 4 * c:4 * c + 4].reshape(4, 128, 128)),
                 cbv=np.ascontiguousarray(cache_b_v[0, 4 * c:4 * c + 4].reshape(4, 128, 128)), hm=hm)
        in_maps.append(m)
    nc = build_program()
    res = run_bass_kernel_spmd(nc, in_maps, core_ids=list(range(NCORES)))
    R = res.results
    y_prompt = np.stack([np.concatenate([R[4 * b + s]["y_p"] for s in range(4)], 0) for b in range(2)], 0)
    y_sample = np.concatenate([R[c]["y_s"].reshape(4, 32, D) for c in range(NCORES)], 0)
    pak = np.stack([R[4 * b + 3]["ak_p"].reshape(512, 8, 64) for b in range(2)], 0)[None]
    pav = np.stack([R[4 * b + 3]["av_p"].reshape(512, 8, 64) for b in range(2)], 0)[None]
    pbk = np.stack([R[4 * b + 3]["bk_p"].reshape(128, 2, 64) for b in range(2)], 0)[None]
    pbv = np.stack([R[4 * b + 3]["bv_p"].reshape(128, 2, 64) for b in range(2)], 0)[None]
    sak = np.concatenate([R[c]["ak_s"].reshape(4, 512, 8, 64) for c in range(NCORES)], 0)[None]
    sav = np.concatenate([R[c]["av_s"].reshape(4, 512, 8, 64) for c in range(NCORES)], 0)[None]
    sbk = np.concatenate([R[c]["bk_s"].reshape(4, 128, 2, 64) for c in range(NCORES)], 0)[None]
    sbv = np.concatenate([R[c]["bv_s"].reshape(4, 128, 2, 64) for c in range(NCORES)], 0)[None]
    out = (y_prompt, y_sample, pak, pav, pbk, pbv, sak, sav, sbk, sbv)
    return tuple(np.ascontiguousarray(o, dtype=np.float32) for o in out)
```
